# Optimizing a Trainium2 kernel written in Bass

```python
import math
import jax, jax.numpy as jnp
from jax import lax
import numpy as np

D_MODEL = 2048
BATCH = 16
SEQ = 2048
DEPTH = 2

N_A = DEPTH // 2
N_B = DEPTH - N_A
N_DENSE = (DEPTH + 1) // 2
N_MOE = DEPTH // 2

SSM_EXPAND = 2
D_INNER = SSM_EXPAND * D_MODEL
SSM_HEAD_DIM = 64
SSM_HEADS = D_INNER // SSM_HEAD_DIM
SSM_GROUPS = 8
SSM_HEADS_PER_GROUP = SSM_HEADS // SSM_GROUPS
SSM_STATE = 128
CONV_K = 4
CONV_DIM = D_INNER + 2 * SSM_GROUPS * SSM_STATE
D_IN_PROJ = D_INNER + CONV_DIM + SSM_HEADS
SSM_CHUNK = 128
DT_MIN = 0.001
DT_MAX = 0.1

MLA_HEADS = 16
Q_RANK = 512
KV_RANK = 512
NOPE_DIM = 128
ROPE_DIM = 64
V_DIM = 128
ROPE_THETA = 10000.0
ATTN_BLOCK = 128

D_FF = 5632
N_EXPERTS = 8
TOP_K = 2
D_FF_EXPERT = 2816

PLE_DIM = 256
DEEPNORM_ALPHA = (2.0 * DEPTH) ** 0.25
DEEPNORM_BETA = (8.0 * DEPTH) ** -0.25
LN_EPS = 1e-5
RMS_EPS = 1e-6

kernel_name = "yoco_mamba2_mla_moe_deepnorm_ple"


def layer_norm(x, g, b):
    x32 = x.astype(jnp.float32)
    mu = jnp.mean(x32, -1, keepdims=True)
    var = jnp.mean(jnp.square(x32 - mu), -1, keepdims=True)
    return ((x32 - mu) * lax.rsqrt(var + LN_EPS) * g + b).astype(x.dtype)


def rms_norm(x, g):
    x32 = x.astype(jnp.float32)
    return (x32 * lax.rsqrt(jnp.mean(jnp.square(x32), -1, keepdims=True) + RMS_EPS) * g).astype(x.dtype)


def rope_tables(positions):
    inv_freq = ROPE_THETA ** (-jnp.arange(0, ROPE_DIM, 2, dtype=jnp.float32) / ROPE_DIM)
    ang = positions.astype(jnp.float32)[..., None] * inv_freq
    return jnp.cos(ang), jnp.sin(ang)


def apply_rope(x, cos, sin):
    x1, x2 = jnp.split(x.astype(jnp.float32), 2, axis=-1)
    return jnp.concatenate([x1 * cos - x2 * sin, x2 * cos + x1 * sin], -1).astype(x.dtype)


def causal_depthwise_conv(u, w, b):
    out = lax.conv_general_dilated(
        u, w[:, None, :].astype(u.dtype), window_strides=(1,), padding=[(CONV_K - 1, 0)],
        dimension_numbers=("NWC", "WIO", "NWC"), feature_group_count=u.shape[-1])
    return out + b


def ssd_chunked_scan(xdt, a, bm, cm):
    bsz, seq = xdt.shape[:2]
    nc = seq // SSM_CHUNK

    def to_chunks(t):
        t = t.astype(jnp.float32).reshape((bsz, nc, SSM_CHUNK) + t.shape[2:])
        return jnp.moveaxis(t, 1, 0)

    xs = (to_chunks(xdt), to_chunks(a), to_chunks(bm), to_chunks(cm))
    causal = np.tril(np.ones((SSM_CHUNK, SSM_CHUNK), dtype=bool))[None, :, :, None, None]

    def step(state, inp):
        xc, ac, bc, cc = inp
        acum = jnp.cumsum(ac, axis=1)
        seg = acum[:, :, None] - acum[:, None, :]
        decay = jnp.exp(jnp.where(causal, seg, -jnp.inf))
        cb = jnp.einsum('btgn,bsgn->btsg', cc, bc)
        y = jnp.einsum('btsg,btsgh,bsghp->btghp', cb, decay, xc)
        y = y + jnp.einsum('btgn,bghpn->btghp', cc, state) * jnp.exp(acum)[..., None]
        to_end = jnp.exp(acum[:, -1:] - acum)
        state = (state * jnp.exp(acum[:, -1])[..., None, None]
                 + jnp.einsum('bsgn,bsgh,bsghp->bghpn', bc, to_end, xc))
        return state, y

    state0 = jnp.zeros((bsz, SSM_GROUPS, SSM_HEADS_PER_GROUP, SSM_HEAD_DIM, SSM_STATE), jnp.float32)
    _, ys = lax.scan(step, state0, xs)
    return jnp.moveaxis(ys, 0, 1).reshape(xdt.shape)


def mamba2_mixer(h, w_in, conv_w, conv_b, dt_bias, a_log, d_skip, norm_g, w_out):
    bsz, seq, _ = h.shape
    zxbcdt = h @ w_in
    z, xbc, dt = jnp.split(zxbcdt, [D_INNER, D_INNER + CONV_DIM], axis=-1)
    xbc = jax.nn.silu(causal_depthwise_conv(xbc, conv_w, conv_b))
    xs, bm, cm = jnp.split(xbc, [D_INNER, D_INNER + SSM_GROUPS * SSM_STATE], axis=-1)
    xs = xs.reshape(bsz, seq, SSM_GROUPS, SSM_HEADS_PER_GROUP, SSM_HEAD_DIM).astype(jnp.float32)
    bm = bm.reshape(bsz, seq, SSM_GROUPS, SSM_STATE)
    cm = cm.reshape(bsz, seq, SSM_GROUPS, SSM_STATE)
    dt = jax.nn.softplus(dt.astype(jnp.float32) + dt_bias.astype(jnp.float32))
    dt = dt.reshape(bsz, seq, SSM_GROUPS, SSM_HEADS_PER_GROUP)
    a = -jnp.exp(a_log.astype(jnp.float32)).reshape(SSM_GROUPS, SSM_HEADS_PER_GROUP)
    y = ssd_chunked_scan(xs * dt[..., None], a * dt, bm, cm)
    y = y + d_skip.astype(jnp.float32).reshape(SSM_GROUPS, SSM_HEADS_PER_GROUP)[..., None] * xs
    gsz = D_INNER // SSM_GROUPS
    y = y.reshape(bsz, seq, SSM_GROUPS, gsz) * jax.nn.silu(z.astype(jnp.float32)).reshape(bsz, seq, SSM_GROUPS, gsz)
    y = rms_norm(y, norm_g.reshape(SSM_GROUPS, gsz)).reshape(bsz, seq, D_INNER)
    return y.astype(h.dtype) @ w_out


def shared_latent_kv(h, w_down, norm_g, w_rope, w_uk, w_uv, cos, sin):
    bsz, seq, _ = h.shape
    c_kv = rms_norm(h @ w_down, norm_g)
    k_nope = (c_kv @ w_uk).reshape(bsz, seq, MLA_HEADS, NOPE_DIM)
    v = (c_kv @ w_uv).reshape(bsz, seq, MLA_HEADS, V_DIM)
    k_rope = apply_rope(h @ w_rope, cos, sin)
    return k_nope, k_rope, v


def mla_attention(h, k_nope, k_rope, v, w_dq, q_norm_g, w_uq, w_o, cos, sin):
    bsz, seq, _ = h.shape
    q = (rms_norm(h @ w_dq, q_norm_g) @ w_uq).reshape(bsz, seq, MLA_HEADS, NOPE_DIM + ROPE_DIM)
    q_nope, q_rope = jnp.split(q, [NOPE_DIM], axis=-1)
    q_rope = apply_rope(q_rope, cos[:, :, None], sin[:, :, None])
    scale = (NOPE_DIM + ROPE_DIM) ** -0.5
    outs = []
    for start in range(0, seq, ATTN_BLOCK):
        end = start + ATTN_BLOCK
        s = (jnp.einsum('bqhd,bkhd->bhqk', q_nope[:, start:end], k_nope[:, :end])
             + jnp.einsum('bqhr,bkr->bhqk', q_rope[:, start:end], k_rope[:, :end])).astype(jnp.float32) * scale
        mask = (start + np.arange(ATTN_BLOCK))[:, None] >= np.arange(end)[None, :]
        probs = jax.nn.softmax(jnp.where(mask, s, -jnp.inf), axis=-1).astype(v.dtype)
        outs.append(jnp.einsum('bhqk,bkhd->bqhd', probs, v[:, :end]))
    o = jnp.concatenate(outs, axis=1).reshape(bsz, seq, MLA_HEADS * V_DIM)
    return o @ w_o


def swiglu(h, w_gate, w_up, w_down):
    return (jax.nn.silu(h @ w_gate) * (h @ w_up)) @ w_down


def moe_swiglu(h, w_router, b_router, w_gate, w_up, w_down):
    logits = (h @ w_router).astype(jnp.float32) + b_router.astype(jnp.float32)
    top_val, top_idx = lax.top_k(logits, TOP_K)
    top_w = jax.nn.softmax(top_val, axis=-1)
    combine = jnp.sum(jax.nn.one_hot(top_idx, N_EXPERTS, dtype=jnp.float32) * top_w[..., None], axis=-2)
    out = jnp.zeros_like(h)
    for e in range(N_EXPERTS):
        out = out + combine[..., e:e + 1].astype(h.dtype) * swiglu(h, w_gate[e], w_up[e], w_down[e])
    return out


def setup_inputs(seed: int = 0) -> dict:
    key = jax.random.key(seed)
    ks = iter(jax.random.split(key, 48))

    def normal(shape, scale):
        return jax.random.normal(next(ks), shape, jnp.float32) * scale

    def gain(shape):
        return 1.0 + normal(shape, 0.02)

    x = normal((BATCH, SEQ, D_MODEL), 1.0)
    p = normal((DEPTH, BATCH, SEQ, PLE_DIM), 1.0)
    start = jax.random.randint(next(ks), (BATCH, 1), 0, 4096, dtype=jnp.int32)
    positions = start + jnp.arange(SEQ, dtype=jnp.int32)[None, :]

    ssm_w_in = normal((N_A, D_MODEL, D_IN_PROJ), D_MODEL ** -0.5)
    ssm_conv_w = normal((N_A, CONV_K, CONV_DIM), CONV_K ** -0.5)
    ssm_conv_b = normal((N_A, CONV_DIM), 0.02)
    dt0 = jnp.exp(jax.random.uniform(next(ks), (N_A, SSM_HEADS), jnp.float32,
                                     math.log(DT_MIN), math.log(DT_MAX)))
    ssm_dt_bias = dt0 + jnp.log(-jnp.expm1(-dt0))
    ssm_a_log = jnp.log(jax.random.uniform(next(ks), (N_A, SSM_HEADS), jnp.float32, 1.0, 16.0))
    ssm_d = gain((N_A, SSM_HEADS))
    ssm_norm_g = gain((N_A, D_INNER))
    ssm_w_out = normal((N_A, D_INNER, D_MODEL), D_INNER ** -0.5 * DEEPNORM_BETA)

    kv_w_down = normal((D_MODEL, KV_RANK), D_MODEL ** -0.5)
    kv_norm_g = gain((KV_RANK,))
    kv_w_rope = normal((D_MODEL, ROPE_DIM), D_MODEL ** -0.5)
    kv_w_uk = normal((KV_RANK, MLA_HEADS * NOPE_DIM), KV_RANK ** -0.5)
    kv_w_uv = normal((KV_RANK, MLA_HEADS * V_DIM), KV_RANK ** -0.5)

    mla_w_dq = normal((N_B, D_MODEL, Q_RANK), D_MODEL ** -0.5)
    mla_q_norm_g = gain((N_B, Q_RANK))
    mla_w_uq = normal((N_B, Q_RANK, MLA_HEADS * (NOPE_DIM + ROPE_DIM)), Q_RANK ** -0.5)
    mla_w_o = normal((N_B, MLA_HEADS * V_DIM, D_MODEL), (MLA_HEADS * V_DIM) ** -0.5 * DEEPNORM_BETA)

    ffn_w_gate = normal((N_DENSE, D_MODEL, D_FF), D_MODEL ** -0.5)
    ffn_w_up = normal((N_DENSE, D_MODEL, D_FF), D_MODEL ** -0.5)
    ffn_w_down = normal((N_DENSE, D_FF, D_MODEL), D_FF ** -0.5 * DEEPNORM_BETA)

    moe_w_router = normal((N_MOE, D_MODEL, N_EXPERTS), D_MODEL ** -0.5)
    moe_b_router = normal((N_MOE, N_EXPERTS), 0.01)
    moe_w_gate = normal((N_MOE, N_EXPERTS, D_MODEL, D_FF_EXPERT), D_MODEL ** -0.5)
    moe_w_up = normal((N_MOE, N_EXPERTS, D_MODEL, D_FF_EXPERT), D_MODEL ** -0.5)
    moe_w_down = normal((N_MOE, N_EXPERTS, D_FF_EXPERT, D_MODEL), D_FF_EXPERT ** -0.5 * DEEPNORM_BETA)

    ln1_g = gain((DEPTH, D_MODEL))
    ln1_b = normal((DEPTH, D_MODEL), 0.02)
    ln2_g = gain((DEPTH, D_MODEL))
    ln2_b = normal((DEPTH, D_MODEL), 0.02)

    ple_w_proj = normal((DEPTH, PLE_DIM, D_MODEL), PLE_DIM ** -0.5)
    ple_w_gate = normal((DEPTH, D_MODEL, D_MODEL), D_MODEL ** -0.5)

    return {
        "x": x, "p": p, "positions": positions,
        "ssm_w_in": ssm_w_in, "ssm_conv_w": ssm_conv_w, "ssm_conv_b": ssm_conv_b,
        "ssm_dt_bias": ssm_dt_bias, "ssm_a_log": ssm_a_log, "ssm_d": ssm_d,
        "ssm_norm_g": ssm_norm_g, "ssm_w_out": ssm_w_out,
        "kv_w_down": kv_w_down, "kv_norm_g": kv_norm_g, "kv_w_rope": kv_w_rope,
        "kv_w_uk": kv_w_uk, "kv_w_uv": kv_w_uv,
        "mla_w_dq": mla_w_dq, "mla_q_norm_g": mla_q_norm_g, "mla_w_uq": mla_w_uq, "mla_w_o": mla_w_o,
        "ffn_w_gate": ffn_w_gate, "ffn_w_up": ffn_w_up, "ffn_w_down": ffn_w_down,
        "moe_w_router": moe_w_router, "moe_b_router": moe_b_router,
        "moe_w_gate": moe_w_gate, "moe_w_up": moe_w_up, "moe_w_down": moe_w_down,
        "ln1_g": ln1_g, "ln1_b": ln1_b, "ln2_g": ln2_g, "ln2_b": ln2_b,
        "ple_w_proj": ple_w_proj, "ple_w_gate": ple_w_gate,
    }


def reference(x, p, positions,
              ssm_w_in, ssm_conv_w, ssm_conv_b, ssm_dt_bias, ssm_a_log, ssm_d, ssm_norm_g, ssm_w_out,
              kv_w_down, kv_norm_g, kv_w_rope, kv_w_uk, kv_w_uv,
              mla_w_dq, mla_q_norm_g, mla_w_uq, mla_w_o,
              ffn_w_gate, ffn_w_up, ffn_w_down,
              moe_w_router, moe_b_router, moe_w_gate, moe_w_up, moe_w_down,
              ln1_g, ln1_b, ln2_g, ln2_b,
              ple_w_proj, ple_w_gate):
    cos, sin = rope_tables(positions)
    h = x
    k_nope = k_rope = v = None
    for i in range(DEPTH):
        if i < N_A:
            mix = mamba2_mixer(h, ssm_w_in[i], ssm_conv_w[i], ssm_conv_b[i], ssm_dt_bias[i],
                               ssm_a_log[i], ssm_d[i], ssm_norm_g[i], ssm_w_out[i])
        else:
            if i == N_A:
                k_nope, k_rope, v = shared_latent_kv(h, kv_w_down, kv_norm_g, kv_w_rope,
                                                     kv_w_uk, kv_w_uv, cos, sin)
            j = i - N_A
            mix = mla_attention(h, k_nope, k_rope, v, mla_w_dq[j], mla_q_norm_g[j],
                                mla_w_uq[j], mla_w_o[j], cos, sin)
        h = layer_norm(DEEPNORM_ALPHA * h + mix, ln1_g[i], ln1_b[i])
        j = i // 2
        if i % 2 == 0:
            ff = swiglu(h, ffn_w_gate[j], ffn_w_up[j], ffn_w_down[j])
        else:
            ff = moe_swiglu(h, moe_w_router[j], moe_b_router[j], moe_w_gate[j], moe_w_up[j], moe_w_down[j])
        h = layer_norm(DEEPNORM_ALPHA * h + ff, ln2_g[i], ln2_b[i])
        h = h + jax.nn.sigmoid(h @ ple_w_gate[i]) * (p[i] @ ple_w_proj[i])
    return h
```

```python
import math
from contextlib import ExitStack
import numpy as np
import concourse.bass as bass
import concourse.mybir as mybir
from concourse.bass_utils import run_bass_kernel_spmd

F32 = mybir.dt.float32
BF16 = mybir.dt.bfloat16
I32 = mybir.dt.int32
AF = mybir.ActivationFunctionType
ALU = mybir.AluOpType
AX = mybir.AxisListType

NCORES = 8
D = 2048
SEQ = 2048
NSEQ = 2
T = NSEQ * SEQ
DIN = 4096
NH = 64
HD = 64
NG = 8
NST = 128
CONVD = 6144
DINP = 10304
DFF = 5632
NE = 8
DFE = 2816
PLE = 256
MH = 16
QR = 512
KVR = 512
ALPHA = (2.0 * 2) ** 0.25
LN_EPS = 1e-5
RMS_EPS = 1e-6
SCALE = (128 + 64) ** -0.5
TWO_PI = 2.0 * math.pi

SAME_ENGINE_SYNC = True
EMBED_WAIT = True
KROT = 4


class Buf:
    __slots__ = ("name", "writers", "readers", "prev")

    def __init__(self, name):
        self.name = name
        self.writers = []
        self.readers = []
        self.prev = []


class Stream:
    __slots__ = ("name", "is_dma", "ops", "sems")

    def __init__(self, name, is_dma):
        self.name = name
        self.is_dma = is_dma
        self.ops = []
        self.sems = None


class Op:
    __slots__ = ("eng", "fn", "stream", "pos", "deps", "signaled", "sig")

    def __init__(self, eng, fn, stream):
        self.eng = eng
        self.fn = fn
        self.stream = stream
        self.pos = len(stream.ops)
        stream.ops.append(self)
        self.deps = []
        self.signaled = False
        self.sig = None


class Sched:
    def __init__(self, nc):
        self.nc = nc
        self.engs = {"pe": [], "act": [], "dve": [], "pool": [], "sp": []}
        self.streams = {e: Stream(e, False) for e in self.engs}
        self.dma_streams = {}
        self.waited = {e: {} for e in self.engs}

    def _dma_stream(self, key):
        s = self.dma_streams.get(key)
        if s is None:
            s = Stream("dma_" + key, True)
            self.dma_streams[key] = s
        return s

    def _add_dep(self, op, p):
        if p is op:
            return
        eng = op.eng
        if (not p.stream.is_dma) and p.stream.name == eng:
            if eng == "pe" or not SAME_ENGINE_SYNC:
                return
        if self.waited[eng].get(p.stream.name, -1) >= p.pos:
            return
        for i, q in enumerate(op.deps):
            if q.stream is p.stream:
                if q.pos < p.pos:
                    op.deps[i] = p
                return
        op.deps.append(p)

    def op(self, eng, fn, reads=(), writes=(), pwrites=(), dma_key=None, extra=()):
        stream = self._dma_stream(dma_key) if dma_key is not None else self.streams[eng]
        o = Op(eng, fn, stream)
        for b in reads:
            for p in b.writers:
                self._add_dep(o, p)
        for b in writes:
            for p in b.readers:
                self._add_dep(o, p)
            for p in b.writers:
                self._add_dep(o, p)
        for b in pwrites:
            for p in b.readers:
                self._add_dep(o, p)
            for p in b.prev:
                self._add_dep(o, p)
        for p in extra:
            self._add_dep(o, p)
        w = self.waited[eng]
        for p in o.deps:
            p.signaled = True
            if w.get(p.stream.name, -1) < p.pos:
                w[p.stream.name] = p.pos
        for b in writes:
            b.prev = self._compact(b.readers + b.writers)
            b.writers = [o]
            b.readers = []
        for b in pwrites:
            b.writers.append(o)
            if len(b.writers) > 48:
                b.writers = self._compact(b.writers)
        for b in reads:
            b.readers.append(o)
            if len(b.readers) > 48:
                b.readers = self._compact(b.readers)
        self.engs[eng].append(o)
        return o

    @staticmethod
    def _compact(ops):
        last = {}
        for r in ops:
            k = r.stream.name
            if k not in last or last[k].pos < r.pos:
                last[k] = r
        return list(last.values())

    def barrier(self):
        lasts = []
        for s in list(self.streams.values()) + list(self.dma_streams.values()):
            if s.ops:
                lasts.append(s.ops[-1])
        for e in self.engs:
            self.op(e, ("nop", {}), extra=lasts)

    def emit(self):
        nc = self.nc
        nsem = 0
        for s in list(self.streams.values()) + list(self.dma_streams.values()):
            if not any(o.signaled for o in s.ops):
                continue
            if s.is_dma:
                s.sems = [nc.alloc_semaphore("s_" + s.name)]
                nsem += 1
                cnt = 0
                for o in s.ops:
                    cnt += 16
                    o.sig = (s.sems[0], cnt)
            else:
                s.sems = [nc.alloc_semaphore(f"s_{s.name}{k}") for k in range(KROT)]
                nsem += KROT
                j = 0
                for o in s.ops:
                    if o.signaled:
                        o.sig = (s.sems[j % KROT], j // KROT + 1)
                        j += 1
        self.nsem = nsem
        engmap = {"pe": "tensor", "act": "scalar", "dve": "vector", "pool": "gpsimd", "sp": "sync"}
        with nc.Block() as block:
            for ename, ops in self.engs.items():
                if not ops:
                    continue

                def body(e, ops=ops):
                    for o in ops:
                        deps = o.deps
                        emb = None
                        if EMBED_WAIT and deps and not callable(o.fn):
                            emb = deps[-1]
                            deps = deps[:-1]
                        for p in deps:
                            e.wait_ge(p.sig[0], p.sig[1])
                        if callable(o.fn):
                            ins = o.fn(e)
                        else:
                            name, kw = o.fn
                            ins = getattr(e, name)(**kw)
                        if emb is not None:
                            ins._wait_ge(emb.sig[0], emb.sig[1])
                        if o.sig is not None:
                            if o.stream.is_dma:
                                ins.then_inc(o.sig[0], 16)
                            elif o.signaled:
                                ins.then_inc(o.sig[0], 1)

                getattr(block, engmap[ename])(body)


class Tl:
    __slots__ = ("t", "b")

    def __init__(self, t, b):
        self.t = t
        self.b = b

    def __getitem__(self, k):
        return self.t[k]


class Rot:
    def __init__(self, tiles):
        self.tiles = tiles
        self.i = 0

    def next(self):
        t = self.tiles[self.i % len(self.tiles)]
        self.i += 1
        return t


class Ctx:
    def __init__(self, nc, dbg=()):
        self.nc = nc
        self.S = Sched(nc)
        self.n = 0
        self.dbg = set(dbg)
        self.stack = None
        self.keymap = {}

    def sb(self, shape, dt, name=None):
        self.n += 1
        name = (name or "t") + f"_{self.n}"
        h = self.stack.enter_context(self.nc.sbuf_tensor(name, list(shape), dt))
        return Tl(h, Buf(name))

    def sbn(self, n, shape, dt, name=None):
        return Rot([self.sb(shape, dt, name) for _ in range(n)])

    def ps(self, shape, dt, name):
        return Tl(self.nc.alloc_psum_tensor(name, list(shape), dt), Buf(name))

    def dram(self, name, shape, dt):
        kind = "ExternalOutput" if name in self.dbg else "Internal"
        return Tl(self.nc.dram_tensor(name, list(shape), dt, kind=kind).ap(), Buf(name))

    def inp(self, name, shape, dt):
        return Tl(self.nc.dram_tensor(name, list(shape), dt, kind="ExternalInput").ap(), Buf(name))

    def I(self, eng, name, r=(), w=(), pw=(), key=None, **kw):
        if key is not None:
            key = self.keymap.setdefault(key, f"k{len(self.keymap)}")
        return self.S.op(eng, (name, kw), reads=[x.b for x in r], writes=[x.b for x in w],
                         pwrites=[x.b for x in pw], dma_key=key)

    def dma(self, eng, out, in_, key, r=(), w=(), pw=()):
        return self.I(eng, "dma_start", r=r, w=w, pw=pw, key=key, out=out, in_=in_)


def bc(ap, shape):
    return ap.broadcast_to(list(shape))


class Prog:
    def __init__(self, nc, dbg=(), phases=None):
        self.c = Ctx(nc, dbg)
        self.nc = nc
        self.phases = phases
        c = self.c
        self.wshapes = dict([
            ("ssm_w_in", [D, DINP]), ("conv_w", [128, 48, 4]), ("conv_b", [128, 48]),
            ("ssm_dt_bias", [NH]), ("ssm_a_log", [NH]), ("ssm_d", [NH]), ("ssm_norm_g", [DIN]),
            ("ssm_w_out", [DIN, D]),
            ("kv_w_down", [D, KVR]), ("kv_norm_g", [KVR]), ("kv_w_rope", [D, 64]),
            ("kv_w_uk", [KVR, 2048]), ("kv_w_uv", [KVR, 2048]),
            ("mla_w_dq", [D, QR]), ("mla_q_norm_g", [QR]), ("mla_w_uq", [QR, 3072]), ("mla_w_o", [2048, D]),
            ("ffn_w_gate", [DFF, D]), ("ffn_w_up", [DFF, D]), ("ffn_w_down", [DFF, D]),
            ("moe_w_router", [D, NE]), ("moe_b_router", [NE]),
            ("moe_w_gate", [NE * DFE, D]), ("moe_w_up", [NE * DFE, D]), ("moe_w_down", [NE * DFE, D]),
            ("ln1_g", [2, D]), ("ln1_b", [2, D]), ("ln2_g", [2, D]), ("ln2_b", [2, D]),
            ("ple_w_proj", [2, PLE, D]), ("ple_w_gate", [2, D, D]),
            ("x", [T, D]), ("p", [2, T, PLE]), ("invf", [32]),
        ])
        self._W = {}
        self.y = Tl(nc.dram_tensor("y", [T, D], F32, kind="ExternalOutput").ap(), Buf("y"))
        self.z_scr = c.dram("z_scr", [T, DIN], F32)
        self.dt_scr = c.dram("dt_scr", [T, NH], F32)
        self.xbcT = c.dram("xbcT", [48, 128, T], BF16)
        self.xbc_tok = c.dram("xbc_tok", [T, 5120], BF16)
        self.ynT = c.dram("ynT", [DIN, T], BF16)
        self.hA = c.dram("hA", [T, D], F32)
        self.hB = c.dram("hB", [T, D], F32)
        self.hAT = c.dram("hAT", [D, T], BF16)
        self.hBT = c.dram("hBT", [D, T], BF16)
        self.knT = c.dram("knT", [MH, 128, T], BF16)
        self.krT = c.dram("krT", [64, T], BF16)
        self.v_scr = c.dram("v_scr", [T, 2048], BF16)
        self.qnT = c.dram("qnT", [MH, 128, T], BF16)
        self.qrT = c.dram("qrT", [MH, 64, T], BF16)
        self.oT = c.dram("oT", [2048, T], BF16)
        self.psA_t = nc.alloc_psum_tensor("psA", [128, 2048], F32)
        self.psA = [Tl(self.psA_t[:, i * 512:(i + 1) * 512], Buf(f"psA{i}")) for i in range(4)]
        self.psArot = Rot(self.psA)
        self.ptr = Rot([c.ps([128, 1024], BF16, f"ptr{i}") for i in range(2)])
        self.pm = Rot([c.ps([128, 512], F32, f"pm{i}") for i in range(2)])
        self.final_ops = []

    def Win(self, name):
        if name not in self._W:
            if name == "pos":
                self._W[name] = self.c.inp("pos", [128, T // 128], I32)
            else:
                self._W[name] = self.c.inp(name, self.wshapes[name], F32)
        return self._W[name]

    def phase(self, name):
        return self.phases is None or name in self.phases

    def begin(self):
        self.c.stack = ExitStack()
        self.c.keymap = {}

    def end(self):
        self.c.S.barrier()
        self.c.stack.close()
        self.c.stack = None

    def consts(self):
        c = self.c
        nc = self.nc
        c.stack = ExitStack()
        self.gstack = c.stack
        one_f = c.sb([128, 128], F32, "onef")
        c.I("pool", "memset", w=[one_f], ap=one_f[:], constant=1.0)
        self.ones_f = one_f
        idf = c.sb([128, 128], F32, "idf")
        c.I("pool", "affine_select", r=[one_f], w=[idf], out=idf[:], in_=one_f[:], pattern=[[-1, 128]],
            compare_op=ALU.is_equal, fill=0.0, base=0, channel_multiplier=1)
        idb = c.sb([128, 128], BF16, "idb")
        c.I("dve", "tensor_copy", r=[idf], w=[idb], out=idb[:], in_=idf[:])
        self.idf, self.idb = idf, idb
        uf = c.sb([128, 128], F32, "uf")
        c.I("pool", "affine_select", r=[one_f], w=[uf], out=uf[:], in_=one_f[:], pattern=[[1, 128]],
            compare_op=ALU.is_ge, fill=0.0, base=0, channel_multiplier=-1)
        ub = c.sb([128, 128], BF16, "ub")
        c.I("dve", "tensor_copy", r=[uf], w=[ub], out=ub[:], in_=uf[:])
        self.uf, self.ub = uf, ub
        lsf = c.sb([128, 128], F32, "lsf")
        c.I("pool", "affine_select", r=[one_f], w=[lsf], out=lsf[:], in_=one_f[:], pattern=[[-1, 128]],
            compare_op=ALU.is_gt, fill=0.0, base=0, channel_multiplier=1)
        lsb = c.sb([128, 128], BF16, "lsb")
        c.I("dve", "tensor_copy", r=[lsf], w=[lsb], out=lsb[:], in_=lsf[:])
        self.lsb = lsb
        oneb = c.sb([128, 128], BF16, "oneb")
        c.I("dve", "tensor_copy", r=[one_f], w=[oneb], out=oneb[:], in_=one_f[:])
        self.ones_b = oneb
        zf = c.sb([128, 128], F32, "zf")
        c.I("pool", "memset", w=[zf], ap=zf[:], constant=0.0)
        mbf = c.sb([128, 128], F32, "mbf")
        c.I("pool", "affine_select", r=[zf], w=[mbf], out=mbf[:], in_=zf[:], pattern=[[-1, 128]],
            compare_op=ALU.is_ge, fill=-30000.0, base=0, channel_multiplier=1)
        mbb = c.sb([128, 128], BF16, "mbb")
        c.I("dve", "tensor_copy", r=[mbf], w=[mbb], out=mbb[:], in_=mbf[:])
        self.maskb = mbb

    def bcast_load(self, dram_ap_1d, n, name):
        c = self.c
        t = c.sb([128, n], F32, name)
        c.dma("sp", t[:], dram_ap_1d.partition_broadcast(128), key=name, w=[t])
        return t

    def wload(self, wt, wview, KC, ncols, key, kc_split=1):
        c = self.c
        step = KC // kc_split
        for i in range(kc_split):
            kw = dict(w=[wt]) if i == 0 else dict(pw=[wt])
            c.dma("pool", wt[:, i * step:(i + 1) * step, 0:ncols], wview[:, i * step:(i + 1) * step, :], key=key, **kw)

    def evac(self, k, out_ap, in_ap, r, w=(), pw=()):
        c = self.c
        if k % 2 == 0:
            return c.I("act", "activation", r=r, w=w, pw=pw, out=out_ap, in_=in_ap, func=AF.Copy)
        return c.I("dve", "tensor_copy", r=r, w=w, pw=pw, out=out_ap, in_=in_ap)

    def transpose_to(self, src, src_ap_fn, nblk, dst, dst_ap_fn, first_write=True, rows=128):
        c = self.c
        j = 0
        fw = first_write
        while j < nblk:
            n = min(8, nblk - j)
            pt = self.ptr.next()
            for i in range(n):
                kw = dict(w=[pt]) if i == 0 else dict(pw=[pt])
                c.I("pe", "transpose", r=[src, self.idb], out=pt[0:rows, i * 128:(i + 1) * 128],
                    in_=src_ap_fn(j + i), identity=self.idb[:], **kw)
            self._tk = getattr(self, "_tk", 0) + 1
            kw = dict(w=[dst]) if fw else dict(pw=[dst])
            fw = False
            self.evac(self._tk, dst_ap_fn(j, n), pt[0:rows, 0:n * 128].rearrange("p (a b) -> p a b", b=128),
                      r=[pt], **kw)
            j += n

    def ln_rows(self, r_t, r_ap, g_bc, b_bc, out_f32, out_f32_ap, out_bf, out_bf_ap):
        c = self.c
        st = self.ln_st.next()
        for i in range(4):
            kw = dict(w=[st]) if i == 0 else dict(pw=[st])
            c.I("dve", "bn_stats", r=[r_t], out=st[:, i * 6:(i + 1) * 6], in_=r_ap[:, i * 512:(i + 1) * 512], **kw)
        mv = self.ln_mv.next()
        c.I("dve", "bn_aggr", r=[st], w=[mv], out=mv[:, 0:2], in_=st[:, 0:24])
        c.I("act", "activation", r=[mv], pw=[mv], out=mv[:, 2:3], in_=mv[:, 1:2], func=AF.Sqrt, bias=self.eps_ln[:, 0:1], scale=1.0)
        c.I("dve", "reciprocal", r=[mv], pw=[mv], out=mv[:, 3:4], in_=mv[:, 2:3])
        c.I("dve", "tensor_scalar", r=[r_t, mv], w=[r_t], out=r_ap, in0=r_ap, scalar1=mv[:, 0:1], scalar2=mv[:, 3:4],
            op0=ALU.subtract, op1=ALU.mult)
        c.I("dve", "tensor_tensor", r=[r_t, g_bc], w=[r_t], out=r_ap, in0=r_ap, in1=g_bc[:], op=ALU.mult)
        c.I("dve", "tensor_tensor", r=[r_t, b_bc], w=[out_f32], out=out_f32_ap, in0=r_ap, in1=b_bc[:], op=ALU.add)
        c.I("act", "activation", r=[out_f32], w=[out_bf], out=out_bf_ap, in_=out_f32_ap, func=AF.Copy)

    def ln_setup(self):
        c = self.c
        self.ln_st = c.sbn(2, [128, 24], F32, "lnst")
        self.ln_mv = c.sbn(2, [128, 4], F32, "lnmv")
        self.eps_ln = c.sb([128, 1], F32, "epsln")
        c.I("pool", "memset", w=[self.eps_ln], ap=self.eps_ln[:], constant=LN_EPS)

    def p1(self):
        c = self.c
        W = self.Win
        self.begin()
        xt = c.sb([128, 16, SEQ], BF16, "xt")
        xin = c.sbn(2, [128, D], F32, "xin")
        xbf = c.sbn(2, [128, D], BF16, "xbf")
        wts = c.sbn(2, [128, 16, 512], BF16, "w")
        zst = c.sbn(3, [128, 512], F32, "zst")
        U = c.sbn(2, [128, 3 + SEQ], F32, "U")
        acc = c.sbn(1, [128, SEQ], F32, "cacc")
        vb = c.sbn(2, [128, SEQ], BF16, "vb")
        tokst = c.sbn(1, [128, 16, 512], BF16, "tokst")
        cw = c.sb([128, 48, 4], F32, "cw")
        cb = c.sb([128, 48], F32, "cb")
        c.dma("sp", cw[:], W("conv_w")[:], key="cw", w=[cw])
        c.dma("sp", cb[:], W("conv_b")[:], key="cb", w=[cb])
        dtb = self.bcast_load(W("ssm_dt_bias").t, NH, "dtb")
        dts = c.sbn(2, [128, 4, NH], F32, "dts")
        for u in U.tiles:
            c.I("pool", "memset", w=[u], ap=u[:, 0:3], constant=0.0)
        win = W("ssm_w_in").t.rearrange("(kc p) n -> p kc n", p=128)
        k = 0
        for s in range(NSEQ):
            t0 = s * SEQ
            for tt in range(16):
                xi = xin.next()
                xb = xbf.next()
                c.dma("sp", xi[:], W("x")[t0 + tt * 128:t0 + (tt + 1) * 128, :], key=xi.b.name, w=[xi])
                c.I("act", "activation", r=[xi], w=[xb], out=xb[:], in_=xi[:], func=AF.Copy)
                self.transpose_to(xb, lambda j, xb=xb: xb[:, j * 128:(j + 1) * 128], 16, xt,
                                  lambda j0, n, tt=tt: xt[:, j0:j0 + n, tt * 128:(tt + 1) * 128],
                                  first_write=(tt == 0))
            for cg in range(21):
                ncols = 512 if cg < 20 else 64
                wt = wts.next()
                self.wload(wt, win[:, :, cg * 512:cg * 512 + ncols], 16, ncols, key=wt.b.name, kc_split=2)
                if cg < 8 or cg == 20:
                    for tt in range(16):
                        ps = self.psArot.next()
                        for kc in range(16):
                            kw = dict(w=[ps]) if kc == 0 else dict(pw=[ps])
                            c.I("pe", "matmul", r=[xt, wt], out=ps[:, 0:ncols], lhsT=xt[:, kc, tt * 128:(tt + 1) * 128],
                                rhs=wt[:, kc, 0:ncols], start=(kc == 0), stop=(kc == 15), **kw)
                        if cg < 8:
                            zs = zst.next()
                            k += 1
                            self.evac(k, zs[:], ps[:], r=[ps], w=[zs])
                            c.dma("sp", self.z_scr[t0 + tt * 128:t0 + (tt + 1) * 128, cg * 512:(cg + 1) * 512], zs[:],
                                  key=zs.b.name, r=[zs], pw=[self.z_scr])
                        else:
                            if tt % 4 == 0:
                                ds = dts.next()
                            j = tt % 4
                            kw = dict(w=[ds]) if j == 0 else dict(pw=[ds])
                            a0 = ds[:, j, :]
                            x0 = zst.next()
                            c.I("dve", "tensor_tensor", r=[ps, dtb], w=[x0], out=x0[:, 0:64], in0=ps[:, 0:64], in1=dtb[:], op=ALU.add)
                            c.I("act", "activation", r=[x0], pw=[x0], out=x0[:, 64:128], in_=x0[:, 0:64], func=AF.Abs)
                            c.I("act", "activation", r=[x0], pw=[x0], out=x0[:, 128:192], in_=x0[:, 64:128], func=AF.Exp, scale=-1.0)
                            c.I("act", "activation", r=[x0], pw=[x0], out=x0[:, 192:256], in_=x0[:, 128:192], func=AF.Ln, bias=self.one_col[:, 0:1], scale=1.0)
                            c.I("act", "activation", r=[x0], pw=[x0], out=x0[:, 256:320], in_=x0[:, 0:64], func=AF.Relu)
                            c.I("dve", "tensor_tensor", r=[x0], out=a0, in0=x0[:, 256:320], in1=x0[:, 192:256], op=ALU.add, **kw)
                            if j == 3:
                                g4 = tt // 4
                                dv = self.dt_scr.t[t0 + g4 * 512:t0 + (g4 + 1) * 512, :].rearrange("(n p) h -> p n h", p=128)
                                c.dma("sp", dv, ds[:], key=ds.b.name, r=[ds], pw=[self.dt_scr])
                else:
                    ts_ = tokst.next() if cg < 18 else None
                    for j in range(4):
                        ch = (cg - 8) * 4 + j
                        u = U.next()
                        for sub in range(4):
                            ps = self.psArot.next()
                            for kc in range(16):
                                kw = dict(w=[ps]) if kc == 0 else dict(pw=[ps])
                                c.I("pe", "matmul", r=[xt, wt], out=ps[:], lhsT=wt[:, kc, j * 128:(j + 1) * 128],
                                    rhs=xt[:, kc, sub * 512:(sub + 1) * 512], start=(kc == 0), stop=(kc == 15), **kw)
                            k += 1
                            self.evac(k, u[:, 3 + sub * 512:3 + (sub + 1) * 512], ps[:], r=[ps], pw=[u])
                        a = acc.next()
                        c.I("dve", "tensor_scalar", r=[u, cw, cb], w=[a], out=a[:], in0=u[:, 0:SEQ], scalar1=cw[:, ch, 0:1],
                            scalar2=cb[:, ch:ch + 1], op0=ALU.mult, op1=ALU.add)
                        for kk in range(1, 4):
                            c.I("dve", "scalar_tensor_tensor", r=[u, cw, a], w=[a], out=a[:], in0=u[:, kk:kk + SEQ],
                                scalar=cw[:, ch, kk:kk + 1], in1=a[:], op0=ALU.mult, op1=ALU.add)
                        v = vb.next()
                        c.I("act", "activation", r=[a], w=[v], out=v[:], in_=a[:], func=AF.Silu)
                        c.dma("sp", self.xbcT[ch, :, t0:t0 + SEQ], v[:], key=v.b.name, r=[v], pw=[self.xbcT])
                        if cg < 18:
                            self.transpose_to(v, lambda jj, v=v: v[:, jj * 128:(jj + 1) * 128], 16, ts_,
                                              lambda j0, n, j=j: ts_[:, j0:j0 + n, j * 128:(j + 1) * 128],
                                              first_write=(j == 0))
                    if cg < 18:
                        col0 = (cg - 8) * 512
                        dv = self.xbc_tok.t[t0:t0 + SEQ, col0:col0 + 512].rearrange("(n p) f -> p n f", p=128)
                        c.dma("sp", dv, ts_[:], key=ts_.b.name, r=[ts_], pw=[self.xbc_tok])
        self.end()

    def p2(self):
        c = self.c
        W = self.Win
        self.begin()
        alog = self.bcast_load(W("ssm_a_log").t, NH, "alog")
        A_bc = c.sb([128, NH], F32, "Abc")
        c.I("act", "activation", r=[alog], w=[A_bc], out=A_bc[:], in_=alog[:], func=AF.Exp)
        c.I("dve", "tensor_scalar", r=[A_bc], w=[A_bc], out=A_bc[:], in0=A_bc[:], scalar1=-1.0, scalar2=None, op0=ALU.mult)
        D_bc = self.bcast_load(W("ssm_d").t, NH, "Dbc")
        gn_bc = self.bcast_load(W("ssm_norm_g").t, DIN, "gnbc")
        eps_r = c.sb([128, 1], F32, "epsr")
        c.I("pool", "memset", w=[eps_r], ap=eps_r[:], constant=RMS_EPS)
        state_f = c.sb([128, DIN], F32, "statef")
        state_b = c.sb([128, DIN], BF16, "stateb")
        xtoks = c.sbn(2, [128, 5120], BF16, "xtok")
        BT = c.sbn(2, [128, 8, 128], BF16, "BT")
        CT = c.sbn(2, [128, 8, 128], BF16, "CT")
        dtt = c.sbn(2, [128, NH], F32, "dtt")
        zts = c.sbn(1, [128, DIN], F32, "zt")
        small = c.sbn(2, [128, 8 * NH], F32, "small")
        arhs = c.sb([128, NH, 128], BF16, "arhs")
        Lm = c.sbn(2, [128, 4, 128], BF16, "Lm")
        MT = c.sbn(2, [128, 8, 128], BF16, "MT")
        CBm = c.sb([128, 8, 128], BF16, "CBm")
        xdt = c.sb([128, DIN], BF16, "xdt")
        xdte = c.sb([128, DIN], BF16, "xdte")
        yt = c.sb([128, DIN], F32, "yt")
        tmp = c.sbn(3, [128, 512], F32, "tmp")
        ynb = c.sb([128, DIN], BF16, "ynb")
        ynst = c.sb([128, 32, 512], BF16, "ynst")
        ss = c.sbn(2, [128, 16], F32, "ss")
        v3 = lambda ap, d: ap.rearrange("p (h d) -> p h d", d=d)
        for s in range(NSEQ):
            for ci in range(16):
                tc0 = s * SEQ + ci * 128
                xtok = xtoks.next()
                zt = zts.next()
                c.dma("sp", xtok[:], self.xbc_tok[tc0:tc0 + 128, :], key=xtok.b.name, w=[xtok], r=[self.xbc_tok])
                bt = BT.next()
                ct = CT.next()
                c.dma("sp", bt[:], self.xbcT[32:40, :, tc0:tc0 + 128].rearrange("g n t -> n g t"), key=bt.b.name, w=[bt], r=[self.xbcT])
                c.dma("sp", ct[:], self.xbcT[40:48, :, tc0:tc0 + 128].rearrange("g n t -> n g t"), key=ct.b.name, w=[ct], r=[self.xbcT])
                dt_ = dtt.next()
                c.dma("sp", dt_[:], self.dt_scr[tc0:tc0 + 128, :], key=dt_.b.name, w=[dt_], r=[self.dt_scr])
                c.dma("sp", zt[:], self.z_scr[tc0:tc0 + 128, :], key=zt.b.name, w=[zt], r=[self.z_scr])
                sm = small.next()
                a = sm[:, 0:64]; E = sm[:, 64:128]; acs = sm[:, 128:192]; dec = sm[:, 192:256]; toe = sm[:, 256:320]; tdf = sm[:, 320:384]
                c.I("dve", "tensor_tensor", r=[dt_, A_bc], w=[sm], out=a, in0=dt_[:], in1=A_bc[:], op=ALU.mult)
                pm = self.pm.next()
                c.I("pe", "matmul", r=[self.uf, sm], w=[pm], out=pm[:, 0:64], lhsT=self.uf[:], rhs=a, start=True, stop=True)
                c.I("pe", "matmul", r=[self.ones_f, sm], pw=[pm], out=pm[:, 64:128], lhsT=self.ones_f[:], rhs=a, start=True, stop=True)
                c.I("act", "activation", r=[pm], pw=[sm], out=E, in_=pm[:, 0:64], func=AF.Exp)
                c.I("act", "activation", r=[pm], pw=[sm], out=dec, in_=pm[:, 64:128], func=AF.Exp)
                c.I("act", "activation", r=[pm], pw=[sm], out=acs, in_=pm[:, 0:64], func=AF.Copy)
                c.I("dve", "tensor_tensor", r=[pm, sm], pw=[sm], out=tdf, in0=pm[:, 64:128], in1=acs, op=ALU.subtract)
                c.I("act", "activation", r=[sm], pw=[sm], out=toe, in_=tdf, func=AF.Exp)
                c.I("pool", "tensor_tensor", r=[sm, self.ub], w=[arhs], out=arhs[:], in0=bc(a.unsqueeze(2), [128, NH, 128]),
                    in1=bc(self.ub[:].unsqueeze(1), [128, NH, 128]), op=ALU.mult)
                c.I("pool", "tensor_tensor", r=[xtok, dt_], w=[xdt], out=v3(xdt[:], 64), in0=v3(xtok[:, 0:DIN], 64),
                    in1=bc(dt_[:].unsqueeze(2), [128, NH, 64]), op=ALU.mult)
                c.I("pool", "tensor_tensor", r=[xdt, sm], w=[xdte], out=v3(xdte[:], 64), in0=v3(xdt[:], 64),
                    in1=bc(toe.unsqueeze(2), [128, NH, 64]), op=ALU.mult)
                for g4 in range(2):
                    ps = self.psArot.next()
                    for gg in range(4):
                        g = g4 * 4 + gg
                        kw = dict(w=[ps]) if gg == 0 else dict(pw=[ps])
                        c.I("pe", "matmul", r=[bt, ct], out=ps[:, gg * 128:(gg + 1) * 128], lhsT=bt[:, g, :], rhs=ct[:, g, :],
                            start=True, stop=True, **kw)
                    kw = dict(w=[CBm]) if g4 == 0 else dict(pw=[CBm])
                    c.I("dve", "tensor_tensor", r=[ps, self.uf], out=CBm[:, g4 * 4:(g4 + 1) * 4, :], in0=v3(ps[:], 128),
                        in1=bc(self.uf[:].unsqueeze(1), [128, 4, 128]), op=ALU.mult, **kw)
                for hq in range(16):
                    g = hq // 2
                    ps = self.psArot.next()
                    c.I("pe", "matmul", r=[self.lsb, arhs], w=[ps], out=ps[:], lhsT=self.lsb[:],
                        rhs=arhs[:, hq * 4:(hq + 1) * 4, :].rearrange("p a b -> p (a b)"), start=True, stop=True)
                    lm = Lm.next()
                    c.I("act", "activation", r=[ps], w=[lm], out=lm[:], in_=v3(ps[:], 128), func=AF.Exp)
                    if hq % 2 == 0:
                        mt = MT.next()
                    kw = dict(w=[mt]) if hq % 2 == 0 else dict(pw=[mt])
                    c.I("dve", "tensor_tensor", r=[lm, CBm], out=mt[:, (hq % 2) * 4:(hq % 2) * 4 + 4, :], in0=lm[:],
                        in1=bc(CBm[:, g, :].unsqueeze(1), [128, 4, 128]), op=ALU.mult, **kw)
                    if hq % 2 == 1:
                        gs = slice(g * 512, (g + 1) * 512)
                        psy = self.psArot.next()
                        for hh in range(8):
                            h = g * 8 + hh
                            kw = dict(w=[psy]) if hh == 0 else dict(pw=[psy])
                            c.I("pe", "matmul", r=[mt, xdt], out=psy[:, hh * 64:(hh + 1) * 64], lhsT=mt[:, hh, :],
                                rhs=xdt[:, h * 64:(h + 1) * 64], start=True, stop=True, **kw)
                        ykw = dict(w=[yt]) if g == 0 else dict(pw=[yt])
                        if ci > 0:
                            psi = self.psArot.next()
                            c.I("pe", "matmul", r=[ct, state_b], w=[psi], out=psi[:], lhsT=ct[:, g, :], rhs=state_b[:, gs],
                                start=True, stop=True)
                            t1 = tmp.next()
                            c.I("dve", "tensor_tensor", r=[psi, sm], w=[t1], out=v3(t1[:], 64), in0=v3(psi[:], 64),
                                in1=bc(E[:, g * 8:(g + 1) * 8].unsqueeze(2), [128, 8, 64]), op=ALU.mult)
                            c.I("dve", "tensor_tensor", r=[psy, t1], out=yt[:, gs], in0=psy[:], in1=t1[:], op=ALU.add, **ykw)
                        else:
                            c.I("dve", "tensor_copy", r=[psy], out=yt[:, gs], in_=psy[:], **ykw)
                        t2 = tmp.next()
                        c.I("pool", "tensor_tensor", r=[xtok, D_bc], w=[t2], out=v3(t2[:], 64), in0=v3(xtok[:, gs], 64),
                            in1=bc(D_bc[:, g * 8:(g + 1) * 8].unsqueeze(2), [128, 8, 64]), op=ALU.mult)
                        c.I("dve", "tensor_tensor", r=[yt, t2], pw=[yt], out=yt[:, gs], in0=yt[:, gs], in1=t2[:], op=ALU.add)
                c.I("act", "activation", r=[zt], w=[zt], out=zt[:], in_=zt[:], func=AF.Silu)
                c.I("dve", "tensor_tensor", r=[yt, zt], w=[yt], out=yt[:], in0=yt[:], in1=zt[:], op=ALU.mult)
                s_ = ss.next()
                for g in range(8):
                    t1 = tmp.next()
                    kw = dict(w=[s_, t1]) if g == 0 else dict(w=[t1], pw=[s_])
                    c.I("act", "activation", r=[yt], out=t1[:], in_=yt[:, g * 512:(g + 1) * 512], func=AF.Square,
                        accum_out=s_[:, g:g + 1], **kw)
                c.I("act", "activation", r=[s_, eps_r], pw=[s_], out=s_[:, 8:16], in_=s_[:, 0:8], func=AF.Sqrt,
                    bias=eps_r[:, 0:1], scale=1.0 / 512)
                c.I("dve", "reciprocal", r=[s_], w=[s_], out=s_[:, 0:8], in_=s_[:, 8:16])
                c.I("dve", "tensor_tensor", r=[yt, s_], w=[yt], out=v3(yt[:], 512), in0=v3(yt[:], 512),
                    in1=bc(s_[:, 0:8].unsqueeze(2), [128, 8, 512]), op=ALU.mult)
                c.I("dve", "tensor_tensor", r=[yt, gn_bc], w=[ynb], out=ynb[:], in0=yt[:], in1=gn_bc[:], op=ALU.mult)
                q = ci % 4
                self.transpose_to(ynb, lambda j: ynb[:, j * 128:(j + 1) * 128], 32, ynst,
                                  lambda j0, n, q=q: ynst[:, j0:j0 + n, q * 128:(q + 1) * 128], first_write=(q == 0))
                if q == 3:
                    tb0 = s * SEQ + (ci // 4) * 512
                    c.dma("sp", self.ynT[:, tb0:tb0 + 512].rearrange("(c p) t -> p c t", p=128), ynst[:], key="st_ynst",
                          r=[ynst], pw=[self.ynT])
                if ci < 15:
                    for g in range(8):
                        gs = slice(g * 512, (g + 1) * 512)
                        psn = self.psArot.next()
                        c.I("pe", "matmul", r=[xtok, xdte], w=[psn], out=psn[:], lhsT=xtok[:, DIN + g * 128:DIN + (g + 1) * 128],
                            rhs=xdte[:, gs], start=True, stop=True)
                        skw = dict(w=[state_f]) if g == 0 else dict(pw=[state_f])
                        if ci == 0:
                            c.I("dve", "tensor_copy", r=[psn], out=state_f[:, gs], in_=psn[:], **skw)
                        else:
                            t3 = tmp.next()
                            c.I("dve", "tensor_tensor", r=[state_f, sm], w=[t3], out=v3(t3[:], 64), in0=v3(state_f[:, gs], 64),
                                in1=bc(dec[:, g * 8:(g + 1) * 8].unsqueeze(2), [128, 8, 64]), op=ALU.mult)
                            c.I("dve", "tensor_tensor", r=[t3, psn], out=state_f[:, gs], in0=t3[:], in1=psn[:], op=ALU.add, **skw)
                        bkw = dict(w=[state_b]) if g == 0 else dict(pw=[state_b])
                        c.I("act", "activation", r=[state_f], out=state_b[:, gs], in_=state_f[:, gs], func=AF.Copy, **bkw)
        self.end()

    def linear_ln(self, xT, KC, wview, resid, g_ap, b_ap, out_tok, out_T, TB=512, NCOL=256):
        c = self.c
        self.begin()
        g_bc = self.bcast_load(g_ap, D, "lng")
        b_bc = self.bcast_load(b_ap, D, "lnb")
        xt = c.sb([128, KC, TB], BF16, "xt")
        wts = c.sbn(2, [128, KC, NCOL], BF16, "w")
        r = c.sb([128, TB // 128, D], F32, "r")
        res = c.sbn(2, [128, D], F32, "res")
        hf = c.sbn(2, [128, D], F32, "hf")
        hb = c.sbn(2, [128, D], BF16, "hb")
        stT = c.sb([128, 16, TB], BF16, "stT")
        wv = wview.rearrange("(kc p) n -> p kc n", p=128)
        k = 0
        nt = TB // 128
        for blk in range(T // TB):
            tb0 = blk * TB
            c.dma("sp", xt[:], xT[:, tb0:tb0 + TB].rearrange("(kc p) t -> p kc t", p=128), key="xt", w=[xt], r=[xT])
            for cg in range(D // NCOL):
                wt = wts.next()
                self.wload(wt, wv[:, :, cg * NCOL:(cg + 1) * NCOL], KC, NCOL, key=wt.b.name, kc_split=2)
                for tt in range(nt):
                    ps = self.psArot.next()
                    for kc in range(KC):
                        kw = dict(w=[ps]) if kc == 0 else dict(pw=[ps])
                        c.I("pe", "matmul", r=[xt, wt], out=ps[:, 0:NCOL], lhsT=xt[:, kc, tt * 128:(tt + 1) * 128],
                            rhs=wt[:, kc, :], start=(kc == 0), stop=(kc == KC - 1), **kw)
                    k += 1
                    kw = dict(w=[r]) if (cg == 0 and tt == 0) else dict(pw=[r])
                    self.evac(k, r[:, tt, cg * NCOL:(cg + 1) * NCOL], ps[:, 0:NCOL], r=[ps], **kw)
            for tt in range(nt):
                t0 = tb0 + tt * 128
                rs = res.next()
                c.dma("sp", rs[:], resid[t0:t0 + 128, :], key=rs.b.name, w=[rs], r=[resid])
                c.I("dve", "scalar_tensor_tensor", r=[rs, r], pw=[r], out=r[:, tt, :], in0=rs[:], scalar=ALPHA, in1=r[:, tt, :],
                    op0=ALU.mult, op1=ALU.add)
                f = hf.next()
                b = hb.next()
                self.ln_rows(r, r[:, tt, :], g_bc, b_bc, f, f[:], b, b[:])
                c.dma("sp", out_tok[t0:t0 + 128, :], f[:], key="st_" + f.b.name, r=[f], pw=[out_tok])
                self.transpose_to(b, lambda j, b=b: b[:, j * 128:(j + 1) * 128], 16, stT,
                                  lambda j0, n, tt=tt: stT[:, j0:j0 + n, tt * 128:(tt + 1) * 128], first_write=(tt == 0))
            c.dma("sp", out_T[:, tb0:tb0 + TB].rearrange("(c p) t -> p c t", p=128), stT[:], key="st_stT", r=[stT], pw=[out_T])
        self.end()

    def p3(self):
        W = self.Win
        self.linear_ln(self.ynT, 32, W("ssm_w_out").t, W("x"), W("ln1_g").t[0], W("ln1_b").t[0], self.hA, self.hAT)

    def ffn(self, xT, resid, units, g_ap, b_ap, out_tok, out_T, moe=False):
        c = self.c
        W = self.Win
        self.begin()
        TB = 512
        nt = TB // 128
        g_bc = self.bcast_load(g_ap, D, "lng")
        b_bc = self.bcast_load(b_ap, D, "lnb")
        xt = c.sb([128, 16, TB], BF16, "xt")
        wgs = c.sbn(2, [128, 16, 128], BF16, "wg")
        wus = c.sbn(2, [128, 16, 128], BF16, "wu")
        wds = c.sbn(2, [128, 11, 512], BF16, "wd")
        actT = c.sb([128, 11, TB], BF16, "actT")
        acc = c.sb([128, nt, D], F32, "acc")
        sg = c.sbn(2, [128, 512], F32, "sg")
        res = c.sbn(2, [128, D], F32, "res")
        hb = c.sbn(1, [128, D], BF16, "hb")
        stT = c.sb([128, 16, TB], BF16, "stT")
        if moe:
            wr = c.sb([128, 16, NE], F32, "wr")
            c.dma("sp", wr[:], W("moe_w_router").t.rearrange("(kc p) e -> p kc e", p=128), key="wr", w=[wr])
            br = self.bcast_load(W("moe_b_router").t, NE, "br")
            hTf = c.sb([128, 16, 128], F32, "hTf")
            comb = c.sb([128, nt, NE], F32, "comb")
            rt = c.sbn(2, [128, 64], F32, "rt")
        k = 0
        for blk in range(T // TB):
            tb0 = blk * TB
            c.dma("sp", xt[:], xT[:, tb0:tb0 + TB].rearrange("(kc p) t -> p kc t", p=128), key="xt", w=[xt], r=[xT])
            if moe:
                for tt in range(nt):
                    t0 = tb0 + tt * 128
                    rs = res.next()
                    c.dma("sp", rs[:], resid[t0:t0 + 128, :], key=rs.b.name, w=[rs], r=[resid])
                    for q in range(4):
                        pm = self.pm.next()
                        for i in range(4):
                            kc = q * 4 + i
                            kw = dict(w=[pm]) if i == 0 else dict(pw=[pm])
                            c.I("pe", "transpose", r=[rs, self.idf], out=pm[:, i * 128:(i + 1) * 128],
                                in_=rs[:, kc * 128:(kc + 1) * 128], identity=self.idf[:], **kw)
                        k += 1
                        kw = dict(w=[hTf]) if q == 0 else dict(pw=[hTf])
                        self.evac(k, hTf[:, q * 4:(q + 1) * 4, :], pm[:].rearrange("p (a b) -> p a b", b=128), r=[pm], **kw)
                    pm = self.pm.next()
                    for kc in range(16):
                        kw = dict(w=[pm]) if kc == 0 else dict(pw=[pm])
                        c.I("pe", "matmul", r=[hTf, wr], out=pm[:, 0:NE], lhsT=hTf[:, kc, :], rhs=wr[:, kc, :],
                            start=(kc == 0), stop=(kc == 15), **kw)
                    r_ = rt.next()
                    lg = r_[:, 0:8]; oh1 = r_[:, 8:16]; l2 = r_[:, 16:24]; oh2 = r_[:, 24:32]
                    m1 = r_[:, 32:33]; m2 = r_[:, 33:34]; dd = r_[:, 34:35]; ee = r_[:, 35:36]; e1 = r_[:, 36:37]
                    w1 = r_[:, 37:38]; w2 = r_[:, 38:39]
                    c.I("dve", "tensor_tensor", r=[pm, br], w=[r_], out=lg, in0=pm[:, 0:NE], in1=br[:], op=ALU.add)
                    c.I("dve", "reduce_max", r=[r_], pw=[r_], out=m1, in_=lg, axis=AX.X)
                    c.I("dve", "tensor_scalar", r=[r_], pw=[r_], out=oh1, in0=lg, scalar1=m1, scalar2=None, op0=ALU.is_equal)
                    c.I("dve", "scalar_tensor_tensor", r=[r_], pw=[r_], out=l2, in0=oh1, scalar=-1.0e30, in1=lg, op0=ALU.mult, op1=ALU.add)
                    c.I("dve", "reduce_max", r=[r_], pw=[r_], out=m2, in_=l2, axis=AX.X)
                    c.I("dve", "tensor_scalar", r=[r_], pw=[r_], out=oh2, in0=l2, scalar1=m2, scalar2=None, op0=ALU.is_equal)
                    c.I("dve", "tensor_tensor", r=[r_], pw=[r_], out=dd, in0=m2, in1=m1, op=ALU.subtract)
                    c.I("act", "activation", r=[r_], pw=[r_], out=ee, in_=dd, func=AF.Exp)
                    c.I("dve", "tensor_scalar", r=[r_], pw=[r_], out=e1, in0=ee, scalar1=1.0, scalar2=None, op0=ALU.add)
                    c.I("dve", "reciprocal", r=[r_], pw=[r_], out=w1, in_=e1)
                    c.I("dve", "tensor_tensor", r=[r_], pw=[r_], out=w2, in0=ee, in1=w1, op=ALU.mult)
                    kw = dict(w=[comb]) if tt == 0 else dict(pw=[comb])
                    c.I("dve", "tensor_scalar", r=[r_], out=comb[:, tt, :], in0=oh1, scalar1=w1, scalar2=None, op0=ALU.mult, **kw)
                    c.I("dve", "scalar_tensor_tensor", r=[r_, comb], pw=[comb], out=comb[:, tt, :], in0=oh2, scalar=w2,
                        in1=comb[:, tt, :], op0=ALU.mult, op1=ALU.add)
            for u, (wgv, wuv, wdv, e) in enumerate(units):
                wd3 = wdv.rearrange("(fc p) d -> p fc d", p=128)
                for fc in range(11):
                    wg = wgs.next()
                    wu = wus.next()
                    c.dma("pool", wg[:].rearrange("p a b -> p (a b)"), wgv[fc * 128:(fc + 1) * 128, :], key=wg.b.name, w=[wg])
                    c.dma("pool", wu[:].rearrange("p a b -> p (a b)"), wuv[fc * 128:(fc + 1) * 128, :], key=wu.b.name, w=[wu])
                    for half in range(TB // 512):
                        hs = slice(half * 512, (half + 1) * 512)
                        psg = self.psArot.next()
                        for kc in range(16):
                            kw = dict(w=[psg]) if kc == 0 else dict(pw=[psg])
                            c.I("pe", "matmul", r=[xt, wg], out=psg[:], lhsT=wg[:, kc, :], rhs=xt[:, kc, hs],
                                start=(kc == 0), stop=(kc == 15), **kw)
                        psu = self.psArot.next()
                        for kc in range(16):
                            kw = dict(w=[psu]) if kc == 0 else dict(pw=[psu])
                            c.I("pe", "matmul", r=[xt, wu], out=psu[:], lhsT=wu[:, kc, :], rhs=xt[:, kc, hs],
                                start=(kc == 0), stop=(kc == 15), **kw)
                        s1 = sg.next()
                        c.I("act", "activation", r=[psg], w=[s1], out=s1[:], in_=psg[:], func=AF.Silu)
                        kw = dict(w=[actT]) if (fc == 0 and half == 0) else dict(pw=[actT])
                        c.I("dve", "tensor_tensor", r=[s1, psu], out=actT[:, fc, hs], in0=s1[:], in1=psu[:], op=ALU.mult, **kw)
                for dblk in range(4):
                    ds_ = slice(dblk * 512, (dblk + 1) * 512)
                    wd = wds.next()
                    self.wload(wd, wd3[:, :, ds_], 11, 512, key=wd.b.name)
                    for tt in range(nt):
                        ps = self.psArot.next()
                        for fc in range(11):
                            kw = dict(w=[ps]) if fc == 0 else dict(pw=[ps])
                            c.I("pe", "matmul", r=[actT, wd], out=ps[:], lhsT=actT[:, fc, tt * 128:(tt + 1) * 128],
                                rhs=wd[:, fc, :], start=(fc == 0), stop=(fc == 10), **kw)
                        first = (u == 0)
                        akw = dict(w=[acc]) if (first and dblk == 0 and tt == 0) else dict(pw=[acc])
                        if moe:
                            sc = comb[:, tt, e:e + 1]
                            rr = [ps, comb]
                        else:
                            sc = 1.0
                            rr = [ps]
                        if first:
                            c.I("dve", "tensor_scalar", r=rr, out=acc[:, tt, ds_], in0=ps[:], scalar1=sc, scalar2=None,
                                op0=ALU.mult, **akw)
                        else:
                            c.I("dve", "scalar_tensor_tensor", r=rr + [acc], out=acc[:, tt, ds_], in0=ps[:], scalar=sc,
                                in1=acc[:, tt, ds_], op0=ALU.mult, op1=ALU.add, **akw)
            for tt in range(nt):
                t0 = tb0 + tt * 128
                rs = res.next()
                c.dma("sp", rs[:], resid[t0:t0 + 128, :], key=rs.b.name, w=[rs], r=[resid])
                c.I("dve", "scalar_tensor_tensor", r=[rs, acc], pw=[acc], out=acc[:, tt, :], in0=rs[:], scalar=ALPHA,
                    in1=acc[:, tt, :], op0=ALU.mult, op1=ALU.add)
                b = hb.next()
                self.ln_rows(acc, acc[:, tt, :], g_bc, b_bc, acc, acc[:, tt, :], b, b[:])
                c.dma("sp", out_tok[t0:t0 + 128, :], acc[:, tt, :], key="st_acc", r=[acc], pw=[out_tok])
                self.transpose_to(b, lambda j, b=b: b[:, j * 128:(j + 1) * 128], 16, stT,
                                  lambda j0, n, tt=tt: stT[:, j0:j0 + n, tt * 128:(tt + 1) * 128], first_write=(tt == 0))
            c.dma("sp", out_T[:, tb0:tb0 + TB].rearrange("(c p) t -> p c t", p=128), stT[:], key="st_stT", r=[stT], pw=[out_T])
        self.end()

    def p4(self):
        W = self.Win
        wg, wu, wd = W("ffn_w_gate").t, W("ffn_w_up").t, W("ffn_w_down").t
        units = []
        for u in range(4):
            fs = slice(u * 1408, (u + 1) * 1408)
            units.append((wg[fs, :], wu[fs, :], wd[fs, :], None))
        self.ffn(self.hAT, self.hA, units, W("ln2_g").t[0], W("ln2_b").t[0], self.hB, self.hBT)

    def p9_dense_unused(self):
        W = self.Win
        wg, wu, wd = W("moe_w_gate").t, W("moe_w_up").t, W("moe_w_down").t
        units = []
        for e in range(NE):
            for hf in range(2):
                fs = slice(hf * 1408, (hf + 1) * 1408)
                units.append((wg[e][:, fs], wu[e][:, fs], wd[e][fs, :], e))
        self.ffn(self.hBT, self.hB, units, W("ln2_g").t[1], W("ln2_b").t[1], self.hA, self.hAT, moe=True)

    def ple(self, layer, h_tok, hT, out_tok, out_T):
        c = self.c
        W = self.Win
        self.begin()
        TB = 1024
        nt = TB // 128
        xt = c.sb([128, 16, TB], BF16, "xt")
        pT = c.sb([128, 2, TB], BF16, "pT")
        pin = c.sbn(2, [128, PLE], F32, "pin")
        pbf = c.sbn(2, [128, PLE], BF16, "pbf")
        wgs = c.sbn(2, [128, 16, 512], BF16, "wg")
        wps = c.sbn(2, [128, 2, 512], BF16, "wp")
        hres = c.sb([128, nt, D], F32, "hres")
        sg = c.sbn(2, [128, 512], F32, "sg")
        hb = c.sbn(1, [128, D], BF16, "hb")
        stT = c.sb([128, 16, 512], BF16, "stT")
        wgv = W("ple_w_gate").t[layer].rearrange("(kc p) n -> p kc n", p=128)
        wpv = W("ple_w_proj").t[layer].rearrange("(kc p) n -> p kc n", p=128)
        pv = W("p").t[layer]
        for blk in range(T // TB):
            tb0 = blk * TB
            c.dma("sp", xt[:], hT[:, tb0:tb0 + TB].rearrange("(kc p) t -> p kc t", p=128), key="xt", w=[xt], r=[hT])
            c.dma("sp", hres[:], h_tok[tb0:tb0 + TB, :].rearrange("(n p) d -> p n d", p=128), key="hres", w=[hres], r=[h_tok])
            for tt in range(nt):
                pi = pin.next()
                pb = pbf.next()
                c.dma("sp", pi[:], pv[tb0 + tt * 128:tb0 + (tt + 1) * 128, :], key=pi.b.name, w=[pi])
                c.I("act", "activation", r=[pi], w=[pb], out=pb[:], in_=pi[:], func=AF.Copy)
                self.transpose_to(pb, lambda j, pb=pb: pb[:, j * 128:(j + 1) * 128], 2, pT,
                                  lambda j0, n, tt=tt: pT[:, j0:j0 + n, tt * 128:(tt + 1) * 128], first_write=(tt == 0))
            for cg in range(4):
                cs = slice(cg * 512, (cg + 1) * 512)
                wg = wgs.next()
                wp = wps.next()
                self.wload(wg, wgv[:, :, cs], 16, 512, key=wg.b.name, kc_split=2)
                self.wload(wp, wpv[:, :, cs], 2, 512, key=wp.b.name)
                for tt in range(nt):
                    ts_ = slice(tt * 128, (tt + 1) * 128)
                    ps1 = self.psArot.next()
                    for kc in range(16):
                        kw = dict(w=[ps1]) if kc == 0 else dict(pw=[ps1])
                        c.I("pe", "matmul", r=[xt, wg], out=ps1[:], lhsT=xt[:, kc, ts_], rhs=wg[:, kc, :],
                            start=(kc == 0), stop=(kc == 15), **kw)
                    ps2 = self.psArot.next()
                    for kc in range(2):
                        kw = dict(w=[ps2]) if kc == 0 else dict(pw=[ps2])
                        c.I("pe", "matmul", r=[pT, wp], out=ps2[:], lhsT=pT[:, kc, ts_], rhs=wp[:, kc, :],
                            start=(kc == 0), stop=(kc == 1), **kw)
                    s1 = sg.next()
                    c.I("act", "activation", r=[ps1], w=[s1], out=s1[:], in_=ps1[:], func=AF.Sigmoid)
                    c.I("dve", "tensor_tensor", r=[s1, ps2], w=[s1], out=s1[:], in0=s1[:], in1=ps2[:], op=ALU.mult)
                    c.I("dve", "tensor_tensor", r=[s1, hres], pw=[hres], out=hres[:, tt, cs], in0=hres[:, tt, cs], in1=s1[:], op=ALU.add)
            for tt in range(nt):
                t0 = tb0 + tt * 128
                st = c.dma("sp", out_tok[t0:t0 + 128, :], hres[:, tt, :], key="st_hres", r=[hres], pw=[out_tok])
                if out_T is not None:
                    b = hb.next()
                    q = tt % 4
                    c.I("act", "activation", r=[hres], w=[b], out=b[:], in_=hres[:, tt, :], func=AF.Copy)
                    self.transpose_to(b, lambda j, b=b: b[:, j * 128:(j + 1) * 128], 16, stT,
                                      lambda j0, n, q=q: stT[:, j0:j0 + n, q * 128:(q + 1) * 128], first_write=(q == 0))
                    if q == 3:
                        c0 = tb0 + (tt // 4) * 512
                        c.dma("sp", out_T[:, c0:c0 + 512].rearrange("(c p) t -> p c t", p=128), stT[:], key="st_stT", r=[stT], pw=[out_T])
        self.end()

    def p5(self):
        self.ple(0, self.hB, self.hBT, self.hA, self.hAT)

    def p10(self):
        self.ple(1, self.hA, self.hAT, self.y, None)

    def sin_of(self, ang, out, wk, ki):
        c = self.c
        C1 = 6.28125
        C2 = TWO_PI - C1
        c.I("dve", "tensor_scalar", r=[ang], w=[wk], out=wk[:], in0=ang[:], scalar1=1.0 / TWO_PI, scalar2=0.5, op0=ALU.mult, op1=ALU.add)
        c.I("dve", "tensor_copy", r=[wk], w=[ki], out=ki[:], in_=wk[:])
        c.I("dve", "tensor_copy", r=[ki], w=[wk], out=wk[:], in_=ki[:])
        c.I("dve", "scalar_tensor_tensor", r=[wk, ang], w=[out], out=out[:], in0=wk[:], scalar=-C1, in1=ang[:], op0=ALU.mult, op1=ALU.add)
        c.I("dve", "scalar_tensor_tensor", r=[wk, out], w=[out], out=out[:], in0=wk[:], scalar=-C2, in1=out[:], op0=ALU.mult, op1=ALU.add)
        c.I("dve", "tensor_scalar", r=[out], w=[wk], out=wk[:], in0=out[:], scalar1=-math.pi, scalar2=None, op0=ALU.is_lt)
        c.I("dve", "scalar_tensor_tensor", r=[wk, out], w=[out], out=out[:], in0=wk[:], scalar=TWO_PI, in1=out[:], op0=ALU.mult, op1=ALU.add)
        c.I("dve", "tensor_scalar", r=[out], w=[wk], out=wk[:], in0=out[:], scalar1=math.pi, scalar2=None, op0=ALU.is_gt)
        c.I("dve", "scalar_tensor_tensor", r=[wk, out], w=[out], out=out[:], in0=wk[:], scalar=-TWO_PI, in1=out[:], op0=ALU.mult, op1=ALU.add)
        c.I("dve", "tensor_scalar", r=[out], w=[out], out=out[:], in0=out[:], scalar1=-3.14159, scalar2=3.14159, op0=ALU.max, op1=ALU.min)
        c.I("act", "activation", r=[out], w=[out], out=out[:], in_=out[:], func=AF.Sin)

    def rope(self, src, src_ap, nh, cos_ap, sin_ap, dst, dst_ap, tw, first=True):
        c = self.c
        n = nh * 32
        x1 = src_ap[:, :, 0:32]
        x2 = src_ap[:, :, 32:64]
        cb = bc(cos_ap.unsqueeze(1), [128, nh, 32])
        sb_ = bc(sin_ap.unsqueeze(1), [128, nh, 32])
        t = [tw[:, i * n:(i + 1) * n].rearrange("p (h d) -> p h d", d=32) for i in range(4)]
        c.I("dve", "tensor_tensor", r=[src, self.cosT], w=[tw], out=t[0], in0=x1, in1=cb, op=ALU.mult)
        c.I("dve", "tensor_tensor", r=[src, self.sinT], pw=[tw], out=t[1], in0=x2, in1=sb_, op=ALU.mult)
        c.I("dve", "tensor_tensor", r=[src, self.cosT], pw=[tw], out=t[2], in0=x2, in1=cb, op=ALU.mult)
        c.I("dve", "tensor_tensor", r=[src, self.sinT], pw=[tw], out=t[3], in0=x1, in1=sb_, op=ALU.mult)
        kw = dict(w=[dst]) if first else dict(pw=[dst])
        c.I("dve", "tensor_tensor", r=[tw], out=dst_ap[:, :, 0:32], in0=t[0], in1=t[1], op=ALU.subtract, **kw)
        c.I("dve", "tensor_tensor", r=[tw], pw=[dst], out=dst_ap[:, :, 32:64], in0=t[2], in1=t[3], op=ALU.add)

    def p6(self):
        c = self.c
        W = self.Win
        self.begin()
        TB = 512
        posi = c.sb([128, 32], I32, "posi")
        c.dma("sp", posi[:], W("pos")[:], key="posi", w=[posi])
        posf = c.sb([128, 32], F32, "posf")
        c.I("dve", "tensor_copy", r=[posi], w=[posf], out=posf[:], in_=posi[:])
        invf = self.bcast_load(W("invf").t, 32, "invf")
        ang = c.sb([128, 1024], F32, "ang")
        ang2 = c.sb([128, 1024], F32, "ang2")
        wk = c.sb([128, 1024], F32, "wk")
        ki = c.sb([128, 1024], I32, "ki")
        self.cosT = c.sb([128, 1024], F32, "cosT")
        self.sinT = c.sb([128, 1024], F32, "sinT")
        a3 = ang[:].rearrange("p (n j) -> p n j", j=32)
        c.I("dve", "tensor_tensor", r=[posf, invf], w=[ang], out=a3, in0=bc(posf[:].unsqueeze(2), [128, 32, 32]),
            in1=bc(invf[:].unsqueeze(1), [128, 32, 32]), op=ALU.mult)
        c.I("dve", "tensor_scalar", r=[ang], w=[ang2], out=ang2[:], in0=ang[:], scalar1=math.pi / 2, scalar2=None, op0=ALU.add)
        self.sin_of(ang, self.sinT, wk, ki)
        self.sin_of(ang2, self.cosT, wk, ki)
        cos3 = self.cosT[:].rearrange("p (n j) -> p n j", j=32)
        sin3 = self.sinT[:].rearrange("p (n j) -> p n j", j=32)
        gkv = self.bcast_load(W("kv_norm_g").t, KVR, "gkv")
        gq = self.bcast_load(W("mla_q_norm_g").t, QR, "gq")
        eps_r = c.sb([128, 1], F32, "epsr")
        c.I("pool", "memset", w=[eps_r], ap=eps_r[:], constant=RMS_EPS)
        xt = c.sb([128, 16, TB], BF16, "xt")
        wbig = c.sbn(2, [128, 16, 512], BF16, "wbig")
        wsm = c.sbn(2, [128, 4, 1024], BF16, "wsm")
        ckvT = c.sb([128, 4, TB], BF16, "ckvT")
        cqT = c.sb([128, 4, TB], BF16, "cqT")
        cn = c.sbn(2, [128, 512], F32, "cn")
        cnb = c.sbn(2, [128, 512], BF16, "cnb")
        ssq = c.sbn(2, [128, 4], F32, "ssq")
        rp = c.sbn(2, [128, 1024], F32, "rp")
        tw = c.sbn(2, [128, 2048], F32, "tw")
        krb = c.sbn(2, [128, 64], BF16, "krb")
        krst = c.sb([64, TB], BF16, "krst")
        qrb = c.sbn(2, [128, 1024], BF16, "qrb")
        qrst = c.sb([64, MH, TB], BF16, "qrst")
        fst = c.sbn(3, [128, 512], BF16, "fst")
        wdown = W("kv_w_down").t.rearrange("(kc p) n -> p kc n", p=128)
        wdq = W("mla_w_dq").t.rearrange("(kc p) n -> p kc n", p=128)
        wrope = W("kv_w_rope").t.rearrange("(kc p) n -> p kc n", p=128)
        wuk = W("kv_w_uk").t.rearrange("(kc p) n -> p kc n", p=128)
        wuv = W("kv_w_uv").t.rearrange("(kc p) n -> p kc n", p=128)
        wuq = W("mla_w_uq").t.rearrange("(kc p) (h d) -> p kc h d", p=128, d=192)
        k = 0
        for blk in range(T // TB):
            tb0 = blk * TB
            c.dma("sp", xt[:], self.hAT[:, tb0:tb0 + TB].rearrange("(kc p) t -> p kc t", p=128), key="xt", w=[xt], r=[self.hAT])
            for (wv_, gbc, dstT) in ((wdown, gkv, ckvT), (wdq, gq, cqT)):
                wt = wbig.next()
                self.wload(wt, wv_, 16, 512, key=wt.b.name, kc_split=2)
                for tt in range(4):
                    ts_ = slice(tt * 128, (tt + 1) * 128)
                    ps = self.psArot.next()
                    for kc in range(16):
                        kw = dict(w=[ps]) if kc == 0 else dict(pw=[ps])
                        c.I("pe", "matmul", r=[xt, wt], out=ps[:], lhsT=xt[:, kc, ts_], rhs=wt[:, kc, :], start=(kc == 0), stop=(kc == 15), **kw)
                    x_ = cn.next()
                    q_ = ssq.next()
                    xb = cnb.next()
                    c.I("act", "activation", r=[ps], w=[x_, q_], out=x_[:], in_=ps[:], func=AF.Square, accum_out=q_[:, 0:1])
                    c.I("act", "activation", r=[q_, eps_r], pw=[q_], out=q_[:, 1:2], in_=q_[:, 0:1], func=AF.Sqrt, bias=eps_r[:, 0:1], scale=1.0 / 512)
                    c.I("dve", "reciprocal", r=[q_], pw=[q_], out=q_[:, 2:3], in_=q_[:, 1:2])
                    c.I("dve", "tensor_scalar", r=[ps, q_], w=[x_], out=x_[:], in0=ps[:], scalar1=q_[:, 2:3], scalar2=None, op0=ALU.mult)
                    c.I("dve", "tensor_tensor", r=[x_, gbc], w=[xb], out=xb[:], in0=x_[:], in1=gbc[:], op=ALU.mult)
                    self.transpose_to(xb, lambda j, xb=xb: xb[:, j * 128:(j + 1) * 128], 4, dstT,
                                      lambda j0, n, tt=tt, dstT=dstT: dstT[:, j0:j0 + n, tt * 128:(tt + 1) * 128], first_write=(tt == 0))
            wt = wbig.next()
            self.wload(wt, wrope, 16, 64, key=wt.b.name)
            for tt in range(4):
                ts_ = slice(tt * 128, (tt + 1) * 128)
                gt = (tb0 // 128) + tt
                ps = self.psArot.next()
                for kc in range(16):
                    kw = dict(w=[ps]) if kc == 0 else dict(pw=[ps])
                    c.I("pe", "matmul", r=[xt, wt], out=ps[:, 0:64], lhsT=xt[:, kc, ts_], rhs=wt[:, kc, 0:64], start=(kc == 0), stop=(kc == 15), **kw)
                r_ = rp.next()
                c.I("act", "activation", r=[ps], w=[r_], out=r_[:, 0:64], in_=ps[:, 0:64], func=AF.Copy)
                kb = krb.next()
                self.rope(r_, r_[:, 0:64].rearrange("p (h d) -> p h d", d=64), 1, cos3[:, gt, :], sin3[:, gt, :], kb,
                          kb[:].rearrange("p (h d) -> p h d", d=64), tw.next())
                pt = self.ptr.next()
                c.I("pe", "transpose", r=[kb, self.idb], w=[pt], out=pt[0:64, 0:128], in_=kb[:], identity=self.idb[:])
                kw = dict(w=[krst]) if tt == 0 else dict(pw=[krst])
                k += 1
                self.evac(k, krst[:, ts_], pt[0:64, 0:128], r=[pt], **kw)
            c.dma("sp", self.krT[:, tb0:tb0 + TB], krst[:], key="st_krst", r=[krst], pw=[self.krT])
            for hg in range(4):
                for kind in range(3):
                    wt = wsm.next()
                    if kind == 0:
                        self.wload(wt, wuk[:, :, hg * 512:(hg + 1) * 512], 4, 512, key=wt.b.name)
                    elif kind == 1:
                        self.wload(wt, wuv[:, :, hg * 512:(hg + 1) * 512], 4, 512, key=wt.b.name)
                    else:
                        for j in range(4):
                            kw = dict(w=[wt]) if j == 0 else dict(pw=[wt])
                            c.dma("pool", wt[:, :, j * 128:(j + 1) * 128], wuq[:, :, hg * 4 + j, 0:128], key=wt.b.name, **kw)
                    if kind == 1:
                        for tt in range(4):
                            ts_ = slice(tt * 128, (tt + 1) * 128)
                            ps = self.psArot.next()
                            for kc in range(4):
                                kw = dict(w=[ps]) if kc == 0 else dict(pw=[ps])
                                c.I("pe", "matmul", r=[ckvT, wt], out=ps[:], lhsT=ckvT[:, kc, ts_], rhs=wt[:, kc, 0:512], start=(kc == 0), stop=(kc == 3), **kw)
                            f_ = fst.next()
                            k += 1
                            self.evac(k, f_[:], ps[:], r=[ps], w=[f_])
                            c.dma("sp", self.v_scr[tb0 + tt * 128:tb0 + (tt + 1) * 128, hg * 512:(hg + 1) * 512], f_[:],
                                  key="st_" + f_.b.name, r=[f_], pw=[self.v_scr])
                    else:
                        srcT = ckvT if kind == 0 else cqT
                        dstD = self.knT if kind == 0 else self.qnT
                        for j in range(4):
                            h = hg * 4 + j
                            ps = self.psArot.next()
                            for kc in range(4):
                                kw = dict(w=[ps]) if kc == 0 else dict(pw=[ps])
                                c.I("pe", "matmul", r=[srcT, wt], out=ps[:], lhsT=wt[:, kc, j * 128:(j + 1) * 128], rhs=srcT[:, kc, :], start=(kc == 0), stop=(kc == 3), **kw)
                            f_ = fst.next()
                            k += 1
                            self.evac(k, f_[:], ps[:], r=[ps], w=[f_])
                            c.dma("sp", dstD[h, :, tb0:tb0 + TB], f_[:], key="st_" + f_.b.name, r=[f_], pw=[dstD])
            wt = wsm.next()
            for hh in range(MH):
                kw = dict(w=[wt]) if hh == 0 else dict(pw=[wt])
                c.dma("pool", wt[:, :, hh * 64:(hh + 1) * 64], wuq[:, :, hh, 128:192], key=wt.b.name, **kw)
            for tt in range(4):
                ts_ = slice(tt * 128, (tt + 1) * 128)
                gt = (tb0 // 128) + tt
                r_ = rp.next()
                for hf in range(2):
                    ps = self.psArot.next()
                    for kc in range(4):
                        kw = dict(w=[ps]) if kc == 0 else dict(pw=[ps])
                        c.I("pe", "matmul", r=[cqT, wt], out=ps[:], lhsT=cqT[:, kc, ts_], rhs=wt[:, kc, hf * 512:(hf + 1) * 512], start=(kc == 0), stop=(kc == 3), **kw)
                    k += 1
                    kw = dict(w=[r_]) if hf == 0 else dict(pw=[r_])
                    self.evac(k, r_[:, hf * 512:(hf + 1) * 512], ps[:], r=[ps], **kw)
                qb = qrb.next()
                self.rope(r_, r_[:].rearrange("p (h d) -> p h d", d=64), MH, cos3[:, gt, :], sin3[:, gt, :], qb,
                          qb[:].rearrange("p (h d) -> p h d", d=64), tw.next())
                for hq in range(2):
                    pt = self.ptr.next()
                    for i in range(8):
                        hh = hq * 8 + i
                        kw = dict(w=[pt]) if i == 0 else dict(pw=[pt])
                        c.I("pe", "transpose", r=[qb, self.idb], out=pt[0:64, i * 128:(i + 1) * 128], in_=qb[:, hh * 64:(hh + 1) * 64],
                            identity=self.idb[:], **kw)
                    k += 1
                    kw = dict(w=[qrst]) if (tt == 0 and hq == 0) else dict(pw=[qrst])
                    self.evac(k, qrst[:, hq * 8:(hq + 1) * 8, ts_], pt[0:64, :].rearrange("p (a b) -> p a b", b=128), r=[pt], **kw)
            c.dma("sp", self.qrT[:, :, tb0:tb0 + TB].rearrange("h d t -> d h t"), qrst[:], key="st_qrst", r=[qrst], pw=[self.qrT])
        self.end()

    def p7(self):
        c = self.c
        self.begin()
        qn = c.sbn(2, [128, SEQ], BF16, "qn")
        kn = c.sbn(2, [128, SEQ], BF16, "kn")
        qr = c.sbn(2, [64, SEQ], BF16, "qr")
        kr = c.sb([64, SEQ], BF16, "kr")
        vh = c.sbn(2, [128, 16, 128], BF16, "vh")
        Pb = c.sbn(2, [128, SEQ], BF16, "Pb")
        PT = c.sbn(2, [128, 16, 128], BF16, "PT")
        otok = c.sb([128, 16, 2048], BF16, "otok")
        st = c.sbn(2, [128, 8], F32, "st")
        stT = c.sbn(2, [128, 16, 128], BF16, "stT")
        S_ = self.psA_t
        for s in range(NSEQ):
            t0 = s * SEQ
            c.dma("sp", kr[:], self.krT[:, t0:t0 + SEQ], key="kr", w=[kr], r=[self.krT])
            for h in range(MH):
                q_ = qn.next(); k_ = kn.next(); qr_ = qr.next(); v_ = vh.next()
                c.dma("sp", q_[:], self.qnT[h, :, t0:t0 + SEQ], key=q_.b.name, w=[q_], r=[self.qnT])
                c.dma("sp", k_[:], self.knT[h, :, t0:t0 + SEQ], key=k_.b.name, w=[k_], r=[self.knT])
                c.dma("sp", qr_[:], self.qrT[h, :, t0:t0 + SEQ], key=qr_.b.name, w=[qr_], r=[self.qrT])
                c.dma("sp", v_[:], self.v_scr[t0:t0 + SEQ, h * 128:(h + 1) * 128].rearrange("(n p) d -> p n d", p=128),
                      key=v_.b.name, w=[v_], r=[self.v_scr])
                for i in range(16):
                    nk = (i + 1) * 128
                    nb = (nk + 511) // 512
                    qs = slice(i * 128, (i + 1) * 128)
                    if nb <= 2:
                        self._sflip = 1 - getattr(self, "_sflip", 0)
                        b0 = 2 * self._sflip
                    else:
                        b0 = 0
                    so = b0 * 512
                    banks = self.psA[b0:b0 + nb]
                    for kb in range(nb):
                        ncols = min(512, nk - kb * 512)
                        ks = slice(kb * 512, kb * 512 + ncols)
                        os_ = slice(so + kb * 512, so + kb * 512 + ncols)
                        last = (kb == nb - 1)
                        c.I("pe", "matmul", r=[q_, k_], w=[banks[kb]], out=S_[:, os_], lhsT=q_[:, qs], rhs=k_[:, ks], start=True, stop=False)
                        c.I("pe", "matmul", r=[qr_, kr], pw=[banks[kb]], out=S_[:, os_], lhsT=qr_[:, qs], rhs=kr[:, ks], start=False, stop=(not last))
                        if last:
                            c.I("pe", "matmul", r=[self.idb, self.maskb], pw=[banks[kb]], out=S_[:, so + i * 128:so + (i + 1) * 128], lhsT=self.idb[:],
                                rhs=self.maskb[:], start=False, stop=True)
                    t_ = st.next()
                    c.I("dve", "reduce_max", r=banks, w=[t_], out=t_[:, 0:1], in_=S_[:, so:so + nk], axis=AX.X)
                    c.I("dve", "tensor_scalar", r=[t_], pw=[t_], out=t_[:, 1:2], in0=t_[:, 0:1], scalar1=-SCALE, scalar2=None, op0=ALU.mult)
                    p_ = Pb.next()
                    c.I("act", "activation", r=banks + [t_], w=[p_], pw=[t_], out=p_[:, 0:nk], in_=S_[:, so:so + nk], func=AF.Exp,
                        bias=t_[:, 1:2], scale=SCALE, accum_out=t_[:, 2:3])
                    pt_ = PT.next()
                    self.transpose_to(p_, lambda j, p_=p_: p_[:, j * 128:(j + 1) * 128], i + 1, pt_,
                                      lambda j0, n, pt_=pt_: pt_[:, j0:j0 + n, :], first_write=True)
                    po = self.pm.next()
                    for kb in range(i + 1):
                        kw = dict(w=[po]) if kb == 0 else dict(pw=[po])
                        c.I("pe", "matmul", r=[pt_, v_], out=po[:, 0:128], lhsT=pt_[:, kb, :], rhs=v_[:, kb, :], start=(kb == 0), stop=(kb == i), **kw)
                    c.I("dve", "reciprocal", r=[t_], pw=[t_], out=t_[:, 3:4], in_=t_[:, 2:3])
                    kw = dict(w=[otok]) if (h == 0 and i == 0) else dict(pw=[otok])
                    c.I("act", "activation", r=[po, t_], out=otok[:, i, h * 128:(h + 1) * 128], in_=po[:, 0:128], func=AF.Copy,
                        scale=t_[:, 3:4], **kw)
            for i in range(16):
                sT = stT.next()
                self.transpose_to(otok, lambda j, i=i: otok[:, i, j * 128:(j + 1) * 128], 16, sT,
                                  lambda j0, n, sT=sT: sT[:, j0:j0 + n, :], first_write=True)
                c.dma("sp", self.oT[:, t0 + i * 128:t0 + (i + 1) * 128].rearrange("(c p) t -> p c t", p=128), sT[:],
                      key="st_" + sT.b.name, r=[sT], pw=[self.oT])
        self.end()

    def p8(self):
        W = self.Win
        self.linear_ln(self.oT, 16, W("mla_w_o").t, self.hA, W("ln1_g").t[1], W("ln1_b").t[1], self.hB, self.hBT, NCOL=512)

    def p9(self):
        c = self.c
        W = self.Win
        NCH, CH = 23, 512
        NSLOT = NCH * CH
        resid, xT_unused = self.hB, self.hBT
        out_tok, out_T = self.hA, self.hAT
        Xs = c.dram("moe_xs", [NSLOT, D], BF16)
        Ys = c.dram("moe_ys", [NSLOT, D], F32)
        WG, WU, WD = W("moe_w_gate"), W("moe_w_up"), W("moe_w_down")
        IOA = bass.IndirectOffsetOnAxis
        outer = ExitStack()
        c.stack = outer
        c.keymap = {}
        NT = T // 128
        M1 = c.sb([128, NT, NE], F32, "M1")
        M2 = c.sb([128, NT, NE], F32, "M2")
        WT = c.sb([128, NT, 2], F32, "WT")
        RK = c.sb([128, NT, NE], F32, "RK")
        POSf = c.sb([128, NT, 2], F32, "POSf")
        POSi = c.sb([128, NT, 2], I32, "POSi")
        idxi = c.sb([128, NCH, 22], I32, "idxi")
        c.stack = ExitStack()
        wr = c.sb([128, 16, NE], F32, "wr")
        c.dma("sp", wr[:], W("moe_w_router").t.rearrange("(kc p) e -> p kc e", p=128), key="wr", w=[wr])
        br = self.bcast_load(W("moe_b_router").t, NE, "br")
        hTf = c.sb([128, 16, 128], F32, "hTf")
        res = c.sbn(2, [128, D], F32, "res")
        hbf_all = c.sb([128, T // 128, D], BF16, "hbfall")
        rt = c.sbn(2, [128, 64], F32, "rt")
        run = c.sb([128, NE], F32, "run")
        c.I("pool", "memset", w=[run], ap=run[:], constant=0.0)
        suf = c.sb([128, 128], F32, "suf")
        c.I("pool", "affine_select", r=[self.ones_f], w=[suf], out=suf[:], in_=self.ones_f[:], pattern=[[1, 128]],
            compare_op=ALU.is_gt, fill=0.0, base=0, channel_multiplier=-1)
        k = 0
        for tt in range(NT):
            t0 = tt * 128
            rs = res.next()
            c.dma("sp", rs[:], resid[t0:t0 + 128, :], key=rs.b.name, w=[rs], r=[resid])
            hkw = dict(w=[hbf_all]) if tt == 0 else dict(pw=[hbf_all])
            c.I("act", "activation", r=[rs], out=hbf_all[:, tt, :], in_=rs[:], func=AF.Copy, **hkw)
            for q in range(4):
                pm = self.pm.next()
                for i in range(4):
                    kc = q * 4 + i
                    kw = dict(w=[pm]) if i == 0 else dict(pw=[pm])
                    c.I("pe", "transpose", r=[rs, self.idf], out=pm[:, i * 128:(i + 1) * 128],
                        in_=rs[:, kc * 128:(kc + 1) * 128], identity=self.idf[:], **kw)
                k += 1
                kw = dict(w=[hTf]) if q == 0 else dict(pw=[hTf])
                self.evac(k, hTf[:, q * 4:(q + 1) * 4, :], pm[:].rearrange("p (a b) -> p a b", b=128), r=[pm], **kw)
            pm = self.pm.next()
            for kc in range(16):
                kw = dict(w=[pm]) if kc == 0 else dict(pw=[pm])
                c.I("pe", "matmul", r=[hTf, wr], out=pm[:, 0:NE], lhsT=hTf[:, kc, :], rhs=wr[:, kc, :],
                    start=(kc == 0), stop=(kc == 15), **kw)
            r_ = rt.next()
            lg = r_[:, 0:8]; oh1 = M1[:, tt, :]; l2 = r_[:, 16:24]; oh2 = M2[:, tt, :]
            m1 = r_[:, 32:33]; m2 = r_[:, 33:34]; dd = r_[:, 34:35]; ee = r_[:, 35:36]; e1 = r_[:, 36:37]
            w1 = WT[:, tt, 0:1]; w2 = WT[:, tt, 1:2]; mm = r_[:, 40:48]
            c.I("dve", "tensor_tensor", r=[pm, br], w=[r_], out=lg, in0=pm[:, 0:NE], in1=br[:], op=ALU.add)
            c.I("dve", "reduce_max", r=[r_], pw=[r_], out=m1, in_=lg, axis=AX.X)
            c.I("dve", "tensor_scalar", r=[r_], pw=[M1], out=oh1, in0=lg, scalar1=m1, scalar2=None, op0=ALU.is_equal)
            c.I("dve", "scalar_tensor_tensor", r=[r_, M1], pw=[r_], out=l2, in0=oh1, scalar=-1.0e30, in1=lg, op0=ALU.mult, op1=ALU.add)
            c.I("dve", "reduce_max", r=[r_], pw=[r_], out=m2, in_=l2, axis=AX.X)
            c.I("dve", "tensor_scalar", r=[r_], pw=[M2], out=oh2, in0=l2, scalar1=m2, scalar2=None, op0=ALU.is_equal)
            c.I("dve", "tensor_tensor", r=[r_], pw=[r_], out=dd, in0=m2, in1=m1, op=ALU.subtract)
            c.I("act", "activation", r=[r_], pw=[r_], out=ee, in_=dd, func=AF.Exp)
            c.I("dve", "tensor_scalar", r=[r_], pw=[r_], out=e1, in0=ee, scalar1=1.0, scalar2=None, op0=ALU.add)
            c.I("dve", "reciprocal", r=[r_], pw=[WT], out=w1, in_=e1)
            c.I("dve", "tensor_tensor", r=[r_, WT], pw=[WT], out=w2, in0=ee, in1=w1, op=ALU.mult)
            c.I("dve", "tensor_tensor", r=[M1, M2], pw=[r_], out=mm, in0=oh1, in1=oh2, op=ALU.add)
            pr_ = self.pm.next()
            c.I("pe", "matmul", r=[suf, r_], w=[pr_], out=pr_[:, 0:NE], lhsT=suf[:], rhs=mm, start=True, stop=True)
            c.I("pe", "matmul", r=[self.ones_f, r_], pw=[pr_], out=pr_[:, 8:16], lhsT=self.ones_f[:], rhs=mm, start=True, stop=True)
            c.I("dve", "tensor_tensor", r=[pr_, run], pw=[RK], out=RK[:, tt, :], in0=pr_[:, 0:NE], in1=run[:], op=ALU.add)
            c.I("dve", "tensor_tensor", r=[pr_, run], w=[run], out=run[:], in0=pr_[:, 8:16], in1=run[:], op=ALU.add)
        cs = c.sb([128, 64], F32, "cs")
        ci_ = c.sb([128, 8], I32, "ci")
        x_ = cs[:, 0:8]; xr = cs[:, 8:16]; fx = cs[:, 16:24]; pc = cs[:, 24:32]; off = cs[:, 32:40]; eoff = cs[:, 40:48]
        c.I("dve", "tensor_scalar", r=[run], w=[cs], out=x_, in0=run[:], scalar1=511.0, scalar2=1.0 / 512, op0=ALU.add, op1=ALU.mult)
        c.I("dve", "tensor_copy", r=[cs], w=[ci_], out=ci_[:], in_=x_)
        c.I("dve", "tensor_copy", r=[ci_], pw=[cs], out=xr, in_=ci_[:])
        c.I("dve", "tensor_tensor", r=[cs], pw=[cs], out=fx, in0=xr, in1=x_, op=ALU.is_gt)
        c.I("dve", "tensor_tensor", r=[cs], pw=[cs], out=xr, in0=xr, in1=fx, op=ALU.subtract)
        c.I("dve", "tensor_scalar", r=[cs], pw=[cs], out=pc, in0=xr, scalar1=512.0, scalar2=None, op0=ALU.mult)
        c.I("pool", "memset", r=[cs], pw=[cs], ap=cs[:, 32:33], constant=0.0)
        for e in range(1, NE):
            c.I("dve", "tensor_tensor", r=[cs], pw=[cs], out=cs[:, 32 + e:33 + e], in0=cs[:, 31 + e:32 + e], in1=cs[:, 23 + e:24 + e], op=ALU.add)
        c.I("dve", "tensor_tensor", r=[cs], pw=[cs], out=eoff, in0=off, in1=pc, op=ALU.add)
        cst_i = c.sb([128, NCH], I32, "csti")
        c.I("pool", "iota", w=[cst_i], out=cst_i[:], pattern=[[CH, NCH]], base=0, channel_multiplier=0)
        cst = c.sb([128, NCH], F32, "cst")
        c.I("dve", "tensor_copy", r=[cst_i], w=[cst], out=cst[:], in_=cst_i[:])
        ec = c.sb([128, NCH], F32, "ec")
        tq = c.sb([128, NCH], F32, "tq")
        for e in range(NE):
            if e == 0:
                c.I("dve", "tensor_scalar", r=[cst, cs], w=[ec], out=ec[:], in0=cst[:], scalar1=cs[:, 40:41], scalar2=None, op0=ALU.is_ge)
            else:
                c.I("dve", "tensor_scalar", r=[cst, cs], w=[tq], out=tq[:], in0=cst[:], scalar1=cs[:, 40 + e:41 + e], scalar2=None, op0=ALU.is_ge)
                c.I("dve", "tensor_tensor", r=[ec, tq], w=[ec], out=ec[:], in0=ec[:], in1=tq[:], op=ALU.add)
        c.I("dve", "tensor_scalar", r=[ec], w=[ec], out=ec[:], in0=ec[:], scalar1=float(NE - 1), scalar2=None, op0=ALU.min)
        pcol_i = c.sb([128, 1], I32, "pcoli")
        c.I("pool", "iota", w=[pcol_i], out=pcol_i[:], pattern=[[0, 1]], base=0, channel_multiplier=1)
        pcol = c.sb([128, 1], F32, "pcol")
        c.I("dve", "tensor_copy", r=[pcol_i], w=[pcol], out=pcol[:], in_=pcol_i[:])
        fco_i = c.sb([128, 22], I32, "fcoi")
        c.I("pool", "iota", w=[fco_i], out=fco_i[:], pattern=[[128, 22]], base=0, channel_multiplier=0)
        fco = c.sb([128, 22], F32, "fco")
        c.I("dve", "tensor_copy", r=[fco_i], w=[fco], out=fco[:], in_=fco_i[:])
        base = c.sb([128, NCH], F32, "base")
        c.I("dve", "tensor_scalar", r=[ec, pcol], w=[base], out=base[:], in0=ec[:], scalar1=float(DFE), scalar2=pcol[:, 0:1], op0=ALU.mult, op1=ALU.add)
        idxf = c.sb([128, NCH, 22], F32, "idxf")
        c.I("dve", "tensor_tensor", r=[base, fco], w=[idxf], out=idxf[:], in0=bc(base[:].unsqueeze(2), [128, NCH, 22]),
            in1=bc(fco[:].unsqueeze(1), [128, NCH, 22]), op=ALU.add)
        c.I("dve", "tensor_copy", r=[idxf], w=[idxi], out=idxi[:], in_=idxf[:])
        t8 = c.sbn(2, [128, 16], F32, "t8")
        for tt in range(NT):
            t0 = tt * 128
            a_ = t8.next()
            c.I("dve", "tensor_tensor", r=[RK, cs], w=[a_], out=a_[:, 0:8], in0=RK[:, tt, :], in1=off, op=ALU.add)
            for kk, M in enumerate((M1, M2)):
                c.I("dve", "tensor_tensor", r=[a_, M], pw=[a_], out=a_[:, 8:16], in0=a_[:, 0:8], in1=M[:, tt, :], op=ALU.mult)
                kw = dict(w=[POSf]) if (tt == 0 and kk == 0) else dict(pw=[POSf])
                c.I("dve", "reduce_sum", r=[a_], out=POSf[:, tt, kk:kk + 1], in_=a_[:, 8:16], axis=AX.X, **kw)
        c.I("dve", "tensor_copy", r=[POSf], w=[POSi], out=POSi[:], in_=POSf[:])
        for tt in range(NT):
            for kk in range(2):
                c.I("pool", "indirect_dma_start", r=[hbf_all, POSi], pw=[Xs], key="sc_hbf", out=Xs[:, :],
                    out_offset=IOA(ap=POSi[:, tt, kk:kk + 1], axis=0), in_=hbf_all[:, tt, :], in_offset=None)
        c.S.barrier()
        c.stack.close()
        c.stack = ExitStack()
        xin = c.sb([128, 4, D], BF16, "xin")
        xt = c.sb([128, 16, CH], BF16, "xt")
        wgs = c.sbn(2, [128, 16, 128], BF16, "wg")
        wus = c.sbn(2, [128, 16, 128], BF16, "wu")
        wdl = [c.sb([128, D], BF16, "wd") for _ in range(11)]
        actT = c.sb([128, 11, CH], BF16, "actT")
        acc = c.sb([128, 4, D], F32, "acc")
        sg = c.sbn(2, [128, 512], F32, "sg")
        for ch in range(NCH):
            s0 = ch * CH
            c.dma("sp", xin[:], Xs[s0:s0 + CH, :].rearrange("(n p) d -> p n d", p=128), key="xin", w=[xin], r=[Xs])
            for n in range(4):
                self.transpose_to(xin, lambda j, n=n: xin[:, n, j * 128:(j + 1) * 128], 16, xt,
                                  lambda j0, nn, n=n: xt[:, j0:j0 + nn, n * 128:(n + 1) * 128], first_write=(n == 0))
            for u in range(2):
                for fl in range(11):
                    fc = u * 11 + fl
                    wg = wgs.next()
                    wu = wus.next()
                    ix = IOA(ap=idxi[:, ch, fc:fc + 1], axis=0)
                    c.I("pool", "indirect_dma_start", r=[idxi], w=[wg], key=wg.b.name, out=wg[:].rearrange("p a b -> p (a b)"),
                        out_offset=None, in_=WG[:, :], in_offset=ix)
                    c.I("pool", "indirect_dma_start", r=[idxi], w=[wu], key=wu.b.name, out=wu[:].rearrange("p a b -> p (a b)"),
                        out_offset=None, in_=WU[:, :], in_offset=IOA(ap=idxi[:, ch, fc:fc + 1], axis=0))
                    c.I("pool", "indirect_dma_start", r=[idxi], w=[wdl[fl]], key=wdl[fl].b.name, out=wdl[fl][:],
                        out_offset=None, in_=WD[:, :], in_offset=IOA(ap=idxi[:, ch, fc:fc + 1], axis=0))
                    psg = self.psArot.next()
                    for kc in range(16):
                        kw = dict(w=[psg]) if kc == 0 else dict(pw=[psg])
                        c.I("pe", "matmul", r=[xt, wg], out=psg[:], lhsT=wg[:, kc, :], rhs=xt[:, kc, :], start=(kc == 0), stop=(kc == 15), **kw)
                    psu = self.psArot.next()
                    for kc in range(16):
                        kw = dict(w=[psu]) if kc == 0 else dict(pw=[psu])
                        c.I("pe", "matmul", r=[xt, wu], out=psu[:], lhsT=wu[:, kc, :], rhs=xt[:, kc, :], start=(kc == 0), stop=(kc == 15), **kw)
                    s1 = sg.next()
                    c.I("act", "activation", r=[psg], w=[s1], out=s1[:], in_=psg[:], func=AF.Silu)
                    kw = dict(w=[actT]) if fl == 0 else dict(pw=[actT])
                    c.I("dve", "tensor_tensor", r=[s1, psu], out=actT[:, fl, :], in0=s1[:], in1=psu[:], op=ALU.mult, **kw)
                for dblk in range(4):
                    ds_ = slice(dblk * 512, (dblk + 1) * 512)
                    for tt in range(4):
                        ps = self.psArot.next()
                        for fl in range(11):
                            kw = dict(w=[ps]) if fl == 0 else dict(pw=[ps])
                            c.I("pe", "matmul", r=[actT, wdl[fl]], out=ps[:], lhsT=actT[:, fl, tt * 128:(tt + 1) * 128],
                                rhs=wdl[fl][:, ds_], start=(fl == 0), stop=(fl == 10), **kw)
                        akw = dict(w=[acc]) if (u == 0 and dblk == 0 and tt == 0) else dict(pw=[acc])
                        if u == 0:
                            k += 1
                            self.evac(k, acc[:, tt, ds_], ps[:], r=[ps], **akw)
                        else:
                            c.I("dve", "tensor_tensor", r=[ps, acc], out=acc[:, tt, ds_], in0=ps[:], in1=acc[:, tt, ds_], op=ALU.add, **akw)
            c.dma("sp", Ys[s0:s0 + CH, :].rearrange("(n p) d -> p n d", p=128), acc[:], key="st_acc", r=[acc], pw=[Ys])
        c.S.barrier()
        c.stack.close()
        c.stack = ExitStack()
        g_bc = self.bcast_load(W("ln2_g").t[1], D, "lng")
        b_bc = self.bcast_load(W("ln2_b").t[1], D, "lnb")
        g1s = c.sbn(2, [128, D], F32, "g1")
        g2s = c.sbn(2, [128, D], F32, "g2")
        res = c.sbn(2, [128, D], F32, "res")
        hb = c.sbn(2, [128, D], BF16, "hb")
        stT = c.sb([128, 16, 512], BF16, "stT")
        for tt in range(NT):
            t0 = tt * 128
            g1 = g1s.next()
            g2 = g2s.next()
            rs = res.next()
            c.I("pool", "indirect_dma_start", r=[Ys, POSi], w=[g1], key=g1.b.name, out=g1[:], out_offset=None, in_=Ys[:, :],
                in_offset=IOA(ap=POSi[:, tt, 0:1], axis=0))
            c.I("pool", "indirect_dma_start", r=[Ys, POSi], w=[g2], key=g2.b.name, out=g2[:], out_offset=None, in_=Ys[:, :],
                in_offset=IOA(ap=POSi[:, tt, 1:2], axis=0))
            c.dma("sp", rs[:], resid[t0:t0 + 128, :], key=rs.b.name, w=[rs], r=[resid])
            c.I("dve", "tensor_scalar", r=[g1, WT], w=[g1], out=g1[:], in0=g1[:], scalar1=WT[:, tt, 0:1], scalar2=None, op0=ALU.mult)
            c.I("dve", "scalar_tensor_tensor", r=[g2, WT, g1], w=[g1], out=g1[:], in0=g2[:], scalar=WT[:, tt, 1:2], in1=g1[:],
                op0=ALU.mult, op1=ALU.add)
            c.I("dve", "scalar_tensor_tensor", r=[rs, g1], w=[g1], out=g1[:], in0=rs[:], scalar=ALPHA, in1=g1[:], op0=ALU.mult, op1=ALU.add)
            b = hb.next()
            self.ln_rows(g1, g1[:], g_bc, b_bc, g1, g1[:], b, b[:])
            c.dma("sp", out_tok[t0:t0 + 128, :], g1[:], key="st_" + g1.b.name, r=[g1], pw=[out_tok])
            q = tt % 4
            self.transpose_to(b, lambda j, b=b: b[:, j * 128:(j + 1) * 128], 16, stT,
                              lambda j0, n, q=q: stT[:, j0:j0 + n, q * 128:(q + 1) * 128], first_write=(q == 0))
            if q == 3:
                tb0 = (tt // 4) * 512
                c.dma("sp", out_T[:, tb0:tb0 + 512].rearrange("(c p) t -> p c t", p=128), stT[:], key="st_stT", r=[stT], pw=[out_T])
        c.S.barrier()
        c.stack.close()
        outer.close()
        c.stack = None

    def build(self):
        c = self.c
        self.consts()
        g = self.gstack
        c.stack = g
        self.one_col = c.sb([128, 1], F32, "onecol")
        c.I("pool", "memset", w=[self.one_col], ap=self.one_col[:], constant=1.0)
        self.ln_setup()
        c.stack = None
        for ph in ("p1", "p2", "p3", "p4", "p5", "p6", "p7", "p8", "p9", "p10"):
            if self.phase(ph) and hasattr(self, ph):
                getattr(self, ph)()
        c.S.barrier()
        c.S.emit()


def build_program(dbg=(), phases=None):
    nc = bass.Bass("TRN2", target_bir_lowering=False)
    pr = Prog(nc, dbg=dbg, phases=phases)
    pr.build()
    return nc, pr


def _tile_rows(w, nfc):
    w = np.asarray(w)
    return w.reshape(16, 128, nfc, 128).transpose(2, 1, 0, 3).reshape(nfc * 128, D)


def _tile_gu(w):
    w = np.asarray(w)
    return w.reshape(NE, 16, 128, 22, 128).transpose(0, 3, 2, 1, 4).reshape(NE * DFE, D)


def make_in_maps(inputs):
    f = lambda a: np.ascontiguousarray(np.asarray(a))
    invf = (10000.0 ** (-np.arange(0, 64, 2, dtype=np.float32) / 64)).astype(np.float32)
    shared = {
        "invf": invf,
        "ssm_w_in": f(inputs["ssm_w_in"][0]),
        "conv_w": f(np.asarray(inputs["ssm_conv_w"][0]).reshape(4, 48, 128).transpose(2, 1, 0)),
        "conv_b": f(np.asarray(inputs["ssm_conv_b"][0]).reshape(48, 128).T),
        "ssm_dt_bias": f(inputs["ssm_dt_bias"][0]), "ssm_a_log": f(inputs["ssm_a_log"][0]),
        "ssm_d": f(inputs["ssm_d"][0]), "ssm_norm_g": f(inputs["ssm_norm_g"][0]),
        "ssm_w_out": f(inputs["ssm_w_out"][0]),
        "kv_w_down": f(inputs["kv_w_down"]), "kv_norm_g": f(inputs["kv_norm_g"]), "kv_w_rope": f(inputs["kv_w_rope"]),
        "kv_w_uk": f(inputs["kv_w_uk"]), "kv_w_uv": f(inputs["kv_w_uv"]),
        "mla_w_dq": f(inputs["mla_w_dq"][0]), "mla_q_norm_g": f(inputs["mla_q_norm_g"][0]),
        "mla_w_uq": f(inputs["mla_w_uq"][0]), "mla_w_o": f(inputs["mla_w_o"][0]),
        "ffn_w_gate": f(_tile_rows(inputs["ffn_w_gate"][0], 44)), "ffn_w_up": f(_tile_rows(inputs["ffn_w_up"][0], 44)),
        "ffn_w_down": f(inputs["ffn_w_down"][0]),
        "moe_w_router": f(inputs["moe_w_router"][0]), "moe_b_router": f(inputs["moe_b_router"][0]),
        "moe_w_gate": f(_tile_gu(inputs["moe_w_gate"][0])), "moe_w_up": f(_tile_gu(inputs["moe_w_up"][0])),
        "moe_w_down": f(np.asarray(inputs["moe_w_down"][0]).reshape(NE * DFE, D)),
        "ln1_g": f(inputs["ln1_g"]), "ln1_b": f(inputs["ln1_b"]), "ln2_g": f(inputs["ln2_g"]), "ln2_b": f(inputs["ln2_b"]),
        "ple_w_proj": f(inputs["ple_w_proj"]), "ple_w_gate": f(inputs["ple_w_gate"]),
    }
    x = np.asarray(inputs["x"])
    p = np.asarray(inputs["p"])
    pos = np.asarray(inputs["positions"])
    maps = []
    for ci in range(NCORES):
        b0 = ci * NSEQ
        m = dict(shared)
        m["x"] = f(x[b0:b0 + NSEQ].reshape(T, D))
        m["p"] = f(p[:, b0:b0 + NSEQ].reshape(2, T, PLE))
        m["pos"] = f(pos[b0:b0 + NSEQ].reshape(T // 128, 128).T.astype(np.int32))
        maps.append(m)
    return maps


def kernel(**inputs):
    nc, pr = build_program()
    maps = make_in_maps(inputs)
    used = set(pr._W.keys())
    maps = [{k: v for k, v in m.items() if k in used} for m in maps]
    res = run_bass_kernel_spmd(nc, maps, core_ids=list(range(NCORES)))
    out = np.stack([r["y"].reshape(NSEQ, SEQ, D) for r in res.results], 0).reshape(NCORES * NSEQ, SEQ, D)
    return out.astype(np.float32)
```

```python
import math
from contextlib import ExitStack
import numpy as np
import concourse.bass as bass
import concourse.mybir as mybir
from concourse.bass_utils import run_bass_kernel_spmd

F32 = mybir.dt.float32
BF16 = mybir.dt.bfloat16
I32 = mybir.dt.int32
AF = mybir.ActivationFunctionType
ALU = mybir.AluOpType
AX = mybir.AxisListType

NCORES = 8
D = 2048
SEQ = 2048
NSEQ = 2
T = NSEQ * SEQ
DIN = 4096
NH = 64
HD = 64
NG = 8
NST = 128
CONVD = 6144
DINP = 10304
DFF = 5632
NE = 8
DFE = 2816
PLE = 256
MH = 16
QR = 512
KVR = 512
ALPHA = (2.0 * 2) ** 0.25
LN_EPS = 1e-5
RMS_EPS = 1e-6
SCALE = (128 + 64) ** -0.5
TWO_PI = 2.0 * math.pi

SAME_ENGINE_SYNC = True
EMBED_WAIT = True
KROT = 4


class Buf:
    __slots__ = ("name", "writers", "readers", "prev")

    def __init__(self, name):
        self.name = name
        self.writers = []
        self.readers = []
        self.prev = []


class Stream:
    __slots__ = ("name", "is_dma", "ops", "sems")

    def __init__(self, name, is_dma):
        self.name = name
        self.is_dma = is_dma
        self.ops = []
        self.sems = None


class Op:
    __slots__ = ("eng", "fn", "stream", "pos", "deps", "signaled", "sig")

    def __init__(self, eng, fn, stream):
        self.eng = eng
        self.fn = fn
        self.stream = stream
        self.pos = len(stream.ops)
        stream.ops.append(self)
        self.deps = []
        self.signaled = False
        self.sig = None


class Sched:
    def __init__(self, nc):
        self.nc = nc
        self.engs = {"pe": [], "act": [], "dve": [], "pool": [], "sp": []}
        self.streams = {e: Stream(e, False) for e in self.engs}
        self.dma_streams = {}
        self.waited = {e: {} for e in self.engs}

    def _dma_stream(self, key):
        s = self.dma_streams.get(key)
        if s is None:
            s = Stream("dma_" + key, True)
            self.dma_streams[key] = s
        return s

    def _add_dep(self, op, p):
        if p is op:
            return
        eng = op.eng
        if (not p.stream.is_dma) and p.stream.name == eng:
            if eng == "pe" or not SAME_ENGINE_SYNC:
                return
        if self.waited[eng].get(p.stream.name, -1) >= p.pos:
            return
        for i, q in enumerate(op.deps):
            if q.stream is p.stream:
                if q.pos < p.pos:
                    op.deps[i] = p
                return
        op.deps.append(p)

    def op(self, eng, fn, reads=(), writes=(), pwrites=(), dma_key=None, extra=()):
        stream = self._dma_stream(dma_key) if dma_key is not None else self.streams[eng]
        o = Op(eng, fn, stream)
        for b in reads:
            for p in b.writers:
                self._add_dep(o, p)
        for b in writes:
            for p in b.readers:
                self._add_dep(o, p)
            for p in b.writers:
                self._add_dep(o, p)
        for b in pwrites:
            for p in b.readers:
                self._add_dep(o, p)
            for p in b.prev:
                self._add_dep(o, p)
        for p in extra:
            self._add_dep(o, p)
        w = self.waited[eng]
        for p in o.deps:
            p.signaled = True
            if w.get(p.stream.name, -1) < p.pos:
                w[p.stream.name] = p.pos
        for b in writes:
            b.prev = self._compact(b.readers + b.writers)
            b.writers = [o]
            b.readers = []
        for b in pwrites:
            b.writers.append(o)
            if len(b.writers) > 48:
                b.writers = self._compact(b.writers)
        for b in reads:
            b.readers.append(o)
            if len(b.readers) > 48:
                b.readers = self._compact(b.readers)
        self.engs[eng].append(o)
        return o

    @staticmethod
    def _compact(ops):
        last = {}
        for r in ops:
            k = r.stream.name
            if k not in last or last[k].pos < r.pos:
                last[k] = r
        return list(last.values())

    def barrier(self):
        lasts = []
        for s in list(self.streams.values()) + list(self.dma_streams.values()):
            if s.ops:
                lasts.append(s.ops[-1])
        for e in self.engs:
            self.op(e, ("nop", {}), extra=lasts)

    def emit(self):
        nc = self.nc
        nsem = 0
        for s in list(self.streams.values()) + list(self.dma_streams.values()):
            if not any(o.signaled for o in s.ops):
                continue
            if s.is_dma:
                s.sems = [nc.alloc_semaphore("s_" + s.name)]
                nsem += 1
                cnt = 0
                for o in s.ops:
                    cnt += 16
                    o.sig = (s.sems[0], cnt)
            else:
                s.sems = [nc.alloc_semaphore(f"s_{s.name}{k}") for k in range(KROT)]
                nsem += KROT
                j = 0
                for o in s.ops:
                    if o.signaled:
                        o.sig = (s.sems[j % KROT], j // KROT + 1)
                        j += 1
        self.nsem = nsem
        engmap = {"pe": "tensor", "act": "scalar", "dve": "vector", "pool": "gpsimd", "sp": "sync"}
        with nc.Block() as block:
            for ename, ops in self.engs.items():
                if not ops:
                    continue

                def body(e, ops=ops):
                    for o in ops:
                        deps = o.deps
                        emb = None
                        if EMBED_WAIT and deps and not callable(o.fn):
                            emb = deps[-1]
                            deps = deps[:-1]
                        for p in deps:
                            e.wait_ge(p.sig[0], p.sig[1])
                        if callable(o.fn):
                            ins = o.fn(e)
                        else:
                            name, kw = o.fn
                            ins = getattr(e, name)(**kw)
                        if emb is not None:
                            ins._wait_ge(emb.sig[0], emb.sig[1])
                        if o.sig is not None:
                            if o.stream.is_dma:
                                ins.then_inc(o.sig[0], 16)
                            elif o.signaled:
                                ins.then_inc(o.sig[0], 1)

                getattr(block, engmap[ename])(body)


class Tl:
    __slots__ = ("t", "b")

    def __init__(self, t, b):
        self.t = t
        self.b = b

    def __getitem__(self, k):
        return self.t[k]


class Rot:
    def __init__(self, tiles):
        self.tiles = tiles
        self.i = 0

    def next(self):
        t = self.tiles[self.i % len(self.tiles)]
        self.i += 1
        return t


class Ctx:
    def __init__(self, nc, dbg=()):
        self.nc = nc
        self.S = Sched(nc)
        self.n = 0
        self.dbg = set(dbg)
        self.stack = None
        self.keymap = {}

    def sb(self, shape, dt, name=None):
        self.n += 1
        name = (name or "t") + f"_{self.n}"
        h = self.stack.enter_context(self.nc.sbuf_tensor(name, list(shape), dt))
        return Tl(h, Buf(name))

    def sbn(self, n, shape, dt, name=None):
        return Rot([self.sb(shape, dt, name) for _ in range(n)])

    def ps(self, shape, dt, name):
        return Tl(self.nc.alloc_psum_tensor(name, list(shape), dt), Buf(name))

    def dram(self, name, shape, dt):
        kind = "ExternalOutput" if name in self.dbg else "Internal"
        return Tl(self.nc.dram_tensor(name, list(shape), dt, kind=kind).ap(), Buf(name))

    def inp(self, name, shape, dt):
        return Tl(self.nc.dram_tensor(name, list(shape), dt, kind="ExternalInput").ap(), Buf(name))

    def I(self, eng, name, r=(), w=(), pw=(), key=None, **kw):
        if key is not None:
            key = self.keymap.setdefault(key, f"k{len(self.keymap)}")
        return self.S.op(eng, (name, kw), reads=[x.b for x in r], writes=[x.b for x in w],
                         pwrites=[x.b for x in pw], dma_key=key)

    def dma(self, eng, out, in_, key, r=(), w=(), pw=()):
        return self.I(eng, "dma_start", r=r, w=w, pw=pw, key=key, out=out, in_=in_)


def bc(ap, shape):
    return ap.broadcast_to(list(shape))


class Prog:
    def __init__(self, nc, dbg=(), phases=None):
        self.c = Ctx(nc, dbg)
        self.nc = nc
        self.phases = phases
        c = self.c
        self.wshapes = dict([
            ("ssm_w_in", [D, DINP]), ("conv_w", [128, 48, 4]), ("conv_b", [128, 48]),
            ("ssm_dt_bias", [NH]), ("ssm_a_log", [NH]), ("ssm_d", [NH]), ("ssm_norm_g", [DIN]),
            ("ssm_w_out", [DIN, D]),
            ("kv_w_down", [D, KVR]), ("kv_norm_g", [KVR]), ("kv_w_rope", [D, 64]),
            ("kv_w_uk", [KVR, 2048]), ("kv_w_uv", [KVR, 2048]),
            ("mla_w_dq", [D, QR]), ("mla_q_norm_g", [QR]), ("mla_w_uq", [QR, 3072]), ("mla_w_o", [2048, D]),
            ("ffn_w_gate", [DFF, D]), ("ffn_w_up", [DFF, D]), ("ffn_w_down", [DFF, D]),
            ("moe_w_router", [D, NE]), ("moe_b_router", [NE]),
            ("moe_w_gate", [NE * DFE, D]), ("moe_w_up", [NE * DFE, D]), ("moe_w_down", [NE * DFE, D]),
            ("ln1_g", [2, D]), ("ln1_b", [2, D]), ("ln2_g", [2, D]), ("ln2_b", [2, D]),
            ("ple_w_proj", [2, PLE, D]), ("ple_w_gate", [2, D, D]),
            ("x", [T, D]), ("p", [2, T, PLE]), ("invf", [32]),
        ])
        self._W = {}
        self.y = Tl(nc.dram_tensor("y", [T, D], F32, kind="ExternalOutput").ap(), Buf("y"))
        self.z_scr = c.dram("z_scr", [T, DIN], F32)
        self.dt_scr = c.dram("dt_scr", [T, NH], F32)
        self.xbcT = c.dram("xbcT", [48, 128, T], BF16)
        self.xbc_tok = c.dram("xbc_tok", [T, 5120], BF16)
        self.ynT = c.dram("ynT", [DIN, T], BF16)
        self.hA = c.dram("hA", [T, D], F32)
        self.hB = c.dram("hB", [T, D], F32)
        self.hAT = c.dram("hAT", [D, T], BF16)
        self.hBT = c.dram("hBT", [D, T], BF16)
        self.knT = c.dram("knT", [MH, 128, T], BF16)
        self.krT = c.dram("krT", [64, T], BF16)
        self.v_scr = c.dram("v_scr", [T, 2048], BF16)
        self.qnT = c.dram("qnT", [MH, 128, T], BF16)
        self.qrT = c.dram("qrT", [MH, 64, T], BF16)
        self.oT = c.dram("oT", [2048, T], BF16)
        self.psA_t = nc.alloc_psum_tensor("psA", [128, 2048], F32)
        self.psA = [Tl(self.psA_t[:, i * 512:(i + 1) * 512], Buf(f"psA{i}")) for i in range(4)]
        self.psArot = Rot(self.psA)
        self.ptr = Rot([c.ps([128, 1024], BF16, f"ptr{i}") for i in range(2)])
        self.pm = Rot([c.ps([128, 512], F32, f"pm{i}") for i in range(2)])
        self.final_ops = []

    def Win(self, name):
        if name not in self._W:
            if name == "pos":
                self._W[name] = self.c.inp("pos", [128, T // 128], I32)
            else:
                self._W[name] = self.c.inp(name, self.wshapes[name], F32)
        return self._W[name]

    def phase(self, name):
        return self.phases is None or name in self.phases

    def begin(self):
        self.c.stack = ExitStack()
        self.c.keymap = {}

    def end(self):
        self.c.S.barrier()
        self.c.stack.close()
        self.c.stack = None

    def consts(self):
        c = self.c
        nc = self.nc
        c.stack = ExitStack()
        self.gstack = c.stack
        one_f = c.sb([128, 128], F32, "onef")
        c.I("pool", "memset", w=[one_f], ap=one_f[:], constant=1.0)
        self.ones_f = one_f
        idf = c.sb([128, 128], F32, "idf")
        c.I("pool", "affine_select", r=[one_f], w=[idf], out=idf[:], in_=one_f[:], pattern=[[-1, 128]],
            compare_op=ALU.is_equal, fill=0.0, base=0, channel_multiplier=1)
        idb = c.sb([128, 128], BF16, "idb")
        c.I("dve", "tensor_copy", r=[idf], w=[idb], out=idb[:], in_=idf[:])
        self.idf, self.idb = idf, idb
        uf = c.sb([128, 128], F32, "uf")
        c.I("pool", "affine_select", r=[one_f], w=[uf], out=uf[:], in_=one_f[:], pattern=[[1, 128]],
            compare_op=ALU.is_ge, fill=0.0, base=0, channel_multiplier=-1)
        ub = c.sb([128, 128], BF16, "ub")
        c.I("dve", "tensor_copy", r=[uf], w=[ub], out=ub[:], in_=uf[:])
        self.uf, self.ub = uf, ub
        lsf = c.sb([128, 128], F32, "lsf")
        c.I("pool", "affine_select", r=[one_f], w=[lsf], out=lsf[:], in_=one_f[:], pattern=[[-1, 128]],
            compare_op=ALU.is_gt, fill=0.0, base=0, channel_multiplier=1)
        lsb = c.sb([128, 128], BF16, "lsb")
        c.I("dve", "tensor_copy", r=[lsf], w=[lsb], out=lsb[:], in_=lsf[:])
        self.lsb = lsb
        oneb = c.sb([128, 128], BF16, "oneb")
        c.I("dve", "tensor_copy", r=[one_f], w=[oneb], out=oneb[:], in_=one_f[:])
        self.ones_b = oneb
        zf = c.sb([128, 128], F32, "zf")
        c.I("pool", "memset", w=[zf], ap=zf[:], constant=0.0)
        mbf = c.sb([128, 128], F32, "mbf")
        c.I("pool", "affine_select", r=[zf], w=[mbf], out=mbf[:], in_=zf[:], pattern=[[-1, 128]],
            compare_op=ALU.is_ge, fill=-30000.0, base=0, channel_multiplier=1)
        mbb = c.sb([128, 128], BF16, "mbb")
        c.I("dve", "tensor_copy", r=[mbf], w=[mbb], out=mbb[:], in_=mbf[:])
        self.maskb = mbb

    def bcast_load(self, dram_ap_1d, n, name):
        c = self.c
        t = c.sb([128, n], F32, name)
        c.dma("sp", t[:], dram_ap_1d.partition_broadcast(128), key=name, w=[t])
        return t

    def wload(self, wt, wview, KC, ncols, key, kc_split=1):
        c = self.c
        step = KC // kc_split
        for i in range(kc_split):
            kw = dict(w=[wt]) if i == 0 else dict(pw=[wt])
            c.dma("pool", wt[:, i * step:(i + 1) * step, 0:ncols], wview[:, i * step:(i + 1) * step, :], key=key, **kw)

    def evac(self, k, out_ap, in_ap, r, w=(), pw=()):
        c = self.c
        if k % 2 == 0:
            return c.I("act", "activation", r=r, w=w, pw=pw, out=out_ap, in_=in_ap, func=AF.Copy)
        return c.I("dve", "tensor_copy", r=r, w=w, pw=pw, out=out_ap, in_=in_ap)

    def transpose_to(self, src, src_ap_fn, nblk, dst, dst_ap_fn, first_write=True, rows=128):
        c = self.c
        j = 0
        fw = first_write
        while j < nblk:
            n = min(8, nblk - j)
            pt = self.ptr.next()
            for i in range(n):
                kw = dict(w=[pt]) if i == 0 else dict(pw=[pt])
                c.I("pe", "transpose", r=[src, self.idb], out=pt[0:rows, i * 128:(i + 1) * 128],
                    in_=src_ap_fn(j + i), identity=self.idb[:], **kw)
            self._tk = getattr(self, "_tk", 0) + 1
            kw = dict(w=[dst]) if fw else dict(pw=[dst])
            fw = False
            self.evac(self._tk, dst_ap_fn(j, n), pt[0:rows, 0:n * 128].rearrange("p (a b) -> p a b", b=128),
                      r=[pt], **kw)
            j += n

    def ln_rows(self, r_t, r_ap, g_bc, b_bc, out_f32, out_f32_ap, out_bf, out_bf_ap):
        c = self.c
        st = self.ln_st.next()
        for i in range(4):
            kw = dict(w=[st]) if i == 0 else dict(pw=[st])
            c.I("dve", "bn_stats", r=[r_t], out=st[:, i * 6:(i + 1) * 6], in_=r_ap[:, i * 512:(i + 1) * 512], **kw)
        mv = self.ln_mv.next()
        c.I("dve", "bn_aggr", r=[st], w=[mv], out=mv[:, 0:2], in_=st[:, 0:24])
        c.I("act", "activation", r=[mv], pw=[mv], out=mv[:, 2:3], in_=mv[:, 1:2], func=AF.Sqrt, bias=self.eps_ln[:, 0:1], scale=1.0)
        c.I("dve", "reciprocal", r=[mv], pw=[mv], out=mv[:, 3:4], in_=mv[:, 2:3])
        c.I("dve", "tensor_scalar", r=[r_t, mv], w=[r_t], out=r_ap, in0=r_ap, scalar1=mv[:, 0:1], scalar2=mv[:, 3:4],
            op0=ALU.subtract, op1=ALU.mult)
        c.I("dve", "tensor_tensor", r=[r_t, g_bc], w=[r_t], out=r_ap, in0=r_ap, in1=g_bc[:], op=ALU.mult)
        c.I("dve", "tensor_tensor", r=[r_t, b_bc], w=[out_f32], out=out_f32_ap, in0=r_ap, in1=b_bc[:], op=ALU.add)
        c.I("act", "activation", r=[out_f32], w=[out_bf], out=out_bf_ap, in_=out_f32_ap, func=AF.Copy)

    def ln_setup(self):
        c = self.c
        self.ln_st = c.sbn(2, [128, 24], F32, "lnst")
        self.ln_mv = c.sbn(2, [128, 4], F32, "lnmv")
        self.eps_ln = c.sb([128, 1], F32, "epsln")
        c.I("pool", "memset", w=[self.eps_ln], ap=self.eps_ln[:], constant=LN_EPS)

    def p1(self):
        c = self.c
        W = self.Win
        self.begin()
        xt = c.sb([128, 16, SEQ], BF16, "xt")
        xin = c.sbn(2, [128, D], F32, "xin")
        xbf = c.sbn(2, [128, D], BF16, "xbf")
        wts = c.sbn(2, [128, 16, 512], BF16, "w")
        zst = c.sbn(3, [128, 512], F32, "zst")
        U = c.sbn(2, [128, 3 + SEQ], F32, "U")
        acc = c.sbn(1, [128, SEQ], F32, "cacc")
        vb = c.sbn(2, [128, SEQ], BF16, "vb")
        tokst = c.sbn(1, [128, 16, 512], BF16, "tokst")
        cw = c.sb([128, 48, 4], F32, "cw")
        cb = c.sb([128, 48], F32, "cb")
        c.dma("sp", cw[:], W("conv_w")[:], key="cw", w=[cw])
        c.dma("sp", cb[:], W("conv_b")[:], key="cb", w=[cb])
        dtb = self.bcast_load(W("ssm_dt_bias").t, NH, "dtb")
        dts = c.sbn(2, [128, 4, NH], F32, "dts")
        for u in U.tiles:
            c.I("pool", "memset", w=[u], ap=u[:, 0:3], constant=0.0)
        win = W("ssm_w_in").t.rearrange("(kc p) n -> p kc n", p=128)
        k = 0
        for s in range(NSEQ):
            t0 = s * SEQ
            for tt in range(16):
                xi = xin.next()
                xb = xbf.next()
                c.dma("sp", xi[:], W("x")[t0 + tt * 128:t0 + (tt + 1) * 128, :], key=xi.b.name, w=[xi])
                c.I("act", "activation", r=[xi], w=[xb], out=xb[:], in_=xi[:], func=AF.Copy)
                self.transpose_to(xb, lambda j, xb=xb: xb[:, j * 128:(j + 1) * 128], 16, xt,
                                  lambda j0, n, tt=tt: xt[:, j0:j0 + n, tt * 128:(tt + 1) * 128],
                                  first_write=(tt == 0))
            for cg in range(21):
                ncols = 512 if cg < 20 else 64
                wt = wts.next()
                self.wload(wt, win[:, :, cg * 512:cg * 512 + ncols], 16, ncols, key=wt.b.name, kc_split=2)
                if cg < 8 or cg == 20:
                    for tt in range(16):
                        ps = self.psArot.next()
                        for kc in range(16):
                            kw = dict(w=[ps]) if kc == 0 else dict(pw=[ps])
                            c.I("pe", "matmul", r=[xt, wt], out=ps[:, 0:ncols], lhsT=xt[:, kc, tt * 128:(tt + 1) * 128],
                                rhs=wt[:, kc, 0:ncols], start=(kc == 0), stop=(kc == 15), **kw)
                        if cg < 8:
                            zs = zst.next()
                            k += 1
                            self.evac(k, zs[:], ps[:], r=[ps], w=[zs])
                            c.dma("sp", self.z_scr[t0 + tt * 128:t0 + (tt + 1) * 128, cg * 512:(cg + 1) * 512], zs[:],
                                  key=zs.b.name, r=[zs], pw=[self.z_scr])
                        else:
                            if tt % 4 == 0:
                                ds = dts.next()
                            j = tt % 4
                            kw = dict(w=[ds]) if j == 0 else dict(pw=[ds])
                            a0 = ds[:, j, :]
                            x0 = zst.next()
                            c.I("dve", "tensor_tensor", r=[ps, dtb], w=[x0], out=x0[:, 0:64], in0=ps[:, 0:64], in1=dtb[:], op=ALU.add)
                            c.I("act", "activation", r=[x0], pw=[x0], out=x0[:, 64:128], in_=x0[:, 0:64], func=AF.Abs)
                            c.I("act", "activation", r=[x0], pw=[x0], out=x0[:, 128:192], in_=x0[:, 64:128], func=AF.Exp, scale=-1.0)
                            c.I("act", "activation", r=[x0], pw=[x0], out=x0[:, 192:256], in_=x0[:, 128:192], func=AF.Ln, bias=self.one_col[:, 0:1], scale=1.0)
                            c.I("act", "activation", r=[x0], pw=[x0], out=x0[:, 256:320], in_=x0[:, 0:64], func=AF.Relu)
                            c.I("dve", "tensor_tensor", r=[x0], out=a0, in0=x0[:, 256:320], in1=x0[:, 192:256], op=ALU.add, **kw)
                            if j == 3:
                                g4 = tt // 4
                                dv = self.dt_scr.t[t0 + g4 * 512:t0 + (g4 + 1) * 512, :].rearrange("(n p) h -> p n h", p=128)
                                c.dma("sp", dv, ds[:], key=ds.b.name, r=[ds], pw=[self.dt_scr])
                else:
                    ts_ = tokst.next() if cg < 18 else None
                    for j in range(4):
                        ch = (cg - 8) * 4 + j
                        u = U.next()
                        for sub in range(4):
                            ps = self.psArot.next()
                            for kc in range(16):
                                kw = dict(w=[ps]) if kc == 0 else dict(pw=[ps])
                                c.I("pe", "matmul", r=[xt, wt], out=ps[:], lhsT=wt[:, kc, j * 128:(j + 1) * 128],
                                    rhs=xt[:, kc, sub * 512:(sub + 1) * 512], start=(kc == 0), stop=(kc == 15), **kw)
                            k += 1
                            self.evac(k, u[:, 3 + sub * 512:3 + (sub + 1) * 512], ps[:], r=[ps], pw=[u])
                        a = acc.next()
                        c.I("dve", "tensor_scalar", r=[u, cw, cb], w=[a], out=a[:], in0=u[:, 0:SEQ], scalar1=cw[:, ch, 0:1],
                            scalar2=cb[:, ch:ch + 1], op0=ALU.mult, op1=ALU.add)
                        for kk in range(1, 4):
                            c.I("dve", "scalar_tensor_tensor", r=[u, cw, a], w=[a], out=a[:], in0=u[:, kk:kk + SEQ],
                                scalar=cw[:, ch, kk:kk + 1], in1=a[:], op0=ALU.mult, op1=ALU.add)
                        v = vb.next()
                        c.I("act", "activation", r=[a], w=[v], out=v[:], in_=a[:], func=AF.Silu)
                        c.dma("sp", self.xbcT[ch, :, t0:t0 + SEQ], v[:], key=v.b.name, r=[v], pw=[self.xbcT])
                        if cg < 18:
                            self.transpose_to(v, lambda jj, v=v: v[:, jj * 128:(jj + 1) * 128], 16, ts_,
                                              lambda j0, n, j=j: ts_[:, j0:j0 + n, j * 128:(j + 1) * 128],
                                              first_write=(j == 0))
                    if cg < 18:
                        col0 = (cg - 8) * 512
                        dv = self.xbc_tok.t[t0:t0 + SEQ, col0:col0 + 512].rearrange("(n p) f -> p n f", p=128)
                        c.dma("sp", dv, ts_[:], key=ts_.b.name, r=[ts_], pw=[self.xbc_tok])
        self.end()

    def p2(self):
        c = self.c
        W = self.Win
        self.begin()
        alog = self.bcast_load(W("ssm_a_log").t, NH, "alog")
        A_bc = c.sb([128, NH], F32, "Abc")
        c.I("act", "activation", r=[alog], w=[A_bc], out=A_bc[:], in_=alog[:], func=AF.Exp)
        c.I("dve", "tensor_scalar", r=[A_bc], w=[A_bc], out=A_bc[:], in0=A_bc[:], scalar1=-1.0, scalar2=None, op0=ALU.mult)
        D_bc = self.bcast_load(W("ssm_d").t, NH, "Dbc")
        gn_bc = self.bcast_load(W("ssm_norm_g").t, DIN, "gnbc")
        eps_r = c.sb([128, 1], F32, "epsr")
        c.I("pool", "memset", w=[eps_r], ap=eps_r[:], constant=RMS_EPS)
        state_f = c.sb([128, DIN], F32, "statef")
        state_b = c.sb([128, DIN], BF16, "stateb")
        xtoks = c.sbn(2, [128, 5120], BF16, "xtok")
        BT = c.sbn(2, [128, 8, 128], BF16, "BT")
        CT = c.sbn(2, [128, 8, 128], BF16, "CT")
        dtt = c.sbn(2, [128, NH], F32, "dtt")
        zts = c.sbn(1, [128, DIN], F32, "zt")
        small = c.sbn(2, [128, 8 * NH], F32, "small")
        arhs = c.sb([128, NH, 128], BF16, "arhs")
        Lm = c.sbn(2, [128, 4, 128], BF16, "Lm")
        MT = c.sbn(2, [128, 8, 128], BF16, "MT")
        CBm = c.sb([128, 8, 128], BF16, "CBm")
        xdt = c.sb([128, DIN], BF16, "xdt")
        xdte = c.sb([128, DIN], BF16, "xdte")
        yt = c.sb([128, DIN], F32, "yt")
        tmp = c.sbn(3, [128, 512], F32, "tmp")
        ynb = c.sb([128, DIN], BF16, "ynb")
        ynst = c.sb([128, 32, 512], BF16, "ynst")
        ss = c.sbn(2, [128, 16], F32, "ss")
        v3 = lambda ap, d: ap.rearrange("p (h d) -> p h d", d=d)
        for s in range(NSEQ):
            for ci in range(16):
                tc0 = s * SEQ + ci * 128
                xtok = xtoks.next()
                zt = zts.next()
                c.dma("sp", xtok[:], self.xbc_tok[tc0:tc0 + 128, :], key=xtok.b.name, w=[xtok], r=[self.xbc_tok])
                bt = BT.next()
                ct = CT.next()
                c.dma("sp", bt[:], self.xbcT[32:40, :, tc0:tc0 + 128].rearrange("g n t -> n g t"), key=bt.b.name, w=[bt], r=[self.xbcT])
                c.dma("sp", ct[:], self.xbcT[40:48, :, tc0:tc0 + 128].rearrange("g n t -> n g t"), key=ct.b.name, w=[ct], r=[self.xbcT])
                dt_ = dtt.next()
                c.dma("sp", dt_[:], self.dt_scr[tc0:tc0 + 128, :], key=dt_.b.name, w=[dt_], r=[self.dt_scr])
                c.dma("sp", zt[:], self.z_scr[tc0:tc0 + 128, :], key=zt.b.name, w=[zt], r=[self.z_scr])
                sm = small.next()
                a = sm[:, 0:64]; E = sm[:, 64:128]; acs = sm[:, 128:192]; dec = sm[:, 192:256]; toe = sm[:, 256:320]; tdf = sm[:, 320:384]
                c.I("dve", "tensor_tensor", r=[dt_, A_bc], w=[sm], out=a, in0=dt_[:], in1=A_bc[:], op=ALU.mult)
                pm = self.pm.next()
                c.I("pe", "matmul", r=[self.uf, sm], w=[pm], out=pm[:, 0:64], lhsT=self.uf[:], rhs=a, start=True, stop=True)
                c.I("pe", "matmul", r=[self.ones_f, sm], pw=[pm], out=pm[:, 64:128], lhsT=self.ones_f[:], rhs=a, start=True, stop=True)
                c.I("act", "activation", r=[pm], pw=[sm], out=E, in_=pm[:, 0:64], func=AF.Exp)
                c.I("act", "activation", r=[pm], pw=[sm], out=dec, in_=pm[:, 64:128], func=AF.Exp)
                c.I("act", "activation", r=[pm], pw=[sm], out=acs, in_=pm[:, 0:64], func=AF.Copy)
                c.I("dve", "tensor_tensor", r=[pm, sm], pw=[sm], out=tdf, in0=pm[:, 64:128], in1=acs, op=ALU.subtract)
                c.I("act", "activation", r=[sm], pw=[sm], out=toe, in_=tdf, func=AF.Exp)
                c.I("pool", "tensor_tensor", r=[sm, self.ub], w=[arhs], out=arhs[:], in0=bc(a.unsqueeze(2), [128, NH, 128]),
                    in1=bc(self.ub[:].unsqueeze(1), [128, NH, 128]), op=ALU.mult)
                c.I("pool", "tensor_tensor", r=[xtok, dt_], w=[xdt], out=v3(xdt[:], 64), in0=v3(xtok[:, 0:DIN], 64),
                    in1=bc(dt_[:].unsqueeze(2), [128, NH, 64]), op=ALU.mult)
                c.I("pool", "tensor_tensor", r=[xdt, sm], w=[xdte], out=v3(xdte[:], 64), in0=v3(xdt[:], 64),
                    in1=bc(toe.unsqueeze(2), [128, NH, 64]), op=ALU.mult)
                for g4 in range(2):
                    ps = self.psArot.next()
                    for gg in range(4):
                        g = g4 * 4 + gg
                        kw = dict(w=[ps]) if gg == 0 else dict(pw=[ps])
                        c.I("pe", "matmul", r=[bt, ct], out=ps[:, gg * 128:(gg + 1) * 128], lhsT=bt[:, g, :], rhs=ct[:, g, :],
                            start=True, stop=True, **kw)
                    kw = dict(w=[CBm]) if g4 == 0 else dict(pw=[CBm])
                    c.I("dve", "tensor_tensor", r=[ps, self.uf], out=CBm[:, g4 * 4:(g4 + 1) * 4, :], in0=v3(ps[:], 128),
                        in1=bc(self.uf[:].unsqueeze(1), [128, 4, 128]), op=ALU.mult, **kw)
                for hq in range(16):
                    g = hq // 2
                    ps = self.psArot.next()
                    c.I("pe", "matmul", r=[self.lsb, arhs], w=[ps], out=ps[:], lhsT=self.lsb[:],
                        rhs=arhs[:, hq * 4:(hq + 1) * 4, :].rearrange("p a b -> p (a b)"), start=True, stop=True)
                    lm = Lm.next()
                    c.I("act", "activation", r=[ps], w=[lm], out=lm[:], in_=v3(ps[:], 128), func=AF.Exp)
                    if hq % 2 == 0:
                        mt = MT.next()
                    kw = dict(w=[mt]) if hq % 2 == 0 else dict(pw=[mt])
                    c.I("dve", "tensor_tensor", r=[lm, CBm], out=mt[:, (hq % 2) * 4:(hq % 2) * 4 + 4, :], in0=lm[:],
                        in1=bc(CBm[:, g, :].unsqueeze(1), [128, 4, 128]), op=ALU.mult, **kw)
                    if hq % 2 == 1:
                        gs = slice(g * 512, (g + 1) * 512)
                        psy = self.psArot.next()
                        for hh in range(8):
                            h = g * 8 + hh
                            kw = dict(w=[psy]) if hh == 0 else dict(pw=[psy])
                            c.I("pe", "matmul", r=[mt, xdt], out=psy[:, hh * 64:(hh + 1) * 64], lhsT=mt[:, hh, :],
                                rhs=xdt[:, h * 64:(h + 1) * 64], start=True, stop=True, **kw)
                        ykw = dict(w=[yt]) if g == 0 else dict(pw=[yt])
                        if ci > 0:
                            psi = self.psArot.next()
                            c.I("pe", "matmul", r=[ct, state_b], w=[psi], out=psi[:], lhsT=ct[:, g, :], rhs=state_b[:, gs],
                                start=True, stop=True)
                            t1 = tmp.next()
                            c.I("dve", "tensor_tensor", r=[psi, sm], w=[t1], out=v3(t1[:], 64), in0=v3(psi[:], 64),
                                in1=bc(E[:, g * 8:(g + 1) * 8].unsqueeze(2), [128, 8, 64]), op=ALU.mult)
                            c.I("dve", "tensor_tensor", r=[psy, t1], out=yt[:, gs], in0=psy[:], in1=t1[:], op=ALU.add, **ykw)
                        else:
                            c.I("dve", "tensor_copy", r=[psy], out=yt[:, gs], in_=psy[:], **ykw)
                        t2 = tmp.next()
                        c.I("pool", "tensor_tensor", r=[xtok, D_bc], w=[t2], out=v3(t2[:], 64), in0=v3(xtok[:, gs], 64),
                            in1=bc(D_bc[:, g * 8:(g + 1) * 8].unsqueeze(2), [128, 8, 64]), op=ALU.mult)
                        c.I("dve", "tensor_tensor", r=[yt, t2], pw=[yt], out=yt[:, gs], in0=yt[:, gs], in1=t2[:], op=ALU.add)
                c.I("act", "activation", r=[zt], w=[zt], out=zt[:], in_=zt[:], func=AF.Silu)
                c.I("dve", "tensor_tensor", r=[yt, zt], w=[yt], out=yt[:], in0=yt[:], in1=zt[:], op=ALU.mult)
                s_ = ss.next()
                for g in range(8):
                    t1 = tmp.next()
                    kw = dict(w=[s_, t1]) if g == 0 else dict(w=[t1], pw=[s_])
                    c.I("act", "activation", r=[yt], out=t1[:], in_=yt[:, g * 512:(g + 1) * 512], func=AF.Square,
                        accum_out=s_[:, g:g + 1], **kw)
                c.I("act", "activation", r=[s_, eps_r], pw=[s_], out=s_[:, 8:16], in_=s_[:, 0:8], func=AF.Sqrt,
                    bias=eps_r[:, 0:1], scale=1.0 / 512)
                c.I("dve", "reciprocal", r=[s_], w=[s_], out=s_[:, 0:8], in_=s_[:, 8:16])
                c.I("dve", "tensor_tensor", r=[yt, s_], w=[yt], out=v3(yt[:], 512), in0=v3(yt[:], 512),
                    in1=bc(s_[:, 0:8].unsqueeze(2), [128, 8, 512]), op=ALU.mult)
                c.I("dve", "tensor_tensor", r=[yt, gn_bc], w=[ynb], out=ynb[:], in0=yt[:], in1=gn_bc[:], op=ALU.mult)
                q = ci % 4
                self.transpose_to(ynb, lambda j: ynb[:, j * 128:(j + 1) * 128], 32, ynst,
                                  lambda j0, n, q=q: ynst[:, j0:j0 + n, q * 128:(q + 1) * 128], first_write=(q == 0))
                if q == 3:
                    tb0 = s * SEQ + (ci // 4) * 512
                    c.dma("sp", self.ynT[:, tb0:tb0 + 512].rearrange("(c p) t -> p c t", p=128), ynst[:], key="st_ynst",
                          r=[ynst], pw=[self.ynT])
                if ci < 15:
                    for g in range(8):
                        gs = slice(g * 512, (g + 1) * 512)
                        psn = self.psArot.next()
                        c.I("pe", "matmul", r=[xtok, xdte], w=[psn], out=psn[:], lhsT=xtok[:, DIN + g * 128:DIN + (g + 1) * 128],
                            rhs=xdte[:, gs], start=True, stop=True)
                        skw = dict(w=[state_f]) if g == 0 else dict(pw=[state_f])
                        if ci == 0:
                            c.I("dve", "tensor_copy", r=[psn], out=state_f[:, gs], in_=psn[:], **skw)
                        else:
                            t3 = tmp.next()
                            c.I("dve", "tensor_tensor", r=[state_f, sm], w=[t3], out=v3(t3[:], 64), in0=v3(state_f[:, gs], 64),
                                in1=bc(dec[:, g * 8:(g + 1) * 8].unsqueeze(2), [128, 8, 64]), op=ALU.mult)
                            c.I("dve", "tensor_tensor", r=[t3, psn], out=state_f[:, gs], in0=t3[:], in1=psn[:], op=ALU.add, **skw)
                        bkw = dict(w=[state_b]) if g == 0 else dict(pw=[state_b])
                        c.I("act", "activation", r=[state_f], out=state_b[:, gs], in_=state_f[:, gs], func=AF.Copy, **bkw)
        self.end()

    def linear_ln(self, xT, KC, wview, resid, g_ap, b_ap, out_tok, out_T, TB=512, NCOL=256):
        c = self.c
        self.begin()
        g_bc = self.bcast_load(g_ap, D, "lng")
        b_bc = self.bcast_load(b_ap, D, "lnb")
        xts = c.sbn(2, [128, KC, TB], BF16, "xt")
        wts = c.sbn(2, [128, KC, NCOL], BF16, "w")
        r = c.sb([128, TB // 128, D], F32, "r")
        res = c.sbn(2, [128, D], F32, "res")
        hf = c.sbn(2, [128, D], F32, "hf")
        hb = c.sbn(2, [128, D], BF16, "hb")
        stT = c.sb([128, 16, TB], BF16, "stT")
        wv = wview.rearrange("(kc p) n -> p kc n", p=128)
        k = 0
        nt = TB // 128
        for blk in range(T // TB):
            tb0 = blk * TB
            xt = xts.next()
            c.dma("sp", xt[:], xT[:, tb0:tb0 + TB].rearrange("(kc p) t -> p kc t", p=128), key=xt.b.name, w=[xt], r=[xT])
            for cg in range(D // NCOL):
                wt = wts.next()
                self.wload(wt, wv[:, :, cg * NCOL:(cg + 1) * NCOL], KC, NCOL, key=wt.b.name, kc_split=2)
                for tt in range(nt):
                    ps = self.psArot.next()
                    for kc in range(KC):
                        kw = dict(w=[ps]) if kc == 0 else dict(pw=[ps])
                        c.I("pe", "matmul", r=[xt, wt], out=ps[:, 0:NCOL], lhsT=xt[:, kc, tt * 128:(tt + 1) * 128],
                            rhs=wt[:, kc, :], start=(kc == 0), stop=(kc == KC - 1), **kw)
                    k += 1
                    kw = dict(w=[r]) if (cg == 0 and tt == 0) else dict(pw=[r])
                    self.evac(k, r[:, tt, cg * NCOL:(cg + 1) * NCOL], ps[:, 0:NCOL], r=[ps], **kw)
            for tt in range(nt):
                t0 = tb0 + tt * 128
                rs = res.next()
                c.dma("sp", rs[:], resid[t0:t0 + 128, :], key=rs.b.name, w=[rs], r=[resid])
                c.I("dve", "scalar_tensor_tensor", r=[rs, r], pw=[r], out=r[:, tt, :], in0=rs[:], scalar=ALPHA, in1=r[:, tt, :],
                    op0=ALU.mult, op1=ALU.add)
                f = hf.next()
                b = hb.next()
                self.ln_rows(r, r[:, tt, :], g_bc, b_bc, f, f[:], b, b[:])
                c.dma("sp", out_tok[t0:t0 + 128, :], f[:], key="st_" + f.b.name, r=[f], pw=[out_tok])
                self.transpose_to(b, lambda j, b=b: b[:, j * 128:(j + 1) * 128], 16, stT,
                                  lambda j0, n, tt=tt: stT[:, j0:j0 + n, tt * 128:(tt + 1) * 128], first_write=(tt == 0))
            c.dma("sp", out_T[:, tb0:tb0 + TB].rearrange("(c p) t -> p c t", p=128), stT[:], key="st_stT", r=[stT], pw=[out_T])
        self.end()

    def p3(self):
        W = self.Win
        self.linear_ln(self.ynT, 32, W("ssm_w_out").t, W("x"), W("ln1_g").t[0], W("ln1_b").t[0], self.hA, self.hAT)

    def ffn(self, xT, resid, units, g_ap, b_ap, out_tok, out_T, moe=False):
        c = self.c
        W = self.Win
        self.begin()
        TB = 512
        nt = TB // 128
        g_bc = self.bcast_load(g_ap, D, "lng")
        b_bc = self.bcast_load(b_ap, D, "lnb")
        xts = c.sbn(2, [128, 16, TB], BF16, "xt")
        wgs = c.sbn(2, [128, 16, 128], BF16, "wg")
        wus = c.sbn(2, [128, 16, 128], BF16, "wu")
        wds = c.sbn(2, [128, 11, 512], BF16, "wd")
        actT = c.sb([128, 11, TB], BF16, "actT")
        acc = c.sb([128, nt, D], F32, "acc")
        sg = c.sbn(2, [128, 512], F32, "sg")
        res = c.sbn(2, [128, D], F32, "res")
        hb = c.sbn(1, [128, D], BF16, "hb")
        stT = c.sb([128, 16, TB], BF16, "stT")
        if moe:
            wr = c.sb([128, 16, NE], F32, "wr")
            c.dma("sp", wr[:], W("moe_w_router").t.rearrange("(kc p) e -> p kc e", p=128), key="wr", w=[wr])
            br = self.bcast_load(W("moe_b_router").t, NE, "br")
            hTf = c.sb([128, 16, 128], F32, "hTf")
            comb = c.sb([128, nt, NE], F32, "comb")
            rt = c.sbn(2, [128, 64], F32, "rt")
        k = 0
        for blk in range(T // TB):
            tb0 = blk * TB
            xt = xts.next()
            c.dma("sp", xt[:], xT[:, tb0:tb0 + TB].rearrange("(kc p) t -> p kc t", p=128), key=xt.b.name, w=[xt], r=[xT])
            if moe:
                for tt in range(nt):
                    t0 = tb0 + tt * 128
                    rs = res.next()
                    c.dma("sp", rs[:], resid[t0:t0 + 128, :], key=rs.b.name, w=[rs], r=[resid])
                    for q in range(4):
                        pm = self.pm.next()
                        for i in range(4):
                            kc = q * 4 + i
                            kw = dict(w=[pm]) if i == 0 else dict(pw=[pm])
                            c.I("pe", "transpose", r=[rs, self.idf], out=pm[:, i * 128:(i + 1) * 128],
                                in_=rs[:, kc * 128:(kc + 1) * 128], identity=self.idf[:], **kw)
                        k += 1
                        kw = dict(w=[hTf]) if q == 0 else dict(pw=[hTf])
                        self.evac(k, hTf[:, q * 4:(q + 1) * 4, :], pm[:].rearrange("p (a b) -> p a b", b=128), r=[pm], **kw)
                    pm = self.pm.next()
                    for kc in range(16):
                        kw = dict(w=[pm]) if kc == 0 else dict(pw=[pm])
                        c.I("pe", "matmul", r=[hTf, wr], out=pm[:, 0:NE], lhsT=hTf[:, kc, :], rhs=wr[:, kc, :],
                            start=(kc == 0), stop=(kc == 15), **kw)
                    r_ = rt.next()
                    lg = r_[:, 0:8]; oh1 = r_[:, 8:16]; l2 = r_[:, 16:24]; oh2 = r_[:, 24:32]
                    m1 = r_[:, 32:33]; m2 = r_[:, 33:34]; dd = r_[:, 34:35]; ee = r_[:, 35:36]; e1 = r_[:, 36:37]
                    w1 = r_[:, 37:38]; w2 = r_[:, 38:39]
                    c.I("dve", "tensor_tensor", r=[pm, br], w=[r_], out=lg, in0=pm[:, 0:NE], in1=br[:], op=ALU.add)
                    c.I("dve", "reduce_max", r=[r_], pw=[r_], out=m1, in_=lg, axis=AX.X)
                    c.I("dve", "tensor_scalar", r=[r_], pw=[r_], out=oh1, in0=lg, scalar1=m1, scalar2=None, op0=ALU.is_equal)
                    c.I("dve", "scalar_tensor_tensor", r=[r_], pw=[r_], out=l2, in0=oh1, scalar=-1.0e30, in1=lg, op0=ALU.mult, op1=ALU.add)
                    c.I("dve", "reduce_max", r=[r_], pw=[r_], out=m2, in_=l2, axis=AX.X)
                    c.I("dve", "tensor_scalar", r=[r_], pw=[r_], out=oh2, in0=l2, scalar1=m2, scalar2=None, op0=ALU.is_equal)
                    c.I("dve", "tensor_tensor", r=[r_], pw=[r_], out=dd, in0=m2, in1=m1, op=ALU.subtract)
                    c.I("act", "activation", r=[r_], pw=[r_], out=ee, in_=dd, func=AF.Exp)
                    c.I("dve", "tensor_scalar", r=[r_], pw=[r_], out=e1, in0=ee, scalar1=1.0, scalar2=None, op0=ALU.add)
                    c.I("dve", "reciprocal", r=[r_], pw=[r_], out=w1, in_=e1)
                    c.I("dve", "tensor_tensor", r=[r_], pw=[r_], out=w2, in0=ee, in1=w1, op=ALU.mult)
                    kw = dict(w=[comb]) if tt == 0 else dict(pw=[comb])
                    c.I("dve", "tensor_scalar", r=[r_], out=comb[:, tt, :], in0=oh1, scalar1=w1, scalar2=None, op0=ALU.mult, **kw)
                    c.I("dve", "scalar_tensor_tensor", r=[r_, comb], pw=[comb], out=comb[:, tt, :], in0=oh2, scalar=w2,
                        in1=comb[:, tt, :], op0=ALU.mult, op1=ALU.add)
            for u, (wgv, wuv, wdv, e) in enumerate(units):
                wd3 = wdv.rearrange("(fc p) d -> p fc d", p=128)
                for fc in range(11):
                    wg = wgs.next()
                    wu = wus.next()
                    c.dma("pool", wg[:].rearrange("p a b -> p (a b)"), wgv[fc * 128:(fc + 1) * 128, :], key=wg.b.name, w=[wg])
                    c.dma("pool", wu[:].rearrange("p a b -> p (a b)"), wuv[fc * 128:(fc + 1) * 128, :], key=wu.b.name, w=[wu])
                    for half in range(TB // 512):
                        hs = slice(half * 512, (half + 1) * 512)
                        psg = self.psArot.next()
                        for kc in range(16):
                            kw = dict(w=[psg]) if kc == 0 else dict(pw=[psg])
                            c.I("pe", "matmul", r=[xt, wg], out=psg[:], lhsT=wg[:, kc, :], rhs=xt[:, kc, hs],
                                start=(kc == 0), stop=(kc == 15), **kw)
                        psu = self.psArot.next()
                        for kc in range(16):
                            kw = dict(w=[psu]) if kc == 0 else dict(pw=[psu])
                            c.I("pe", "matmul", r=[xt, wu], out=psu[:], lhsT=wu[:, kc, :], rhs=xt[:, kc, hs],
                                start=(kc == 0), stop=(kc == 15), **kw)
                        s1 = sg.next()
                        c.I("act", "activation", r=[psg], w=[s1], out=s1[:], in_=psg[:], func=AF.Silu)
                        kw = dict(w=[actT]) if (fc == 0 and half == 0) else dict(pw=[actT])
                        c.I("dve", "tensor_tensor", r=[s1, psu], out=actT[:, fc, hs], in0=s1[:], in1=psu[:], op=ALU.mult, **kw)
                for dblk in range(4):
                    ds_ = slice(dblk * 512, (dblk + 1) * 512)
                    wd = wds.next()
                    self.wload(wd, wd3[:, :, ds_], 11, 512, key=wd.b.name)
                    for tt in range(nt):
                        ps = self.psArot.next()
                        for fc in range(11):
                            kw = dict(w=[ps]) if fc == 0 else dict(pw=[ps])
                            c.I("pe", "matmul", r=[actT, wd], out=ps[:], lhsT=actT[:, fc, tt * 128:(tt + 1) * 128],
                                rhs=wd[:, fc, :], start=(fc == 0), stop=(fc == 10), **kw)
                        first = (u == 0)
                        akw = dict(w=[acc]) if (first and dblk == 0 and tt == 0) else dict(pw=[acc])
                        if moe:
                            sc = comb[:, tt, e:e + 1]
                            rr = [ps, comb]
                        else:
                            sc = 1.0
                            rr = [ps]
                        if first:
                            c.I("dve", "tensor_scalar", r=rr, out=acc[:, tt, ds_], in0=ps[:], scalar1=sc, scalar2=None,
                                op0=ALU.mult, **akw)
                        else:
                            c.I("dve", "scalar_tensor_tensor", r=rr + [acc], out=acc[:, tt, ds_], in0=ps[:], scalar=sc,
                                in1=acc[:, tt, ds_], op0=ALU.mult, op1=ALU.add, **akw)
            for tt in range(nt):
                t0 = tb0 + tt * 128
                rs = res.next()
                c.dma("sp", rs[:], resid[t0:t0 + 128, :], key=rs.b.name, w=[rs], r=[resid])
                c.I("dve", "scalar_tensor_tensor", r=[rs, acc], pw=[acc], out=acc[:, tt, :], in0=rs[:], scalar=ALPHA,
                    in1=acc[:, tt, :], op0=ALU.mult, op1=ALU.add)
                b = hb.next()
                self.ln_rows(acc, acc[:, tt, :], g_bc, b_bc, acc, acc[:, tt, :], b, b[:])
                c.dma("sp", out_tok[t0:t0 + 128, :], acc[:, tt, :], key="st_acc", r=[acc], pw=[out_tok])
                self.transpose_to(b, lambda j, b=b: b[:, j * 128:(j + 1) * 128], 16, stT,
                                  lambda j0, n, tt=tt: stT[:, j0:j0 + n, tt * 128:(tt + 1) * 128], first_write=(tt == 0))
            c.dma("sp", out_T[:, tb0:tb0 + TB].rearrange("(c p) t -> p c t", p=128), stT[:], key="st_stT", r=[stT], pw=[out_T])
        self.end()

    def p4(self):
        W = self.Win
        wg, wu, wd = W("ffn_w_gate").t, W("ffn_w_up").t, W("ffn_w_down").t
        units = []
        for u in range(4):
            fs = slice(u * 1408, (u + 1) * 1408)
            units.append((wg[fs, :], wu[fs, :], wd[fs, :], None))
        self.ffn(self.hAT, self.hA, units, W("ln2_g").t[0], W("ln2_b").t[0], self.hB, self.hBT)

    def p9_dense_unused(self):
        W = self.Win
        wg, wu, wd = W("moe_w_gate").t, W("moe_w_up").t, W("moe_w_down").t
        units = []
        for e in range(NE):
            for hf in range(2):
                fs = slice(hf * 1408, (hf + 1) * 1408)
                units.append((wg[e][:, fs], wu[e][:, fs], wd[e][fs, :], e))
        self.ffn(self.hBT, self.hB, units, W("ln2_g").t[1], W("ln2_b").t[1], self.hA, self.hAT, moe=True)

    def ple(self, layer, h_tok, hT, out_tok, out_T):
        c = self.c
        W = self.Win
        self.begin()
        TB = 1024
        nt = TB // 128
        xt = c.sb([128, 16, TB], BF16, "xt")
        pT = c.sb([128, 2, TB], BF16, "pT")
        pin = c.sbn(2, [128, PLE], F32, "pin")
        pbf = c.sbn(2, [128, PLE], BF16, "pbf")
        wgs = c.sbn(2, [128, 16, 512], BF16, "wg")
        wps = c.sbn(2, [128, 2, 512], BF16, "wp")
        hres = c.sb([128, nt, D], F32, "hres")
        sg = c.sbn(2, [128, 512], F32, "sg")
        hb = c.sbn(1, [128, D], BF16, "hb")
        stT = c.sb([128, 16, 512], BF16, "stT")
        wgv = W("ple_w_gate").t[layer].rearrange("(kc p) n -> p kc n", p=128)
        wpv = W("ple_w_proj").t[layer].rearrange("(kc p) n -> p kc n", p=128)
        pv = W("p").t[layer]
        for blk in range(T // TB):
            tb0 = blk * TB
            c.dma("sp", xt[:], hT[:, tb0:tb0 + TB].rearrange("(kc p) t -> p kc t", p=128), key="xt", w=[xt], r=[hT])
            c.dma("sp", hres[:], h_tok[tb0:tb0 + TB, :].rearrange("(n p) d -> p n d", p=128), key="hres", w=[hres], r=[h_tok])
            for tt in range(nt):
                pi = pin.next()
                pb = pbf.next()
                c.dma("sp", pi[:], pv[tb0 + tt * 128:tb0 + (tt + 1) * 128, :], key=pi.b.name, w=[pi])
                c.I("act", "activation", r=[pi], w=[pb], out=pb[:], in_=pi[:], func=AF.Copy)
                self.transpose_to(pb, lambda j, pb=pb: pb[:, j * 128:(j + 1) * 128], 2, pT,
                                  lambda j0, n, tt=tt: pT[:, j0:j0 + n, tt * 128:(tt + 1) * 128], first_write=(tt == 0))
            for cg in range(4):
                cs = slice(cg * 512, (cg + 1) * 512)
                wg = wgs.next()
                wp = wps.next()
                self.wload(wg, wgv[:, :, cs], 16, 512, key=wg.b.name, kc_split=2)
                self.wload(wp, wpv[:, :, cs], 2, 512, key=wp.b.name)
                for tt in range(nt):
                    ts_ = slice(tt * 128, (tt + 1) * 128)
                    ps1 = self.psArot.next()
                    for kc in range(16):
                        kw = dict(w=[ps1]) if kc == 0 else dict(pw=[ps1])
                        c.I("pe", "matmul", r=[xt, wg], out=ps1[:], lhsT=xt[:, kc, ts_], rhs=wg[:, kc, :],
                            start=(kc == 0), stop=(kc == 15), **kw)
                    ps2 = self.psArot.next()
                    for kc in range(2):
                        kw = dict(w=[ps2]) if kc == 0 else dict(pw=[ps2])
                        c.I("pe", "matmul", r=[pT, wp], out=ps2[:], lhsT=pT[:, kc, ts_], rhs=wp[:, kc, :],
                            start=(kc == 0), stop=(kc == 1), **kw)
                    s1 = sg.next()
                    c.I("act", "activation", r=[ps1], w=[s1], out=s1[:], in_=ps1[:], func=AF.Sigmoid)
                    c.I("dve", "tensor_tensor", r=[s1, ps2], w=[s1], out=s1[:], in0=s1[:], in1=ps2[:], op=ALU.mult)
                    c.I("dve", "tensor_tensor", r=[s1, hres], pw=[hres], out=hres[:, tt, cs], in0=hres[:, tt, cs], in1=s1[:], op=ALU.add)
            for tt in range(nt):
                t0 = tb0 + tt * 128
                st = c.dma("sp", out_tok[t0:t0 + 128, :], hres[:, tt, :], key="st_hres", r=[hres], pw=[out_tok])
                if out_T is not None:
                    b = hb.next()
                    q = tt % 4
                    c.I("act", "activation", r=[hres], w=[b], out=b[:], in_=hres[:, tt, :], func=AF.Copy)
                    self.transpose_to(b, lambda j, b=b: b[:, j * 128:(j + 1) * 128], 16, stT,
                                      lambda j0, n, q=q: stT[:, j0:j0 + n, q * 128:(q + 1) * 128], first_write=(q == 0))
                    if q == 3:
                        c0 = tb0 + (tt // 4) * 512
                        c.dma("sp", out_T[:, c0:c0 + 512].rearrange("(c p) t -> p c t", p=128), stT[:], key="st_stT", r=[stT], pw=[out_T])
        self.end()

    def p5(self):
        self.ple(0, self.hB, self.hBT, self.hA, self.hAT)

    def p10(self):
        self.ple(1, self.hA, self.hAT, self.y, None)

    def sin_of(self, ang, out, wk, ki):
        c = self.c
        C1 = 6.28125
        C2 = TWO_PI - C1
        c.I("dve", "tensor_scalar", r=[ang], w=[wk], out=wk[:], in0=ang[:], scalar1=1.0 / TWO_PI, scalar2=0.5, op0=ALU.mult, op1=ALU.add)
        c.I("dve", "tensor_copy", r=[wk], w=[ki], out=ki[:], in_=wk[:])
        c.I("dve", "tensor_copy", r=[ki], w=[wk], out=wk[:], in_=ki[:])
        c.I("dve", "scalar_tensor_tensor", r=[wk, ang], w=[out], out=out[:], in0=wk[:], scalar=-C1, in1=ang[:], op0=ALU.mult, op1=ALU.add)
        c.I("dve", "scalar_tensor_tensor", r=[wk, out], w=[out], out=out[:], in0=wk[:], scalar=-C2, in1=out[:], op0=ALU.mult, op1=ALU.add)
        c.I("dve", "tensor_scalar", r=[out], w=[wk], out=wk[:], in0=out[:], scalar1=-math.pi, scalar2=None, op0=ALU.is_lt)
        c.I("dve", "scalar_tensor_tensor", r=[wk, out], w=[out], out=out[:], in0=wk[:], scalar=TWO_PI, in1=out[:], op0=ALU.mult, op1=ALU.add)
        c.I("dve", "tensor_scalar", r=[out], w=[wk], out=wk[:], in0=out[:], scalar1=math.pi, scalar2=None, op0=ALU.is_gt)
        c.I("dve", "scalar_tensor_tensor", r=[wk, out], w=[out], out=out[:], in0=wk[:], scalar=-TWO_PI, in1=out[:], op0=ALU.mult, op1=ALU.add)
        c.I("dve", "tensor_scalar", r=[out], w=[out], out=out[:], in0=out[:], scalar1=-3.14159, scalar2=3.14159, op0=ALU.max, op1=ALU.min)
        c.I("act", "activation", r=[out], w=[out], out=out[:], in_=out[:], func=AF.Sin)

    def rope(self, src, src_ap, nh, cos_ap, sin_ap, dst, dst_ap, tw, first=True):
        c = self.c
        n = nh * 32
        x1 = src_ap[:, :, 0:32]
        x2 = src_ap[:, :, 32:64]
        cb = bc(cos_ap.unsqueeze(1), [128, nh, 32])
        sb_ = bc(sin_ap.unsqueeze(1), [128, nh, 32])
        t = [tw[:, i * n:(i + 1) * n].rearrange("p (h d) -> p h d", d=32) for i in range(4)]
        c.I("dve", "tensor_tensor", r=[src, self.cosT], w=[tw], out=t[0], in0=x1, in1=cb, op=ALU.mult)
        c.I("dve", "tensor_tensor", r=[src, self.sinT], pw=[tw], out=t[1], in0=x2, in1=sb_, op=ALU.mult)
        c.I("dve", "tensor_tensor", r=[src, self.cosT], pw=[tw], out=t[2], in0=x2, in1=cb, op=ALU.mult)
        c.I("dve", "tensor_tensor", r=[src, self.sinT], pw=[tw], out=t[3], in0=x1, in1=sb_, op=ALU.mult)
        kw = dict(w=[dst]) if first else dict(pw=[dst])
        c.I("dve", "tensor_tensor", r=[tw], out=dst_ap[:, :, 0:32], in0=t[0], in1=t[1], op=ALU.subtract, **kw)
        c.I("dve", "tensor_tensor", r=[tw], pw=[dst], out=dst_ap[:, :, 32:64], in0=t[2], in1=t[3], op=ALU.add)

    def p6(self):
        c = self.c
        W = self.Win
        self.begin()
        TB = 512
        posi = c.sb([128, 32], I32, "posi")
        c.dma("sp", posi[:], W("pos")[:], key="posi", w=[posi])
        posf = c.sb([128, 32], F32, "posf")
        c.I("dve", "tensor_copy", r=[posi], w=[posf], out=posf[:], in_=posi[:])
        invf = self.bcast_load(W("invf").t, 32, "invf")
        ang = c.sb([128, 1024], F32, "ang")
        ang2 = c.sb([128, 1024], F32, "ang2")
        wk = c.sb([128, 1024], F32, "wk")
        ki = c.sb([128, 1024], I32, "ki")
        self.cosT = c.sb([128, 1024], F32, "cosT")
        self.sinT = c.sb([128, 1024], F32, "sinT")
        a3 = ang[:].rearrange("p (n j) -> p n j", j=32)
        c.I("dve", "tensor_tensor", r=[posf, invf], w=[ang], out=a3, in0=bc(posf[:].unsqueeze(2), [128, 32, 32]),
            in1=bc(invf[:].unsqueeze(1), [128, 32, 32]), op=ALU.mult)
        c.I("dve", "tensor_scalar", r=[ang], w=[ang2], out=ang2[:], in0=ang[:], scalar1=math.pi / 2, scalar2=None, op0=ALU.add)
        self.sin_of(ang, self.sinT, wk, ki)
        self.sin_of(ang2, self.cosT, wk, ki)
        cos3 = self.cosT[:].rearrange("p (n j) -> p n j", j=32)
        sin3 = self.sinT[:].rearrange("p (n j) -> p n j", j=32)
        gkv = self.bcast_load(W("kv_norm_g").t, KVR, "gkv")
        gq = self.bcast_load(W("mla_q_norm_g").t, QR, "gq")
        eps_r = c.sb([128, 1], F32, "epsr")
        c.I("pool", "memset", w=[eps_r], ap=eps_r[:], constant=RMS_EPS)
        xt = c.sb([128, 16, TB], BF16, "xt")
        wbig = c.sbn(2, [128, 16, 512], BF16, "wbig")
        wsm = c.sbn(2, [128, 4, 1024], BF16, "wsm")
        ckvT = c.sb([128, 4, TB], BF16, "ckvT")
        cqT = c.sb([128, 4, TB], BF16, "cqT")
        cn = c.sbn(2, [128, 512], F32, "cn")
        cnb = c.sbn(2, [128, 512], BF16, "cnb")
        ssq = c.sbn(2, [128, 4], F32, "ssq")
        rp = c.sbn(2, [128, 1024], F32, "rp")
        tw = c.sbn(2, [128, 2048], F32, "tw")
        krb = c.sbn(2, [128, 64], BF16, "krb")
        krst = c.sb([64, TB], BF16, "krst")
        qrb = c.sbn(2, [128, 1024], BF16, "qrb")
        qrst = c.sb([64, MH, TB], BF16, "qrst")
        fst = c.sbn(3, [128, 512], BF16, "fst")
        wdown = W("kv_w_down").t.rearrange("(kc p) n -> p kc n", p=128)
        wdq = W("mla_w_dq").t.rearrange("(kc p) n -> p kc n", p=128)
        wrope = W("kv_w_rope").t.rearrange("(kc p) n -> p kc n", p=128)
        wuk = W("kv_w_uk").t.rearrange("(kc p) n -> p kc n", p=128)
        wuv = W("kv_w_uv").t.rearrange("(kc p) n -> p kc n", p=128)
        wuq = W("mla_w_uq").t.rearrange("(kc p) (h d) -> p kc h d", p=128, d=192)
        k = 0
        for blk in range(T // TB):
            tb0 = blk * TB
            c.dma("sp", xt[:], self.hAT[:, tb0:tb0 + TB].rearrange("(kc p) t -> p kc t", p=128), key="xt", w=[xt], r=[self.hAT])
            for (wv_, gbc, dstT) in ((wdown, gkv, ckvT), (wdq, gq, cqT)):
                wt = wbig.next()
                self.wload(wt, wv_, 16, 512, key=wt.b.name, kc_split=2)
                for tt in range(4):
                    ts_ = slice(tt * 128, (tt + 1) * 128)
                    ps = self.psArot.next()
                    for kc in range(16):
                        kw = dict(w=[ps]) if kc == 0 else dict(pw=[ps])
                        c.I("pe", "matmul", r=[xt, wt], out=ps[:], lhsT=xt[:, kc, ts_], rhs=wt[:, kc, :], start=(kc == 0), stop=(kc == 15), **kw)
                    x_ = cn.next()
                    q_ = ssq.next()
                    xb = cnb.next()
                    c.I("act", "activation", r=[ps], w=[x_, q_], out=x_[:], in_=ps[:], func=AF.Square, accum_out=q_[:, 0:1])
                    c.I("act", "activation", r=[q_, eps_r], pw=[q_], out=q_[:, 1:2], in_=q_[:, 0:1], func=AF.Sqrt, bias=eps_r[:, 0:1], scale=1.0 / 512)
                    c.I("dve", "reciprocal", r=[q_], pw=[q_], out=q_[:, 2:3], in_=q_[:, 1:2])
                    c.I("dve", "tensor_scalar", r=[ps, q_], w=[x_], out=x_[:], in0=ps[:], scalar1=q_[:, 2:3], scalar2=None, op0=ALU.mult)
                    c.I("dve", "tensor_tensor", r=[x_, gbc], w=[xb], out=xb[:], in0=x_[:], in1=gbc[:], op=ALU.mult)
                    self.transpose_to(xb, lambda j, xb=xb: xb[:, j * 128:(j + 1) * 128], 4, dstT,
                                      lambda j0, n, tt=tt, dstT=dstT: dstT[:, j0:j0 + n, tt * 128:(tt + 1) * 128], first_write=(tt == 0))
            wt = wbig.next()
            self.wload(wt, wrope, 16, 64, key=wt.b.name)
            for tt in range(4):
                ts_ = slice(tt * 128, (tt + 1) * 128)
                gt = (tb0 // 128) + tt
                ps = self.psArot.next()
                for kc in range(16):
                    kw = dict(w=[ps]) if kc == 0 else dict(pw=[ps])
                    c.I("pe", "matmul", r=[xt, wt], out=ps[:, 0:64], lhsT=xt[:, kc, ts_], rhs=wt[:, kc, 0:64], start=(kc == 0), stop=(kc == 15), **kw)
                r_ = rp.next()
                c.I("act", "activation", r=[ps], w=[r_], out=r_[:, 0:64], in_=ps[:, 0:64], func=AF.Copy)
                kb = krb.next()
                self.rope(r_, r_[:, 0:64].rearrange("p (h d) -> p h d", d=64), 1, cos3[:, gt, :], sin3[:, gt, :], kb,
                          kb[:].rearrange("p (h d) -> p h d", d=64), tw.next())
                pt = self.ptr.next()
                c.I("pe", "transpose", r=[kb, self.idb], w=[pt], out=pt[0:64, 0:128], in_=kb[:], identity=self.idb[:])
                kw = dict(w=[krst]) if tt == 0 else dict(pw=[krst])
                k += 1
                self.evac(k, krst[:, ts_], pt[0:64, 0:128], r=[pt], **kw)
            c.dma("sp", self.krT[:, tb0:tb0 + TB], krst[:], key="st_krst", r=[krst], pw=[self.krT])
            for hg in range(4):
                for kind in range(3):
                    wt = wsm.next()
                    if kind == 0:
                        self.wload(wt, wuk[:, :, hg * 512:(hg + 1) * 512], 4, 512, key=wt.b.name)
                    elif kind == 1:
                        self.wload(wt, wuv[:, :, hg * 512:(hg + 1) * 512], 4, 512, key=wt.b.name)
                    else:
                        for j in range(4):
                            kw = dict(w=[wt]) if j == 0 else dict(pw=[wt])
                            c.dma("pool", wt[:, :, j * 128:(j + 1) * 128], wuq[:, :, hg * 4 + j, 0:128], key=wt.b.name, **kw)
                    if kind == 1:
                        for tt in range(4):
                            ts_ = slice(tt * 128, (tt + 1) * 128)
                            ps = self.psArot.next()
                            for kc in range(4):
                                kw = dict(w=[ps]) if kc == 0 else dict(pw=[ps])
                                c.I("pe", "matmul", r=[ckvT, wt], out=ps[:], lhsT=ckvT[:, kc, ts_], rhs=wt[:, kc, 0:512], start=(kc == 0), stop=(kc == 3), **kw)
                            f_ = fst.next()
                            k += 1
                            self.evac(k, f_[:], ps[:], r=[ps], w=[f_])
                            c.dma("sp", self.v_scr[tb0 + tt * 128:tb0 + (tt + 1) * 128, hg * 512:(hg + 1) * 512], f_[:],
                                  key="st_" + f_.b.name, r=[f_], pw=[self.v_scr])
                    else:
                        srcT = ckvT if kind == 0 else cqT
                        dstD = self.knT if kind == 0 else self.qnT
                        for j in range(4):
                            h = hg * 4 + j
                            ps = self.psArot.next()
                            for kc in range(4):
                                kw = dict(w=[ps]) if kc == 0 else dict(pw=[ps])
                                c.I("pe", "matmul", r=[srcT, wt], out=ps[:], lhsT=wt[:, kc, j * 128:(j + 1) * 128], rhs=srcT[:, kc, :], start=(kc == 0), stop=(kc == 3), **kw)
                            f_ = fst.next()
                            k += 1
                            self.evac(k, f_[:], ps[:], r=[ps], w=[f_])
                            c.dma("sp", dstD[h, :, tb0:tb0 + TB], f_[:], key="st_" + f_.b.name, r=[f_], pw=[dstD])
            wt = wsm.next()
            for hh in range(MH):
                kw = dict(w=[wt]) if hh == 0 else dict(pw=[wt])
                c.dma("pool", wt[:, :, hh * 64:(hh + 1) * 64], wuq[:, :, hh, 128:192], key=wt.b.name, **kw)
            for tt in range(4):
                ts_ = slice(tt * 128, (tt + 1) * 128)
                gt = (tb0 // 128) + tt
                r_ = rp.next()
                for hf in range(2):
                    ps = self.psArot.next()
                    for kc in range(4):
                        kw = dict(w=[ps]) if kc == 0 else dict(pw=[ps])
                        c.I("pe", "matmul", r=[cqT, wt], out=ps[:], lhsT=cqT[:, kc, ts_], rhs=wt[:, kc, hf * 512:(hf + 1) * 512], start=(kc == 0), stop=(kc == 3), **kw)
                    k += 1
                    kw = dict(w=[r_]) if hf == 0 else dict(pw=[r_])
                    self.evac(k, r_[:, hf * 512:(hf + 1) * 512], ps[:], r=[ps], **kw)
                qb = qrb.next()
                self.rope(r_, r_[:].rearrange("p (h d) -> p h d", d=64), MH, cos3[:, gt, :], sin3[:, gt, :], qb,
                          qb[:].rearrange("p (h d) -> p h d", d=64), tw.next())
                for hq in range(2):
                    pt = self.ptr.next()
                    for i in range(8):
                        hh = hq * 8 + i
                        kw = dict(w=[pt]) if i == 0 else dict(pw=[pt])
                        c.I("pe", "transpose", r=[qb, self.idb], out=pt[0:64, i * 128:(i + 1) * 128], in_=qb[:, hh * 64:(hh + 1) * 64],
                            identity=self.idb[:], **kw)
                    k += 1
                    kw = dict(w=[qrst]) if (tt == 0 and hq == 0) else dict(pw=[qrst])
                    self.evac(k, qrst[:, hq * 8:(hq + 1) * 8, ts_], pt[0:64, :].rearrange("p (a b) -> p a b", b=128), r=[pt], **kw)
            c.dma("sp", self.qrT[:, :, tb0:tb0 + TB].rearrange("h d t -> d h t"), qrst[:], key="st_qrst", r=[qrst], pw=[self.qrT])
        self.end()

    def p7(self):
        c = self.c
        self.begin()
        qn = c.sbn(2, [128, SEQ], BF16, "qn")
        kn = c.sbn(2, [128, SEQ], BF16, "kn")
        qr = c.sbn(2, [64, SEQ], BF16, "qr")
        kr = c.sb([64, SEQ], BF16, "kr")
        vh = c.sbn(2, [128, 16, 128], BF16, "vh")
        Pb = c.sbn(2, [128, SEQ], BF16, "Pb")
        PT = c.sbn(2, [128, 16, 128], BF16, "PT")
        otok = c.sb([128, 16, 2048], BF16, "otok")
        st = c.sbn(2, [128, 8], F32, "st")
        stT = c.sbn(2, [128, 16, 128], BF16, "stT")
        S_ = self.psA_t
        for s in range(NSEQ):
            t0 = s * SEQ
            c.dma("sp", kr[:], self.krT[:, t0:t0 + SEQ], key="kr", w=[kr], r=[self.krT])
            for h in range(MH):
                q_ = qn.next(); k_ = kn.next(); qr_ = qr.next(); v_ = vh.next()
                c.dma("sp", q_[:], self.qnT[h, :, t0:t0 + SEQ], key=q_.b.name, w=[q_], r=[self.qnT])
                c.dma("sp", k_[:], self.knT[h, :, t0:t0 + SEQ], key=k_.b.name, w=[k_], r=[self.knT])
                c.dma("sp", qr_[:], self.qrT[h, :, t0:t0 + SEQ], key=qr_.b.name, w=[qr_], r=[self.qrT])
                c.dma("sp", v_[:], self.v_scr[t0:t0 + SEQ, h * 128:(h + 1) * 128].rearrange("(n p) d -> p n d", p=128),
                      key=v_.b.name, w=[v_], r=[self.v_scr])
                for i in range(16):
                    nk = (i + 1) * 128
                    nb = (nk + 511) // 512
                    qs = slice(i * 128, (i + 1) * 128)
                    if nb <= 2:
                        self._sflip = 1 - getattr(self, "_sflip", 0)
                        b0 = 2 * self._sflip
                    else:
                        b0 = 0
                    so = b0 * 512
                    banks = self.psA[b0:b0 + nb]
                    for kb in range(nb):
                        ncols = min(512, nk - kb * 512)
                        ks = slice(kb * 512, kb * 512 + ncols)
                        os_ = slice(so + kb * 512, so + kb * 512 + ncols)
                        last = (kb == nb - 1)
                        c.I("pe", "matmul", r=[q_, k_], w=[banks[kb]], out=S_[:, os_], lhsT=q_[:, qs], rhs=k_[:, ks], start=True, stop=False)
                        c.I("pe", "matmul", r=[qr_, kr], pw=[banks[kb]], out=S_[:, os_], lhsT=qr_[:, qs], rhs=kr[:, ks], start=False, stop=(not last))
                        if last:
                            c.I("pe", "matmul", r=[self.idb, self.maskb], pw=[banks[kb]], out=S_[:, so + i * 128:so + (i + 1) * 128], lhsT=self.idb[:],
                                rhs=self.maskb[:], start=False, stop=True)
                    t_ = st.next()
                    c.I("dve", "reduce_max", r=banks, w=[t_], out=t_[:, 0:1], in_=S_[:, so:so + nk], axis=AX.X)
                    c.I("dve", "tensor_scalar", r=[t_], pw=[t_], out=t_[:, 1:2], in0=t_[:, 0:1], scalar1=-SCALE, scalar2=None, op0=ALU.mult)
                    p_ = Pb.next()
                    c.I("act", "activation", r=banks + [t_], w=[p_], pw=[t_], out=p_[:, 0:nk], in_=S_[:, so:so + nk], func=AF.Exp,
                        bias=t_[:, 1:2], scale=SCALE, accum_out=t_[:, 2:3])
                    pt_ = PT.next()
                    self.transpose_to(p_, lambda j, p_=p_: p_[:, j * 128:(j + 1) * 128], i + 1, pt_,
                                      lambda j0, n, pt_=pt_: pt_[:, j0:j0 + n, :], first_write=True)
                    po = self.pm.next()
                    for kb in range(i + 1):
                        kw = dict(w=[po]) if kb == 0 else dict(pw=[po])
                        c.I("pe", "matmul", r=[pt_, v_], out=po[:, 0:128], lhsT=pt_[:, kb, :], rhs=v_[:, kb, :], start=(kb == 0), stop=(kb == i), **kw)
                    c.I("dve", "reciprocal", r=[t_], pw=[t_], out=t_[:, 3:4], in_=t_[:, 2:3])
                    kw = dict(w=[otok]) if (h == 0 and i == 0) else dict(pw=[otok])
                    c.I("act", "activation", r=[po, t_], out=otok[:, i, h * 128:(h + 1) * 128], in_=po[:, 0:128], func=AF.Copy,
                        scale=t_[:, 3:4], **kw)
            for i in range(16):
                sT = stT.next()
                self.transpose_to(otok, lambda j, i=i: otok[:, i, j * 128:(j + 1) * 128], 16, sT,
                                  lambda j0, n, sT=sT: sT[:, j0:j0 + n, :], first_write=True)
                c.dma("sp", self.oT[:, t0 + i * 128:t0 + (i + 1) * 128].rearrange("(c p) t -> p c t", p=128), sT[:],
                      key="st_" + sT.b.name, r=[sT], pw=[self.oT])
        self.end()

    def p8(self):
        W = self.Win
        self.linear_ln(self.oT, 16, W("mla_w_o").t, self.hA, W("ln1_g").t[1], W("ln1_b").t[1], self.hB, self.hBT, NCOL=512)

    def p9(self):
        c = self.c
        W = self.Win
        NCH, CH = 23, 512
        NSLOT = NCH * CH
        resid, xT_unused = self.hB, self.hBT
        out_tok, out_T = self.hA, self.hAT
        Xs = c.dram("moe_xs", [NSLOT, D], BF16)
        Ys = c.dram("moe_ys", [NSLOT, D], F32)
        WG, WU, WD = W("moe_w_gate"), W("moe_w_up"), W("moe_w_down")
        IOA = bass.IndirectOffsetOnAxis
        outer = ExitStack()
        c.stack = outer
        c.keymap = {}
        NT = T // 128
        M1 = c.sb([128, NT, NE], F32, "M1")
        M2 = c.sb([128, NT, NE], F32, "M2")
        WT = c.sb([128, NT, 2], F32, "WT")
        RK = c.sb([128, NT, NE], F32, "RK")
        POSf = c.sb([128, NT, 2], F32, "POSf")
        POSi = c.sb([128, NT, 2], I32, "POSi")
        idxi = c.sb([128, NCH, 22], I32, "idxi")
        c.stack = ExitStack()
        wr = c.sb([128, 16, NE], F32, "wr")
        c.dma("sp", wr[:], W("moe_w_router").t.rearrange("(kc p) e -> p kc e", p=128), key="wr", w=[wr])
        br = self.bcast_load(W("moe_b_router").t, NE, "br")
        hTf = c.sb([128, 16, 128], F32, "hTf")
        res = c.sbn(2, [128, D], F32, "res")
        hbf_all = c.sb([128, T // 128, D], BF16, "hbfall")
        rt = c.sbn(2, [128, 64], F32, "rt")
        run = c.sb([128, NE], F32, "run")
        c.I("pool", "memset", w=[run], ap=run[:], constant=0.0)
        suf = c.sb([128, 128], F32, "suf")
        c.I("pool", "affine_select", r=[self.ones_f], w=[suf], out=suf[:], in_=self.ones_f[:], pattern=[[1, 128]],
            compare_op=ALU.is_gt, fill=0.0, base=0, channel_multiplier=-1)
        k = 0
        for tt in range(NT):
            t0 = tt * 128
            rs = res.next()
            c.dma("sp", rs[:], resid[t0:t0 + 128, :], key=rs.b.name, w=[rs], r=[resid])
            hkw = dict(w=[hbf_all]) if tt == 0 else dict(pw=[hbf_all])
            c.I("act", "activation", r=[rs], out=hbf_all[:, tt, :], in_=rs[:], func=AF.Copy, **hkw)
            for q in range(4):
                pm = self.pm.next()
                for i in range(4):
                    kc = q * 4 + i
                    kw = dict(w=[pm]) if i == 0 else dict(pw=[pm])
                    c.I("pe", "transpose", r=[rs, self.idf], out=pm[:, i * 128:(i + 1) * 128],
                        in_=rs[:, kc * 128:(kc + 1) * 128], identity=self.idf[:], **kw)
                k += 1
                kw = dict(w=[hTf]) if q == 0 else dict(pw=[hTf])
                self.evac(k, hTf[:, q * 4:(q + 1) * 4, :], pm[:].rearrange("p (a b) -> p a b", b=128), r=[pm], **kw)
            pm = self.pm.next()
            for kc in range(16):
                kw = dict(w=[pm]) if kc == 0 else dict(pw=[pm])
                c.I("pe", "matmul", r=[hTf, wr], out=pm[:, 0:NE], lhsT=hTf[:, kc, :], rhs=wr[:, kc, :],
                    start=(kc == 0), stop=(kc == 15), **kw)
            r_ = rt.next()
            lg = r_[:, 0:8]; oh1 = M1[:, tt, :]; l2 = r_[:, 16:24]; oh2 = M2[:, tt, :]
            m1 = r_[:, 32:33]; m2 = r_[:, 33:34]; dd = r_[:, 34:35]; ee = r_[:, 35:36]; e1 = r_[:, 36:37]
            w1 = WT[:, tt, 0:1]; w2 = WT[:, tt, 1:2]; mm = r_[:, 40:48]
            c.I("dve", "tensor_tensor", r=[pm, br], w=[r_], out=lg, in0=pm[:, 0:NE], in1=br[:], op=ALU.add)
            c.I("dve", "reduce_max", r=[r_], pw=[r_], out=m1, in_=lg, axis=AX.X)
            c.I("dve", "tensor_scalar", r=[r_], pw=[M1], out=oh1, in0=lg, scalar1=m1, scalar2=None, op0=ALU.is_equal)
            c.I("dve", "scalar_tensor_tensor", r=[r_, M1], pw=[r_], out=l2, in0=oh1, scalar=-1.0e30, in1=lg, op0=ALU.mult, op1=ALU.add)
            c.I("dve", "reduce_max", r=[r_], pw=[r_], out=m2, in_=l2, axis=AX.X)
            c.I("dve", "tensor_scalar", r=[r_], pw=[M2], out=oh2, in0=l2, scalar1=m2, scalar2=None, op0=ALU.is_equal)
            c.I("dve", "tensor_tensor", r=[r_], pw=[r_], out=dd, in0=m2, in1=m1, op=ALU.subtract)
            c.I("act", "activation", r=[r_], pw=[r_], out=ee, in_=dd, func=AF.Exp)
            c.I("dve", "tensor_scalar", r=[r_], pw=[r_], out=e1, in0=ee, scalar1=1.0, scalar2=None, op0=ALU.add)
            c.I("dve", "reciprocal", r=[r_], pw=[WT], out=w1, in_=e1)
            c.I("dve", "tensor_tensor", r=[r_, WT], pw=[WT], out=w2, in0=ee, in1=w1, op=ALU.mult)
            c.I("dve", "tensor_tensor", r=[M1, M2], pw=[r_], out=mm, in0=oh1, in1=oh2, op=ALU.add)
            pr_ = self.pm.next()
            c.I("pe", "matmul", r=[suf, r_], w=[pr_], out=pr_[:, 0:NE], lhsT=suf[:], rhs=mm, start=True, stop=True)
            c.I("pe", "matmul", r=[self.ones_f, r_], pw=[pr_], out=pr_[:, 8:16], lhsT=self.ones_f[:], rhs=mm, start=True, stop=True)
            c.I("dve", "tensor_tensor", r=[pr_, run], pw=[RK], out=RK[:, tt, :], in0=pr_[:, 0:NE], in1=run[:], op=ALU.add)
            c.I("dve", "tensor_tensor", r=[pr_, run], w=[run], out=run[:], in0=pr_[:, 8:16], in1=run[:], op=ALU.add)
        cs = c.sb([128, 64], F32, "cs")
        ci_ = c.sb([128, 8], I32, "ci")
        x_ = cs[:, 0:8]; xr = cs[:, 8:16]; fx = cs[:, 16:24]; pc = cs[:, 24:32]; off = cs[:, 32:40]; eoff = cs[:, 40:48]
        c.I("dve", "tensor_scalar", r=[run], w=[cs], out=x_, in0=run[:], scalar1=511.0, scalar2=1.0 / 512, op0=ALU.add, op1=ALU.mult)
        c.I("dve", "tensor_copy", r=[cs], w=[ci_], out=ci_[:], in_=x_)
        c.I("dve", "tensor_copy", r=[ci_], pw=[cs], out=xr, in_=ci_[:])
        c.I("dve", "tensor_tensor", r=[cs], pw=[cs], out=fx, in0=xr, in1=x_, op=ALU.is_gt)
        c.I("dve", "tensor_tensor", r=[cs], pw=[cs], out=xr, in0=xr, in1=fx, op=ALU.subtract)
        c.I("dve", "tensor_scalar", r=[cs], pw=[cs], out=pc, in0=xr, scalar1=512.0, scalar2=None, op0=ALU.mult)
        c.I("pool", "memset", r=[cs], pw=[cs], ap=cs[:, 32:33], constant=0.0)
        for e in range(1, NE):
            c.I("dve", "tensor_tensor", r=[cs], pw=[cs], out=cs[:, 32 + e:33 + e], in0=cs[:, 31 + e:32 + e], in1=cs[:, 23 + e:24 + e], op=ALU.add)
        c.I("dve", "tensor_tensor", r=[cs], pw=[cs], out=eoff, in0=off, in1=pc, op=ALU.add)
        cst_i = c.sb([128, NCH], I32, "csti")
        c.I("pool", "iota", w=[cst_i], out=cst_i[:], pattern=[[CH, NCH]], base=0, channel_multiplier=0)
        cst = c.sb([128, NCH], F32, "cst")
        c.I("dve", "tensor_copy", r=[cst_i], w=[cst], out=cst[:], in_=cst_i[:])
        ec = c.sb([128, NCH], F32, "ec")
        tq = c.sb([128, NCH], F32, "tq")
        for e in range(NE):
            if e == 0:
                c.I("dve", "tensor_scalar", r=[cst, cs], w=[ec], out=ec[:], in0=cst[:], scalar1=cs[:, 40:41], scalar2=None, op0=ALU.is_ge)
            else:
                c.I("dve", "tensor_scalar", r=[cst, cs], w=[tq], out=tq[:], in0=cst[:], scalar1=cs[:, 40 + e:41 + e], scalar2=None, op0=ALU.is_ge)
                c.I("dve", "tensor_tensor", r=[ec, tq], w=[ec], out=ec[:], in0=ec[:], in1=tq[:], op=ALU.add)
        c.I("dve", "tensor_scalar", r=[ec], w=[ec], out=ec[:], in0=ec[:], scalar1=float(NE - 1), scalar2=None, op0=ALU.min)
        pcol_i = c.sb([128, 1], I32, "pcoli")
        c.I("pool", "iota", w=[pcol_i], out=pcol_i[:], pattern=[[0, 1]], base=0, channel_multiplier=1)
        pcol = c.sb([128, 1], F32, "pcol")
        c.I("dve", "tensor_copy", r=[pcol_i], w=[pcol], out=pcol[:], in_=pcol_i[:])
        fco_i = c.sb([128, 22], I32, "fcoi")
        c.I("pool", "iota", w=[fco_i], out=fco_i[:], pattern=[[128, 22]], base=0, channel_multiplier=0)
        fco = c.sb([128, 22], F32, "fco")
        c.I("dve", "tensor_copy", r=[fco_i], w=[fco], out=fco[:], in_=fco_i[:])
        base = c.sb([128, NCH], F32, "base")
        c.I("dve", "tensor_scalar", r=[ec, pcol], w=[base], out=base[:], in0=ec[:], scalar1=float(DFE), scalar2=pcol[:, 0:1], op0=ALU.mult, op1=ALU.add)
        idxf = c.sb([128, NCH, 22], F32, "idxf")
        c.I("dve", "tensor_tensor", r=[base, fco], w=[idxf], out=idxf[:], in0=bc(base[:].unsqueeze(2), [128, NCH, 22]),
            in1=bc(fco[:].unsqueeze(1), [128, NCH, 22]), op=ALU.add)
        c.I("dve", "tensor_copy", r=[idxf], w=[idxi], out=idxi[:], in_=idxf[:])
        t8 = c.sbn(2, [128, 16], F32, "t8")
        for tt in range(NT):
            t0 = tt * 128
            a_ = t8.next()
            c.I("dve", "tensor_tensor", r=[RK, cs], w=[a_], out=a_[:, 0:8], in0=RK[:, tt, :], in1=off, op=ALU.add)
            for kk, M in enumerate((M1, M2)):
                c.I("dve", "tensor_tensor", r=[a_, M], pw=[a_], out=a_[:, 8:16], in0=a_[:, 0:8], in1=M[:, tt, :], op=ALU.mult)
                kw = dict(w=[POSf]) if (tt == 0 and kk == 0) else dict(pw=[POSf])
                c.I("dve", "reduce_sum", r=[a_], out=POSf[:, tt, kk:kk + 1], in_=a_[:, 8:16], axis=AX.X, **kw)
        c.I("dve", "tensor_copy", r=[POSf], w=[POSi], out=POSi[:], in_=POSf[:])
        for tt in range(NT):
            for kk in range(2):
                c.I("pool", "indirect_dma_start", r=[hbf_all, POSi], pw=[Xs], key="sc_hbf", out=Xs[:, :],
                    out_offset=IOA(ap=POSi[:, tt, kk:kk + 1], axis=0), in_=hbf_all[:, tt, :], in_offset=None)
        c.S.barrier()
        c.stack.close()
        c.stack = ExitStack()
        xins = c.sbn(2, [128, 4, D], BF16, "xin")
        xts = c.sbn(2, [128, 16, CH], BF16, "xt")
        wgs = c.sbn(2, [128, 16, 128], BF16, "wg")
        wus = c.sbn(2, [128, 16, 128], BF16, "wu")
        wdl = [c.sb([128, D], BF16, "wd") for _ in range(11)]
        actT = c.sb([128, 11, CH], BF16, "actT")
        acc = c.sb([128, 4, D], F32, "acc")
        sg = c.sbn(2, [128, 512], F32, "sg")
        for ch in range(NCH):
            s0 = ch * CH
            xin = xins.next()
            xt = xts.next()
            c.dma("sp", xin[:], Xs[s0:s0 + CH, :].rearrange("(n p) d -> p n d", p=128), key=xin.b.name, w=[xin], r=[Xs])
            for n in range(4):
                self.transpose_to(xin, lambda j, n=n, xin=xin: xin[:, n, j * 128:(j + 1) * 128], 16, xt,
                                  lambda j0, nn, n=n, xt=xt: xt[:, j0:j0 + nn, n * 128:(n + 1) * 128], first_write=(n == 0))
            for u in range(2):
                for fl in range(11):
                    fc = u * 11 + fl
                    wg = wgs.next()
                    wu = wus.next()
                    ix = IOA(ap=idxi[:, ch, fc:fc + 1], axis=0)
                    c.I("pool", "indirect_dma_start", r=[idxi], w=[wg], key=wg.b.name, out=wg[:].rearrange("p a b -> p (a b)"),
                        out_offset=None, in_=WG[:, :], in_offset=ix)
                    c.I("pool", "indirect_dma_start", r=[idxi], w=[wu], key=wu.b.name, out=wu[:].rearrange("p a b -> p (a b)"),
                        out_offset=None, in_=WU[:, :], in_offset=IOA(ap=idxi[:, ch, fc:fc + 1], axis=0))
                    c.I("pool", "indirect_dma_start", r=[idxi], w=[wdl[fl]], key=wdl[fl].b.name, out=wdl[fl][:],
                        out_offset=None, in_=WD[:, :], in_offset=IOA(ap=idxi[:, ch, fc:fc + 1], axis=0))
                    psg = self.psArot.next()
                    for kc in range(16):
                        kw = dict(w=[psg]) if kc == 0 else dict(pw=[psg])
                        c.I("pe", "matmul", r=[xt, wg], out=psg[:], lhsT=wg[:, kc, :], rhs=xt[:, kc, :], start=(kc == 0), stop=(kc == 15), **kw)
                    psu = self.psArot.next()
                    for kc in range(16):
                        kw = dict(w=[psu]) if kc == 0 else dict(pw=[psu])
                        c.I("pe", "matmul", r=[xt, wu], out=psu[:], lhsT=wu[:, kc, :], rhs=xt[:, kc, :], start=(kc == 0), stop=(kc == 15), **kw)
                    s1 = sg.next()
                    c.I("act", "activation", r=[psg], w=[s1], out=s1[:], in_=psg[:], func=AF.Silu)
                    kw = dict(w=[actT]) if fl == 0 else dict(pw=[actT])
                    c.I("dve", "tensor_tensor", r=[s1, psu], out=actT[:, fl, :], in0=s1[:], in1=psu[:], op=ALU.mult, **kw)
                for dblk in range(4):
                    ds_ = slice(dblk * 512, (dblk + 1) * 512)
                    for tt in range(4):
                        ps = self.psArot.next()
                        for fl in range(11):
                            kw = dict(w=[ps]) if fl == 0 else dict(pw=[ps])
                            c.I("pe", "matmul", r=[actT, wdl[fl]], out=ps[:], lhsT=actT[:, fl, tt * 128:(tt + 1) * 128],
                                rhs=wdl[fl][:, ds_], start=(fl == 0), stop=(fl == 10), **kw)
                        akw = dict(w=[acc]) if (u == 0 and dblk == 0 and tt == 0) else dict(pw=[acc])
                        if u == 0:
                            k += 1
                            self.evac(k, acc[:, tt, ds_], ps[:], r=[ps], **akw)
                        else:
                            c.I("dve", "tensor_tensor", r=[ps, acc], out=acc[:, tt, ds_], in0=ps[:], in1=acc[:, tt, ds_], op=ALU.add, **akw)
            c.dma("sp", Ys[s0:s0 + CH, :].rearrange("(n p) d -> p n d", p=128), acc[:], key="st_acc", r=[acc], pw=[Ys])
        c.S.barrier()
        c.stack.close()
        c.stack = ExitStack()
        g_bc = self.bcast_load(W("ln2_g").t[1], D, "lng")
        b_bc = self.bcast_load(W("ln2_b").t[1], D, "lnb")
        g1s = c.sbn(2, [128, D], F32, "g1")
        g2s = c.sbn(2, [128, D], F32, "g2")
        res = c.sbn(2, [128, D], F32, "res")
        hb = c.sbn(2, [128, D], BF16, "hb")
        stT = c.sb([128, 16, 512], BF16, "stT")
        for tt in range(NT):
            t0 = tt * 128
            g1 = g1s.next()
            g2 = g2s.next()
            rs = res.next()
            c.I("pool", "indirect_dma_start", r=[Ys, POSi], w=[g1], key=g1.b.name, out=g1[:], out_offset=None, in_=Ys[:, :],
                in_offset=IOA(ap=POSi[:, tt, 0:1], axis=0))
            c.I("pool", "indirect_dma_start", r=[Ys, POSi], w=[g2], key=g2.b.name, out=g2[:], out_offset=None, in_=Ys[:, :],
                in_offset=IOA(ap=POSi[:, tt, 1:2], axis=0))
            c.dma("sp", rs[:], resid[t0:t0 + 128, :], key=rs.b.name, w=[rs], r=[resid])
            c.I("dve", "tensor_scalar", r=[g1, WT], w=[g1], out=g1[:], in0=g1[:], scalar1=WT[:, tt, 0:1], scalar2=None, op0=ALU.mult)
            c.I("dve", "scalar_tensor_tensor", r=[g2, WT, g1], w=[g1], out=g1[:], in0=g2[:], scalar=WT[:, tt, 1:2], in1=g1[:],
                op0=ALU.mult, op1=ALU.add)
            c.I("dve", "scalar_tensor_tensor", r=[rs, g1], w=[g1], out=g1[:], in0=rs[:], scalar=ALPHA, in1=g1[:], op0=ALU.mult, op1=ALU.add)
            b = hb.next()
            self.ln_rows(g1, g1[:], g_bc, b_bc, g1, g1[:], b, b[:])
            c.dma("sp", out_tok[t0:t0 + 128, :], g1[:], key="st_" + g1.b.name, r=[g1], pw=[out_tok])
            q = tt % 4
            self.transpose_to(b, lambda j, b=b: b[:, j * 128:(j + 1) * 128], 16, stT,
                              lambda j0, n, q=q: stT[:, j0:j0 + n, q * 128:(q + 1) * 128], first_write=(q == 0))
            if q == 3:
                tb0 = (tt // 4) * 512
                c.dma("sp", out_T[:, tb0:tb0 + 512].rearrange("(c p) t -> p c t", p=128), stT[:], key="st_stT", r=[stT], pw=[out_T])
        c.S.barrier()
        c.stack.close()
        outer.close()
        c.stack = None

    def build(self):
        c = self.c
        self.consts()
        g = self.gstack
        c.stack = g
        self.one_col = c.sb([128, 1], F32, "onecol")
        c.I("pool", "memset", w=[self.one_col], ap=self.one_col[:], constant=1.0)
        self.ln_setup()
        c.stack = None
        for ph in ("p1", "p2", "p3", "p4", "p5", "p6", "p7", "p8", "p9", "p10"):
            if self.phase(ph) and hasattr(self, ph):
                getattr(self, ph)()
        c.S.barrier()
        c.S.emit()


def build_program(dbg=(), phases=None):
    nc = bass.Bass("TRN2", target_bir_lowering=False)
    pr = Prog(nc, dbg=dbg, phases=phases)
    pr.build()
    return nc, pr


def _tile_rows(w, nfc):
    w = np.asarray(w)
    return w.reshape(16, 128, nfc, 128).transpose(2, 1, 0, 3).reshape(nfc * 128, D)


def _tile_gu(w):
    w = np.asarray(w)
    return w.reshape(NE, 16, 128, 22, 128).transpose(0, 3, 2, 1, 4).reshape(NE * DFE, D)


def make_in_maps(inputs):
    f = lambda a: np.ascontiguousarray(np.asarray(a))
    invf = (10000.0 ** (-np.arange(0, 64, 2, dtype=np.float32) / 64)).astype(np.float32)
    shared = {
        "invf": invf,
        "ssm_w_in": f(inputs["ssm_w_in"][0]),
        "conv_w": f(np.asarray(inputs["ssm_conv_w"][0]).reshape(4, 48, 128).transpose(2, 1, 0)),
        "conv_b": f(np.asarray(inputs["ssm_conv_b"][0]).reshape(48, 128).T),
        "ssm_dt_bias": f(inputs["ssm_dt_bias"][0]), "ssm_a_log": f(inputs["ssm_a_log"][0]),
        "ssm_d": f(inputs["ssm_d"][0]), "ssm_norm_g": f(inputs["ssm_norm_g"][0]),
        "ssm_w_out": f(inputs["ssm_w_out"][0]),
        "kv_w_down": f(inputs["kv_w_down"]), "kv_norm_g": f(inputs["kv_norm_g"]), "kv_w_rope": f(inputs["kv_w_rope"]),
        "kv_w_uk": f(inputs["kv_w_uk"]), "kv_w_uv": f(inputs["kv_w_uv"]),
        "mla_w_dq": f(inputs["mla_w_dq"][0]), "mla_q_norm_g": f(inputs["mla_q_norm_g"][0]),
        "mla_w_uq": f(inputs["mla_w_uq"][0]), "mla_w_o": f(inputs["mla_w_o"][0]),
        "ffn_w_gate": f(_tile_rows(inputs["ffn_w_gate"][0], 44)), "ffn_w_up": f(_tile_rows(inputs["ffn_w_up"][0], 44)),
        "ffn_w_down": f(inputs["ffn_w_down"][0]),
        "moe_w_router": f(inputs["moe_w_router"][0]), "moe_b_router": f(inputs["moe_b_router"][0]),
        "moe_w_gate": f(_tile_gu(inputs["moe_w_gate"][0])), "moe_w_up": f(_tile_gu(inputs["moe_w_up"][0])),
        "moe_w_down": f(np.asarray(inputs["moe_w_down"][0]).reshape(NE * DFE, D)),
        "ln1_g": f(inputs["ln1_g"]), "ln1_b": f(inputs["ln1_b"]), "ln2_g": f(inputs["ln2_g"]), "ln2_b": f(inputs["ln2_b"]),
        "ple_w_proj": f(inputs["ple_w_proj"]), "ple_w_gate": f(inputs["ple_w_gate"]),
    }
    x = np.asarray(inputs["x"])
    p = np.asarray(inputs["p"])
    pos = np.asarray(inputs["positions"])
    maps = []
    for ci in range(NCORES):
        b0 = ci * NSEQ
        m = dict(shared)
        m["x"] = f(x[b0:b0 + NSEQ].reshape(T, D))
        m["p"] = f(p[:, b0:b0 + NSEQ].reshape(2, T, PLE))
        m["pos"] = f(pos[b0:b0 + NSEQ].reshape(T // 128, 128).T.astype(np.int32))
        maps.append(m)
    return maps


def kernel(**inputs):
    nc, pr = build_program()
    maps = make_in_maps(inputs)
    used = set(pr._W.keys())
    maps = [{k: v for k, v in m.items() if k in used} for m in maps]
    res = run_bass_kernel_spmd(nc, maps, core_ids=list(range(NCORES)))
    out = np.stack([r["y"].reshape(NSEQ, SEQ, D) for r in res.results], 0).reshape(NCORES * NSEQ, SEQ, D)
    return out.astype(np.float32)
```
